# Optimizing a Trainium2 kernel written in Bass

```python
import math
import jax, jax.numpy as jnp
from jax import lax
import numpy as np

D_MODEL = 4096
BATCH = 8
SEQ = 2048
DEPTH = 1

DIFF_HEADS = 8
DIFF_HEAD_DIM = 128
DIFF_V_DIM = 2 * DIFF_HEAD_DIM
DIFF_WIDTH = DIFF_HEADS * DIFF_V_DIM
MLA_HEADS = 16
MLA_Q_RANK = 768
MLA_KV_RANK = 512
MLA_NOPE_DIM = 128
MLA_ROPE_DIM = 64
MLA_V_DIM = 128
MLA_WIDTH = MLA_HEADS * MLA_V_DIM
ROPE_THETA = 10000.0
N_BRANCHES = 2
Q_DIFF_COLS = 2 * DIFF_HEADS * DIFF_HEAD_DIM
K_DIFF_COLS = 2 * DIFF_HEADS * DIFF_HEAD_DIM
V_DIFF_COLS = DIFF_HEADS * DIFF_V_DIM
GATE_COLS = N_BRANCHES * D_MODEL
IN_COLS = (Q_DIFF_COLS + K_DIFF_COLS + V_DIFF_COLS + MLA_Q_RANK + MLA_KV_RANK
           + MLA_ROPE_DIM + GATE_COLS)
MIX_WIDTH = DIFF_WIDTH + MLA_WIDTH
N_EXPERTS = 64
TOP_K = 6
N_GROUPS = 8
TOPK_GROUPS = 4
EXPERT_DIM = 512
SHARED_DIM = 512
ROUTED_SCALE = 2.5
EXPERT_BLOCK = 128
Q_BLOCK = 128
NORM_EPS = 1e-6
N_MOD = 6

kernel_name = "hybrid_diffattn_mla_moe_adaln"


def rms_norm(x, gain):
    x32 = x.astype(jnp.float32)
    y = x32 * lax.rsqrt(jnp.mean(x32 * x32, axis=-1, keepdims=True) + NORM_EPS)
    return (y * gain.astype(jnp.float32)).astype(x.dtype)


def rotary(t, pos):
    half = t.shape[-1] // 2
    inv_freq = ROPE_THETA ** (-jnp.arange(half, dtype=jnp.float32) / half)
    ang = pos.astype(jnp.float32)[:, None, :, None] * inv_freq
    cos, sin = jnp.cos(ang), jnp.sin(ang)
    t32 = t.astype(jnp.float32)
    t1, t2 = t32[..., :half], t32[..., half:]
    return jnp.concatenate([t1 * cos - t2 * sin, t2 * cos + t1 * sin], axis=-1).astype(t.dtype)


def alibi_slopes(n_heads):
    return 2.0 ** (-8.0 * jnp.arange(1, n_heads + 1, dtype=jnp.float32) / n_heads)


def lambda_init(layer):
    return 0.8 - 0.6 * math.exp(-0.3 * layer)


def to_query_blocks(t):
    b, h, s, d = t.shape
    return t.reshape(b, h, s // Q_BLOCK, Q_BLOCK, d).transpose(2, 0, 1, 3, 4)


def from_query_blocks(t):
    nqb, b, h, qb, d = t.shape
    return t.transpose(1, 2, 0, 3, 4).reshape(b, h, nqb * qb, d)


def diff_attention(q, k, v, pos, lam):
    b, h2, s, d = q.shape
    nqb = s // Q_BLOCK
    scale = d ** -0.5
    slopes = jnp.repeat(alibi_slopes(DIFF_HEADS), 2)
    key_idx = jnp.arange(s)

    def block(args):
        qb, qpos, start = args
        sc = jnp.einsum('bhqd,bhkd->bhqk', qb, k, preferred_element_type=jnp.float32) * scale
        dist = jnp.abs(qpos[:, :, None] - pos[:, None, :]).astype(jnp.float32)
        sc = sc - slopes[None, :, None, None] * dist[:, None]
        causal = (start + jnp.arange(Q_BLOCK))[:, None] >= key_idx[None, :]
        sc = jnp.where(causal[None, None], sc, -jnp.inf)
        p = jax.nn.softmax(sc, axis=-1).reshape(b, DIFF_HEADS, 2, Q_BLOCK, s)
        a = p[:, :, 0] - lam * p[:, :, 1]
        return jnp.einsum('bhqk,bhkv->bhqv', a.astype(v.dtype), v)

    qpos = pos.reshape(b, nqb, Q_BLOCK).transpose(1, 0, 2)
    starts = jnp.arange(nqb) * Q_BLOCK
    out = lax.map(block, (to_query_blocks(q), qpos, starts))
    return from_query_blocks(out)


def mla_attention(q_nope, q_rope, k_nope, k_rope, v):
    s = q_nope.shape[2]
    nqb = s // Q_BLOCK
    scale = (MLA_NOPE_DIM + MLA_ROPE_DIM) ** -0.5
    key_idx = jnp.arange(s)

    def block(args):
        qn, qr, start = args
        sc = (jnp.einsum('bhqd,bhkd->bhqk', qn, k_nope, preferred_element_type=jnp.float32)
              + jnp.einsum('bhqd,bkd->bhqk', qr, k_rope, preferred_element_type=jnp.float32)) * scale
        causal = (start + jnp.arange(Q_BLOCK))[:, None] >= key_idx[None, :]
        sc = jnp.where(causal[None, None], sc, -jnp.inf)
        p = jax.nn.softmax(sc, axis=-1)
        return jnp.einsum('bhqk,bhkv->bhqv', p.astype(v.dtype), v)

    starts = jnp.arange(nqb) * Q_BLOCK
    out = lax.map(block, (to_query_blocks(q_nope), to_query_blocks(q_rope), starts))
    return from_query_blocks(out)


def hybrid_mixer(h, pos, w_in, diff_lambda, diff_subln, mla_q_norm, mla_w_uq,
                 mla_kv_norm, mla_w_ukv, w_out, lam_init):
    b, s, _ = h.shape
    proj = h @ w_in
    splits = np.cumsum([Q_DIFF_COLS, K_DIFF_COLS, V_DIFF_COLS, MLA_Q_RANK, MLA_KV_RANK,
                        MLA_ROPE_DIM]).tolist()
    q_d, k_d, v_d, c_q, c_kv, k_pe, g_logits = jnp.split(proj, splits, axis=-1)

    q_d = q_d.reshape(b, s, 2 * DIFF_HEADS, DIFF_HEAD_DIM).transpose(0, 2, 1, 3)
    k_d = k_d.reshape(b, s, 2 * DIFF_HEADS, DIFF_HEAD_DIM).transpose(0, 2, 1, 3)
    v_d = v_d.reshape(b, s, DIFF_HEADS, DIFF_V_DIM).transpose(0, 2, 1, 3)
    lp = diff_lambda.astype(jnp.float32)
    lam = jnp.exp(jnp.sum(lp[0] * lp[1])) - jnp.exp(jnp.sum(lp[2] * lp[3])) + lam_init
    o_d = diff_attention(q_d, k_d, v_d, pos, lam)
    o_d = rms_norm(o_d, diff_subln) * (1.0 - lam_init)
    o_d = o_d.transpose(0, 2, 1, 3).reshape(b, s, DIFF_WIDTH)

    q = (rms_norm(c_q, mla_q_norm) @ mla_w_uq).reshape(b, s, MLA_HEADS, MLA_NOPE_DIM + MLA_ROPE_DIM)
    q = q.transpose(0, 2, 1, 3)
    q_nope, q_rope = q[..., :MLA_NOPE_DIM], rotary(q[..., MLA_NOPE_DIM:], pos)
    kv = (rms_norm(c_kv, mla_kv_norm) @ mla_w_ukv).reshape(b, s, MLA_HEADS, MLA_NOPE_DIM + MLA_V_DIM)
    kv = kv.transpose(0, 2, 1, 3)
    k_nope, v_m = kv[..., :MLA_NOPE_DIM], kv[..., MLA_NOPE_DIM:]
    k_rope = rotary(k_pe[:, None], pos)[:, 0]
    o_m = mla_attention(q_nope, q_rope, k_nope, k_rope, v_m)
    o_m = o_m.transpose(0, 2, 1, 3).reshape(b, s, MLA_WIDTH)

    g = jax.nn.sigmoid(g_logits.astype(jnp.float32)).astype(h.dtype).reshape(b, s, N_BRANCHES, D_MODEL)
    y_d = o_d @ w_out[:DIFF_WIDTH]
    y_m = o_m @ w_out[DIFF_WIDTH:]
    return g[:, :, 0] * y_d + g[:, :, 1] * y_m


def swiglu(x, w1, w3, w2):
    return (jax.nn.silu(x @ w1) * (x @ w3)) @ w2


def moe_ffn(h, router_w, router_bias, exp_w1, exp_w3, exp_w2, shared_w1, shared_w3, shared_w2):
    b, s, d = h.shape
    n = b * s
    hf = h.reshape(n, d)

    scores = jax.nn.sigmoid(jnp.einsum('nd,de->ne', hf, router_w, preferred_element_type=jnp.float32))
    sel = scores + router_bias.astype(jnp.float32)
    grp_score = lax.top_k(sel.reshape(n, N_GROUPS, N_EXPERTS // N_GROUPS), 2)[0].sum(-1)
    _, top_grp = lax.top_k(grp_score, TOPK_GROUPS)
    grp_mask = jnp.any(top_grp[..., None] == jnp.arange(N_GROUPS), axis=1)
    expert_mask = jnp.repeat(grp_mask, N_EXPERTS // N_GROUPS, axis=1)
    _, idx = lax.top_k(jnp.where(expert_mask, sel, -jnp.inf), TOP_K)
    wts = jnp.take_along_axis(scores, idx, axis=1)
    wts = wts / jnp.sum(wts, axis=-1, keepdims=True) * ROUTED_SCALE

    nk = n * TOP_K
    e_flat = idx.reshape(nk)
    tok_flat = jnp.arange(nk, dtype=jnp.int32) // TOP_K
    w_flat = wts.reshape(nk)
    order = jnp.argsort(e_flat)
    e_sorted = e_flat[order]
    sizes = jnp.bincount(e_flat, length=N_EXPERTS)
    padded = (sizes + EXPERT_BLOCK - 1) // EXPERT_BLOCK * EXPERT_BLOCK
    pad_end = jnp.cumsum(padded)
    pad_start = pad_end - padded
    grp_start = jnp.cumsum(sizes) - sizes
    dest = pad_start[e_sorted] + (jnp.arange(nk) - grp_start[e_sorted])
    n_blocks = -(-nk // EXPERT_BLOCK) + N_EXPERTS
    rows = n_blocks * EXPERT_BLOCK
    row_tok = jnp.zeros((rows,), jnp.int32).at[dest].set(tok_flat[order])
    row_w = jnp.zeros((rows,), jnp.float32).at[dest].set(w_flat[order])
    block_expert = jnp.minimum(
        jnp.searchsorted(pad_end, jnp.arange(n_blocks) * EXPERT_BLOCK, side='right'), N_EXPERTS - 1)

    def expert_block(args):
        e, tok, wt = args
        yb = swiglu(hf[tok], exp_w1[e], exp_w3[e], exp_w2[e])
        return yb * wt[:, None].astype(yb.dtype)

    out = lax.map(expert_block, (block_expert, row_tok.reshape(n_blocks, EXPERT_BLOCK),
                                 row_w.reshape(n_blocks, EXPERT_BLOCK)))
    routed = jax.ops.segment_sum(out.reshape(rows, d), row_tok, num_segments=n)
    shared = swiglu(hf, shared_w1, shared_w3, shared_w2)
    return (routed + shared).reshape(b, s, d)


def setup_inputs(seed: int = 0) -> dict:
    key = jax.random.key(seed)
    ks = jax.random.split(key, 24)

    def nrm(k, shape, scale):
        return jax.random.normal(k, shape, jnp.float32) * scale

    d = D_MODEL
    return {
        "x": nrm(ks[0], (BATCH, SEQ, d), 1.0),
        "c": nrm(ks[1], (BATCH, d), 1.0),
        "positions": jnp.arange(SEQ, dtype=jnp.int32)[None, :]
                     + jax.random.randint(ks[2], (BATCH, 1), 0, 1024, dtype=jnp.int32),
        "w_ada": nrm(ks[3], (DEPTH, d, N_MOD * d), 0.5 * d ** -0.5),
        "b_ada": nrm(ks[4], (DEPTH, N_MOD * d), 0.02),
        "norm_attn": 1.0 + nrm(ks[5], (DEPTH, d), 0.02),
        "w_in": nrm(ks[6], (DEPTH, d, IN_COLS), d ** -0.5),
        "diff_lambda": nrm(ks[7], (DEPTH, 4, DIFF_HEAD_DIM), 0.1),
        "diff_subln": 1.0 + nrm(ks[8], (DEPTH, DIFF_V_DIM), 0.02),
        "mla_q_norm": 1.0 + nrm(ks[9], (DEPTH, MLA_Q_RANK), 0.02),
        "mla_w_uq": nrm(ks[10], (DEPTH, MLA_Q_RANK, MLA_HEADS * (MLA_NOPE_DIM + MLA_ROPE_DIM)),
                         MLA_Q_RANK ** -0.5),
        "mla_kv_norm": 1.0 + nrm(ks[11], (DEPTH, MLA_KV_RANK), 0.02),
        "mla_w_ukv": nrm(ks[12], (DEPTH, MLA_KV_RANK, MLA_HEADS * (MLA_NOPE_DIM + MLA_V_DIM)),
                          MLA_KV_RANK ** -0.5),
        "w_out": nrm(ks[13], (DEPTH, MIX_WIDTH, d), MIX_WIDTH ** -0.5),
        "norm_ffn": 1.0 + nrm(ks[14], (DEPTH, d), 0.02),
        "router_w": nrm(ks[15], (DEPTH, d, N_EXPERTS), d ** -0.5),
        "router_bias": nrm(ks[16], (DEPTH, N_EXPERTS), 0.01),
        "exp_w1": nrm(ks[17], (DEPTH, N_EXPERTS, d, EXPERT_DIM), d ** -0.5),
        "exp_w3": nrm(ks[18], (DEPTH, N_EXPERTS, d, EXPERT_DIM), d ** -0.5),
        "exp_w2": nrm(ks[19], (DEPTH, N_EXPERTS, EXPERT_DIM, d), EXPERT_DIM ** -0.5),
        "shared_w1": nrm(ks[20], (DEPTH, d, SHARED_DIM), d ** -0.5),
        "shared_w3": nrm(ks[21], (DEPTH, d, SHARED_DIM), d ** -0.5),
        "shared_w2": nrm(ks[22], (DEPTH, SHARED_DIM, d), SHARED_DIM ** -0.5),
        "final_norm": 1.0 + nrm(ks[23], (d,), 0.02),
    }


def reference(x, c, positions, w_ada, b_ada, norm_attn, w_in, diff_lambda, diff_subln,
              mla_q_norm, mla_w_uq, mla_kv_norm, mla_w_ukv, w_out, norm_ffn, router_w,
              router_bias, exp_w1, exp_w3, exp_w2, shared_w1, shared_w3, shared_w2, final_norm):
    for l in range(DEPTH):
        mod = jnp.einsum('bd,de->be', jax.nn.silu(c), w_ada[l]) + b_ada[l]
        sh_a, sc_a, gt_a, sh_f, sc_f, gt_f = [m[:, None, :] for m in jnp.split(mod, N_MOD, axis=-1)]

        h = rms_norm(x, norm_attn[l]) * (1.0 + sc_a) + sh_a
        x = x + gt_a * hybrid_mixer(h, positions, w_in[l], diff_lambda[l], diff_subln[l],
                                    mla_q_norm[l], mla_w_uq[l], mla_kv_norm[l], mla_w_ukv[l],
                                    w_out[l], lambda_init(l))

        h = rms_norm(x, norm_ffn[l]) * (1.0 + sc_f) + sh_f
        x = x + gt_f * moe_ffn(h, router_w[l], router_bias[l], exp_w1[l], exp_w3[l], exp_w2[l],
                               shared_w1[l], shared_w3[l], shared_w2[l])
    return rms_norm(x, final_norm)
```

```python
import numpy as np
import ml_dtypes
from contextlib import ExitStack
import concourse.bass as bass
import concourse.mybir as mybir
from concourse.bass_utils import run_bass_kernel_spmd

F32 = mybir.dt.float32
BF16 = mybir.dt.bfloat16
I32 = mybir.dt.int32
AF = mybir.ActivationFunctionType
ALU = mybir.AluOpType
AX = mybir.AxisListType

ENG_NAMES = ("pe", "act", "dve", "pool", "sp")
EPOCH_MAX = 30000


class Tl:
    __slots__ = ("name", "w", "r", "fw", "ds")

    def __init__(self, name=""):
        self.name = name
        self.w = {}
        self.r = {}
        self.fw = {}
        self.ds = None


class DSem:
    __slots__ = ("name", "count", "sems", "nops")

    def __init__(self, name):
        self.name = name
        self.nops = 0


class Op:
    __slots__ = ("eng", "fn", "waits", "idx", "needed", "dsem", "didx", "ticket")


class Sched:
    def __init__(self, nc, same_eng_wait=True):
        self.nc = nc
        self.ops = {e: [] for e in ENG_NAMES}
        self.waited = {e: {} for e in ENG_NAMES}
        self.same_eng_wait = same_eng_wait
        self.dsems = []
        self.last = {}
        self.free_ds = []
        self.tile_ds = []

    def dsem(self, name):
        d = DSem(name)
        self.dsems.append(d)
        return d

    def op(self, eng, fn, reads=(), writes=(), pwrites=(), dsem=None):
        if isinstance(dsem, Tl):
            t = dsem
            if t.ds is None:
                t.ds = self.free_ds.pop() if self.free_ds else self.dsem(f"t{len(self.dsems)}")
                self.tile_ds.append(t)
            dsem = t.ds
        o = Op()
        o.eng = eng
        o.fn = fn
        o.needed = False
        o.dsem = dsem
        o.idx = len(self.ops[eng])
        deps = {}

        def add(d):
            for k, ent in d.items():
                cur = deps.get(k)
                if cur is None or cur[0] < ent[0]:
                    deps[k] = ent

        for t in reads:
            add(t.w)
        for t in writes:
            add(t.w)
            add(t.r)
        for t in pwrites:
            add(t.r)
            add(t.fw)
        waits = []
        wd = self.waited[eng]
        for k, (order, dop) in deps.items():
            if k[0] == "e" and k[1] == eng:
                if eng == "pe" or not self.same_eng_wait:
                    continue
            if wd.get(k, -1) >= order:
                continue
            wd[k] = order
            dop.needed = True
            waits.append(dop)
        o.waits = waits
        if dsem is not None:
            o.didx = dsem.nops
            dsem.nops += 1
            key = ("d", id(dsem))
            order = o.didx
        else:
            key = ("e", eng)
            order = o.idx
        ent = (order, o)
        self.last[key] = ent
        for t in writes:
            t.w = {key: ent}
            t.fw = {key: ent}
            t.r = {}
        for t in pwrites:
            t.w[key] = ent
        for t in reads:
            t.r[key] = ent
        self.ops[eng].append(o)
        return o

    def barrier(self):
        t = Tl("bar")
        t.w = dict(self.last)
        for e in ENG_NAMES:
            self.op(e, None, reads=[t])
        for tt in self.tile_ds:
            self.free_ds.append(tt.ds)
            tt.ds = None
        self.tile_ds = []

    def emit(self):
        nc = self.nc
        with ExitStack() as es:
            for e in ENG_NAMES:
                n = 0
                epoch = 0
                sems = [es.enter_context(nc.semaphore(f"c_{e}_0"))]
                for o in self.ops[e]:
                    if o.dsem is None and o.needed:
                        if o.fn is None:
                            o.fn = lambda eng: eng.nop()
                        if n >= EPOCH_MAX:
                            epoch += 1
                            n = 0
                            sems.append(es.enter_context(nc.semaphore(f"c_{e}_{epoch}")))
                        n += 1
                        o.ticket = (sems[epoch], n)
            per = {}
            for e in ENG_NAMES:
                for o in self.ops[e]:
                    if o.dsem is not None:
                        per.setdefault(id(o.dsem), []).append(o)
            for d in self.dsems:
                lst = sorted(per.get(id(d), []), key=lambda o: o.didx)
                if not lst:
                    continue
                d.sems = [es.enter_context(nc.semaphore(f"d_{d.name}_0"))]
                cnt = 0
                ep = 0
                for o in lst:
                    if cnt + 16 > EPOCH_MAX:
                        ep += 1
                        cnt = 0
                        d.sems.append(es.enter_context(nc.semaphore(f"d_{d.name}_{ep}")))
                    cnt += 16
                    o.ticket = (d.sems[ep], cnt)
            engs = {"pe": "tensor", "act": "scalar", "dve": "vector", "pool": "gpsimd", "sp": "sync"}
            with nc.Block() as block:
                def mk(e):
                    ops = self.ops[e]

                    def body(engine):
                        for o in ops:
                            for dop in o.waits:
                                s, v = dop.ticket
                                engine.wait_ge(s, v)
                            if o.fn is None:
                                continue
                            ins = o.fn(engine)
                            if o.dsem is not None:
                                ins.then_inc(o.ticket[0], 16)
                            elif o.needed:
                                ins.then_inc(o.ticket[0], 1)
                    return body
                for e in ENG_NAMES:
                    getattr(block, engs[e])(mk(e))
        return nc


D = 4096
SEQ = 2048
NT = SEQ // 128
NDC = D // 128
IN_COLS = 15680
OFF_Q, OFF_K, OFF_V, OFF_CQ, OFF_CKV, OFF_KPE, OFF_G = 0, 2048, 4096, 6144, 6912, 7424, 7488
EPS = 1e-6
LAM_INIT = 0.8 - 0.6 * 1.0
NE = 64
CAP = 1024

WEIGHT_NAMES = ["w_ada", "b_ada", "norm_attn", "w_in", "diff_lambda", "diff_subln", "mla_q_norm",
                "mla_w_uq", "mla_kv_norm", "mla_w_ukv", "w_out", "norm_ffn", "router_w", "router_bias",
                "exp_w1", "exp_w3", "exp_w2", "shared_w1", "shared_w3", "shared_w2", "final_norm"]
WEIGHT_SHAPES = {
    "w_ada": [D, 6 * D], "b_ada": [6 * D], "norm_attn": [D], "w_in": [D, IN_COLS], "diff_lambda": [4, 128],
    "diff_subln": [256], "mla_q_norm": [768], "mla_w_uq": [768, 3072], "mla_kv_norm": [512],
    "mla_w_ukv": [512, 4096], "w_out": [D, D], "norm_ffn": [D], "router_w": [D, NE], "router_bias": [NE],
    "exp_w1": [NE, D, 512], "exp_w3": [NE, D, 512], "exp_w2": [NE, 512, D], "shared_w1": [D, 512],
    "shared_w3": [D, 512], "shared_w2": [512, D], "final_norm": [D],
}


def stage_weights(stage):
    base = ["w_ada", "b_ada", "norm_attn", "norm_ffn", "w_in"]
    if stage <= 2:
        return base
    base = base + ["diff_lambda", "diff_subln", "mla_q_norm", "mla_w_uq", "mla_kv_norm", "mla_w_ukv"]
    if stage <= 4:
        return base
    base = base + ["w_out", "router_w", "router_bias"]
    if stage <= 5:
        return base
    return WEIGHT_NAMES


def build(stage=99, debug_out=()):
    nc = bass.Bass("TRN2", target_bir_lowering=False)
    S = Sched(nc)
    es = ExitStack()
    W = {}
    wnames = stage_weights(stage)
    for n in wnames:
        W[n] = nc.dram_tensor(n, WEIGHT_SHAPES[n], F32, kind="ExternalInput").ap()
    x_in = nc.dram_tensor("x", [SEQ, D], F32, kind="ExternalInput").ap()
    c_in = nc.dram_tensor("c", [D], F32, kind="ExternalInput").ap()
    pos_in = nc.dram_tensor("pos", [SEQ], I32, kind="ExternalInput").ap()
    ident_in = nc.dram_tensor("ident", [128, 128], F32, kind="ExternalInput").ap()
    rmat_in = nc.dram_tensor("rmat", [128, 64], F32, kind="ExternalInput").ap()
    out_d = nc.dram_tensor("out", [SEQ, D], F32, kind="ExternalOutput").ap()

    def scratch(name, shape, dt):
        kind = "ExternalOutput" if name in debug_out else "Internal"
        return nc.dram_tensor(name, shape, dt, kind=kind).ap()

    def sb(name, shape, dt, stack=es):
        return stack.enter_context(nc.sbuf_tensor(name, shape, dt))

    def ps(name, shape, dt, stack=es):
        return stack.enter_context(nc.psum_tensor(name, shape, dt))

    modv = scratch("modv", [8, D], F32)
    qdT = scratch("qdT", [16, 128, SEQ], BF16)
    kdT = scratch("kdT", [16, 128, SEQ], BF16)
    vd = scratch("vd", [SEQ, 2048], BF16)
    cqT = scratch("cqT", [6, 128, SEQ], F32)
    ckvT = scratch("ckvT", [4, 128, SEQ], F32)
    kpeT = scratch("kpeT", [64, SEQ], F32)
    kpeswT = scratch("kpeswT", [64, SEQ], F32)
    gates = scratch("gates", [SEQ, 2 * D], BF16)
    T_modv = Tl(); T_qdT = Tl(); T_kdT = Tl(); T_vd = Tl(); T_cqT = Tl(); T_ckvT = Tl(); T_kpeT = Tl(); T_gates = Tl()

    ident_f = sb("ident_f", [128, 128], F32)
    ident_b = sb("ident_b", [128, 128], BF16)
    modT = sb("modT", [128, 256], F32)
    T_ident = Tl(); T_modT = Tl()
    d_misc = S.dsem("misc")
    S.op("sp", lambda e: e.dma_start(out=ident_f[:], in_=ident_in), writes=[T_ident], dsem=T_ident)
    S.op("dve", lambda e: e.tensor_copy(out=ident_b[:], in_=ident_f[:]), reads=[T_ident], writes=[T_ident])

    PB = [ps(f"pb{i}", [128, 512], F32) for i in range(8)]
    T_PB = [Tl(f"pb{i}") for i in range(8)]

    with ExitStack() as p0:
        cT = sb("cT", [128, 32], F32, p0)
        bT = sb("bT", [128, 192], F32, p0)
        nT = sb("nT", [128, 64], F32, p0)
        wa = [sb(f"wa{i}", [128, 32, 256], F32, p0) for i in range(2)]
        T_cT = Tl(); T_bT = Tl(); T_nT = Tl(); T_wa = [Tl(), Tl()]
        d_wa = [S.dsem("wa0"), S.dsem("wa1")]

        def small_T(eng, dst, src_vec, n, tl):
            S.op(eng, lambda e: e.dma_start(out=dst, in_=src_vec.rearrange("(j p) -> p j", p=128),
                                            allow_slow_non_contiguous=True), writes=[tl], dsem=tl)
        small_T("sp", cT[:], c_in, 32, T_cT)
        S.op("sp", lambda e: e.dma_start(out=bT[:], in_=W["b_ada"].rearrange("(j p) -> p j", p=128),
                                         allow_slow_non_contiguous=True), writes=[T_bT], dsem=T_bT)
        S.op("sp", lambda e: e.dma_start(out=nT[:, 0:32], in_=W["norm_attn"].rearrange("(j p) -> p j", p=128),
                                         allow_slow_non_contiguous=True), pwrites=[T_nT], dsem=T_nT)
        S.op("sp", lambda e: e.dma_start(out=nT[:, 32:64], in_=W["norm_ffn"].rearrange("(j p) -> p j", p=128),
                                         allow_slow_non_contiguous=True), pwrites=[T_nT], dsem=T_nT)
        S.op("act", lambda e: e.activation(out=cT[:], in_=cT[:], func=AF.Silu), reads=[T_cT], writes=[T_cT])
        NB0 = 96
        pm = PB[0]

        def load_wa(eb):
            i = eb % 2
            S.op("sp", lambda e: e.dma_start(out=wa[i][:], in_=W["w_ada"][:, eb * 256:(eb + 1) * 256]
                                             .rearrange("(j p) c -> p j c", p=128)),
                 writes=[T_wa[i]], dsem=d_wa[i])
        load_wa(0)
        for eb in range(NB0):
            if eb + 1 < NB0:
                load_wa(eb + 1)
            i = eb % 2
            for half in range(2):
                col = eb * 2 + half
                for j in range(32):
                    S.op("pe", lambda e, i=i, j=j, half=half, col=col: e.matmul(
                        pm[:, col:col + 1], lhsT=wa[i][:, j, half * 128:(half + 1) * 128], rhs=cT[:, j:j + 1],
                        start=(j == 0), stop=(j == 31)),
                        reads=[T_wa[i], T_cT], writes=[T_PB[0]])
        S.op("dve", lambda e: e.tensor_tensor(out=modT[:, 0:192], in0=pm[:, 0:192], in1=bT[:], op=ALU.add),
             reads=[T_PB[0], T_bT], writes=[T_modT])
        S.op("dve", lambda e: e.scalar_tensor_tensor(out=modT[:, 192:224], in0=modT[:, 32:64], scalar=1.0, in1=nT[:, 0:32],
                                                     op0=ALU.add, op1=ALU.mult), reads=[T_modT, T_nT], writes=[T_modT])
        S.op("dve", lambda e: e.scalar_tensor_tensor(out=modT[:, 224:256], in0=modT[:, 128:160], scalar=1.0, in1=nT[:, 32:64],
                                                     op0=ALU.add, op1=ALU.mult), reads=[T_modT, T_nT], writes=[T_modT])
        S.op("sp", lambda e: e.dma_start(out=modv.rearrange("i (j p) -> p i j", p=128), in_=modT[:].rearrange("p (i j) -> p i j", j=32),
                                         allow_slow_non_contiguous=True), reads=[T_modT], writes=[T_modv], dsem=T_modv)
        S.barrier()
    if stage <= 0:
        S.emit()
        es.close()
        return nc

    hstack = ExitStack()
    hT = sb("hT", [128, NDC, SEQ], BF16, hstack)
    T_hT = [Tl(f"hT{g}") for g in range(4)]
    with ExitStack() as p1:
        xt = [sb(f"xt{i}", [128, D], F32, p1) for i in range(2)]
        junk = sb("junk", [128, D], BF16, p1)
        xs = sb("xs", [128, 4, D], BF16, p1)
        st = sb("st", [128, NT], F32, p1)
        T_xt = [Tl(), Tl()]; T_junk = Tl(); T_xs = [Tl() for _ in range(4)]; T_st = [Tl() for _ in range(NT)]
        d_xt = [S.dsem("xt0"), S.dsem("xt1")]
        tpb = [PB[1], PB[2]]
        T_tp = [T_PB[1], T_PB[2]]
        ev = 0
        for g in range(4):
            for tt in range(4):
                t = 4 * g + tt
                i = t % 2
                S.op("sp", lambda e, i=i, t=t: e.dma_start(out=xt[i][:], in_=x_in[t * 128:(t + 1) * 128, :]),
                     writes=[T_xt[i]], dsem=d_xt[i])
                S.op("act", lambda e, i=i, t=t: e.activation(out=junk[:], in_=xt[i][:], func=AF.Square, accum_out=st[:, t:t + 1]),
                     reads=[T_xt[i]], writes=[T_junk, T_st[t]])
                S.op("dve", lambda e, t=t: e.tensor_scalar(out=st[:, t:t + 1], in0=st[:, t:t + 1], scalar1=1.0 / D, scalar2=EPS,
                                                           op0=ALU.mult, op1=ALU.add), reads=[T_st[t]], writes=[T_st[t]])
                S.op("act", lambda e, t=t: e.activation(out=st[:, t:t + 1], in_=st[:, t:t + 1], func=AF.Sqrt),
                     reads=[T_st[t]], writes=[T_st[t]])
                S.op("dve", lambda e, t=t: e.reciprocal(out=st[:, t:t + 1], in_=st[:, t:t + 1]), reads=[T_st[t]], writes=[T_st[t]])
                S.op("act", lambda e, i=i, t=t, tt=tt: e.activation(out=xs[:, tt, :], in_=xt[i][:], func=AF.Copy, scale=st[:, t:t + 1]),
                     reads=[T_xt[i], T_st[t]], writes=[T_xs[tt]])
            for dc in range(NDC):
                k = dc % 2
                tpv = tpb[k][:].bitcast(BF16)
                for tt in range(4):
                    S.op("pe", lambda e, tpv=tpv, tt=tt, dc=dc: e.transpose(tpv[:, tt * 128:(tt + 1) * 128],
                                                                           xs[:, tt, dc * 128:(dc + 1) * 128], ident_b[:]),
                         reads=[T_xs[tt], T_ident], writes=[T_tp[k]] if tt == 0 else (), pwrites=() if tt == 0 else [T_tp[k]])
                if ev % 2 == 0:
                    S.op("dve", lambda e, tpv=tpv, dc=dc, g=g: e.tensor_scalar(
                        out=hT[:, dc, g * 512:(g + 1) * 512], in0=tpv[:, 0:512], scalar1=modT[:, 192 + dc:193 + dc],
                        scalar2=modT[:, dc:dc + 1], op0=ALU.mult, op1=ALU.add),
                        reads=[T_tp[k], T_modT], pwrites=[T_hT[g]])
                else:
                    S.op("act", lambda e, tpv=tpv, dc=dc, g=g: e.activation(
                        out=hT[:, dc, g * 512:(g + 1) * 512], in_=tpv[:, 0:512], func=AF.Identity,
                        scale=modT[:, 192 + dc:193 + dc], bias=modT[:, dc:dc + 1]),
                        reads=[T_tp[k], T_modT], pwrites=[T_hT[g]])
                ev += 1
        S.barrier()
    if stage <= 1:
        if "hT_dbg" in debug_out:
            hdbg = scratch("hT_dbg", [128, NDC, SEQ], BF16)
            S.op("sp", lambda e: e.dma_start(out=hdbg, in_=hT[:]), reads=T_hT, dsem=Tl())
            S.barrier()
        S.emit()
        es.close()
        return nc

    with ExitStack() as p2:
        CB = 256
        wb = [sb(f"wb{i}", [128, NDC, CB], BF16, p2) for i in range(2)]
        T_wb = [Tl(), Tl()]
        d_wb = [S.dsem("wb0"), S.dsem("wb1")]
        fstage_b = [sb(f"fsb{i}", [128, SEQ], BF16, p2) for i in range(2)]
        fstage_f = [sb(f"fsf{i}", [128, SEQ], F32, p2) for i in range(2)]
        tstage = [sb(f"tst{i}", [128, NT, CB], BF16, p2) for i in range(2)]
        T_fsb = [Tl(), Tl()]; T_fsf = [Tl(), Tl()]; T_tst = [Tl(), Tl()]
        d_fsb = [S.dsem("fsb0"), S.dsem("fsb1")]
        d_fsf = [S.dsem("fsf0"), S.dsem("fsf1")]
        d_tst = [S.dsem("tst0"), S.dsem("tst1")]
        blocks = []
        for c0 in range(0, 2048, CB):
            blocks.append((OFF_Q + c0, CB, "fb", (qdT, T_qdT, c0 // 128)))
        for c0 in range(0, 2048, CB):
            blocks.append((OFF_K + c0, CB, "fb", (kdT, T_kdT, c0 // 128)))
        for c0 in range(0, 768, CB):
            blocks.append((OFF_CQ + c0, CB, "ff", (cqT, T_cqT, c0 // 128)))
        for c0 in range(0, 512, CB):
            blocks.append((OFF_CKV + c0, CB, "ff", (ckvT, T_ckvT, c0 // 128)))
        blocks.append((OFF_KPE, 64, "kpe", None))
        blocks.append((OFF_KPE, 64, "kpesw", None))
        for c0 in range(0, 2048, CB):
            blocks.append((OFF_V + c0, CB, "tv", c0))
        for c0 in range(0, 2 * D, CB):
            blocks.append((OFF_G + c0, CB, "tg", c0))

        def load_wb(bi):
            col0, ncols, kind, info = blocks[bi]
            i = bi % 2
            S.op("pool", lambda e: e.dma_start(out=wb[i][:, :, 0:ncols], in_=W["w_in"][:, col0:col0 + ncols]
                                               .rearrange("(j p) c -> p j c", p=128)),
                 writes=[T_wb[i]], dsem=d_wb[i])
        load_wb(0)
        pbi = 0
        nfb = 0; nff = 0; ntst = 0
        evc = 0
        for bi in range(len(blocks)):
            if bi + 1 < len(blocks):
                load_wb(bi + 1)
            col0, ncols, kind, info = blocks[bi]
            i = bi % 2
            if kind == "kpesw":
                S.op("dve", lambda e, i=i: e.tensor_scalar(out=wb[i][:, :, 64:96], in0=wb[i][:, :, 32:64], scalar1=-1.0, scalar2=None, op0=ALU.mult),
                     reads=[T_wb[i]], writes=[T_wb[i]])
                S.op("dve", lambda e, i=i: e.tensor_copy(out=wb[i][:, :, 96:128], in_=wb[i][:, :, 0:32]),
                     reads=[T_wb[i]], writes=[T_wb[i]])
            if kind in ("fb", "ff", "kpe", "kpesw"):
                nch = 1 if kind in ("kpe", "kpesw") else ncols // 128
                woff = 64 if kind == "kpesw" else 0
                for ch in range(nch):
                    m = 64 if kind in ("kpe", "kpesw") else 128
                    if kind == "fb":
                        stg, T_stg, d_stg = fstage_b[nfb % 2], T_fsb[nfb % 2], d_fsb[nfb % 2]
                        nfb += 1
                    else:
                        stg, T_stg, d_stg = fstage_f[nff % 2], T_fsf[nff % 2], d_fsf[nff % 2]
                        nff += 1
                    for tb in range(4):
                        bank = pbi % 8
                        pbi += 1
                        for dc in range(NDC):
                            S.op("pe", lambda e, bank=bank, i=i, dc=dc, ch=ch, tb=tb, m=m, woff=woff: e.matmul(
                                PB[bank][0:m, :], lhsT=wb[i][:, dc, woff + ch * 128:woff + ch * 128 + m], rhs=hT[:, dc, tb * 512:(tb + 1) * 512],
                                start=(dc == 0), stop=(dc == NDC - 1)),
                                reads=[T_wb[i], T_hT[tb]], writes=[T_PB[bank]])
                        eng = "dve" if evc % 2 == 0 else "act"
                        evc += 1
                        if eng == "dve":
                            S.op("dve", lambda e, bank=bank, stg=stg, tb=tb, m=m: e.tensor_copy(
                                out=stg[0:m, tb * 512:(tb + 1) * 512], in_=PB[bank][0:m, :]),
                                reads=[T_PB[bank]], writes=[T_stg] if tb == 0 else (), pwrites=() if tb == 0 else [T_stg])
                        else:
                            S.op("act", lambda e, bank=bank, stg=stg, tb=tb, m=m: e.copy(
                                out=stg[0:m, tb * 512:(tb + 1) * 512], in_=PB[bank][0:m, :]),
                                reads=[T_PB[bank]], writes=[T_stg] if tb == 0 else (), pwrites=() if tb == 0 else [T_stg])
                    if kind == "kpe":
                        dst, T_dst = kpeT, T_kpeT
                    elif kind == "kpesw":
                        dst, T_dst = kpeswT, T_kpeT
                    else:
                        dst, T_dst = info[0][info[2] + ch], info[1]
                    S.op("sp", lambda e, dst=dst, stg=stg, m=m: e.dma_start(out=dst, in_=stg[0:m, :]),
                         reads=[T_stg], pwrites=[T_dst], dsem=d_stg)
            else:
                stg, T_stg, d_stg = tstage[ntst % 2], T_tst[ntst % 2], d_tst[ntst % 2]
                ntst += 1
                for t in range(NT):
                    bank = pbi % 8
                    pbi += 1
                    for dc in range(NDC):
                        S.op("pe", lambda e, bank=bank, i=i, dc=dc, t=t: e.matmul(
                            PB[bank][:, 0:CB], lhsT=hT[:, dc, t * 128:(t + 1) * 128], rhs=wb[i][:, dc, :],
                            start=(dc == 0), stop=(dc == NDC - 1)),
                            reads=[T_wb[i], T_hT[t // 4]], writes=[T_PB[bank]])
                    if kind == "tg":
                        S.op("act", lambda e, bank=bank, stg=stg, t=t: e.activation(out=stg[:, t, :], in_=PB[bank][:, 0:CB], func=AF.Sigmoid),
                             reads=[T_PB[bank]], writes=[T_stg] if t == 0 else (), pwrites=() if t == 0 else [T_stg])
                    else:
                        S.op("dve", lambda e, bank=bank, stg=stg, t=t: e.tensor_copy(out=stg[:, t, :], in_=PB[bank][:, 0:CB]),
                             reads=[T_PB[bank]], writes=[T_stg] if t == 0 else (), pwrites=() if t == 0 else [T_stg])
                if kind == "tv":
                    dst, T_dst = vd[:, info:info + CB], T_vd
                else:
                    dst, T_dst = gates[:, info:info + CB], T_gates
                S.op("sp", lambda e, dst=dst, stg=stg: e.dma_start(out=dst.rearrange("(t p) c -> p t c", p=128), in_=stg[:]),
                     reads=[T_stg], pwrites=[T_dst], dsem=d_stg)
        S.barrier()
    hstack.close()
    if stage <= 2:
        S.emit()
        es.close()
        return nc
    qnT = scratch("qnT", [16, 128, SEQ], BF16)
    qrT = scratch("qrT", [16, 64, SEQ], BF16)
    knT = scratch("knT", [16, 128, SEQ], BF16)
    krT = scratch("krT", [64, SEQ], BF16)
    vm = scratch("vm", [SEQ, 2048], BF16)
    T_qnT = Tl(); T_qrT = Tl(); T_knT = Tl(); T_krT = Tl(); T_vm = Tl()
    ones_b = sb("ones_b", [128, 128], BF16)
    p34 = ExitStack()
    rm_f = sb("rm_f", [128, 64], F32, p34)
    rm_b = sb("rm_b", [128, 64], BF16, p34)
    cos2 = sb("cos2", [64, SEQ], F32, p34)
    sin2 = sb("sin2", [64, SEQ], F32, p34)
    T_ones = Tl(); T_rm = Tl(); T_cs = Tl()
    S.op("dve", lambda e: e.memset(ones_b[:], 1.0), writes=[T_ones])
    S.op("sp", lambda e: e.dma_start(out=rm_f[:], in_=rmat_in), writes=[T_rm], dsem=T_rm)
    S.op("dve", lambda e: e.tensor_copy(out=rm_b[:], in_=rm_f[:]), reads=[T_rm], writes=[T_rm])
    PI = float(np.pi)
    with ExitStack() as p3a:
        posi = sb("posi", [64, SEQ], I32, p3a)
        ang = sb("ang", [64, SEQ], F32, p3a)
        tmpa = sb("tmpa", [64, SEQ], F32, p3a)
        pidx_i = sb("pidx_i", [64, 1], I32, p3a)
        invf = sb("invf", [64, 1], F32, p3a)
        frac = sb("frac", [64, SEQ], F32, p3a)
        T_posi = Tl(); T_ang = Tl(); T_tmpa = Tl(); T_invf = Tl(); T_frac = Tl()
        S.op("sp", lambda e: e.dma_start(out=posi[:], in_=pos_in.partition_broadcast(64)), writes=[T_posi], dsem=T_posi)
        S.op("pool", lambda e: e.iota(pidx_i[0:32, :], pattern=[[0, 1]], base=0, channel_multiplier=1), pwrites=[T_invf])
        S.op("pool", lambda e: e.iota(pidx_i[32:64, :], pattern=[[0, 1]], base=0, channel_multiplier=1), pwrites=[T_invf])
        S.op("dve", lambda e: e.tensor_copy(out=invf[:], in_=pidx_i[:]), reads=[T_invf], writes=[T_invf])
        S.op("act", lambda e: e.activation(out=invf[:], in_=invf[:], func=AF.Exp, scale=-float(np.log(10000.0)) / 32.0),
             reads=[T_invf], writes=[T_invf])
        S.op("dve", lambda e: e.tensor_scalar(out=invf[:], in0=invf[:], scalar1=1.0 / (2.0 * PI), scalar2=None, op0=ALU.mult),
             reads=[T_invf], writes=[T_invf])
        S.op("dve", lambda e: e.tensor_copy(out=ang[:], in_=posi[:]), reads=[T_posi], writes=[T_ang])
        S.op("dve", lambda e: e.tensor_scalar(out=ang[:], in0=ang[:], scalar1=invf[:, 0:1], scalar2=None, op0=ALU.mult),
             reads=[T_ang, T_invf], writes=[T_ang])
        ki = posi
        for (dst, shift) in ((sin2, 0.0), (cos2, 0.25)):
            S.op("dve", lambda e, shift=shift: e.tensor_scalar(out=tmpa[:], in0=ang[:], scalar1=shift, scalar2=None, op0=ALU.add),
                 reads=[T_ang], writes=[T_tmpa])
            S.op("dve", lambda e: e.tensor_copy(out=ki[:], in_=tmpa[:]), reads=[T_tmpa], writes=[T_posi])
            S.op("dve", lambda e: e.tensor_copy(out=frac[:], in_=ki[:]), reads=[T_posi], writes=[T_frac])
            S.op("dve", lambda e: e.tensor_tensor(out=tmpa[:], in0=tmpa[:], in1=frac[:], op=ALU.subtract),
                 reads=[T_tmpa, T_frac], writes=[T_tmpa])
            S.op("dve", lambda e: e.tensor_single_scalar(out=frac[:], in_=tmpa[:], scalar=0.5, op=ALU.is_gt),
                 reads=[T_tmpa], writes=[T_frac])
            S.op("dve", lambda e: e.tensor_tensor(out=tmpa[:], in0=tmpa[:], in1=frac[:], op=ALU.subtract),
                 reads=[T_tmpa, T_frac], writes=[T_tmpa])
            S.op("dve", lambda e: e.tensor_single_scalar(out=frac[:], in_=tmpa[:], scalar=-0.5, op=ALU.is_lt),
                 reads=[T_tmpa], writes=[T_frac])
            S.op("dve", lambda e: e.tensor_tensor(out=tmpa[:], in0=tmpa[:], in1=frac[:], op=ALU.add),
                 reads=[T_tmpa, T_frac], writes=[T_tmpa])
            S.op("act", lambda e, dst=dst: e.activation(out=dst[:], in_=tmpa[:], func=AF.Sin, scale=2.0 * PI), reads=[T_tmpa], pwrites=[T_cs])
        S.barrier()
    if "cs_dbg" in debug_out:
        csd = scratch("cs_dbg", [2, 64, SEQ], F32)
        S.op("sp", lambda e: e.dma_start(out=csd[0], in_=cos2[:]), reads=[T_cs], dsem=Tl())
        S.op("sp", lambda e: e.dma_start(out=csd[1], in_=sin2[:]), reads=[T_cs], dsem=Tl())
        S.barrier()
        S.emit(); es.close(); return nc

    def rms_bcast(src, nch, n_feat, scr, rbc, T_src, T_scr, T_rbc):
        for ch in range(nch):
            S.op("act", lambda e, ch=ch: e.activation(out=scr[:, ch, :], in_=src[:, ch, :], func=AF.Square),
                 reads=[T_src], writes=[T_scr[ch]])
        for tb in range(4):
            for ch in range(nch):
                S.op("pe", lambda e, ch=ch, tb=tb: e.matmul(PB[0][:, :], lhsT=ones_b[:], rhs=scr[:, ch, tb * 512:(tb + 1) * 512],
                                                            start=(ch == 0), stop=(ch == nch - 1)),
                     reads=[T_ones, T_scr[ch]], writes=[T_PB[0]])
            S.op("dve", lambda e, tb=tb: e.tensor_scalar(out=rbc[:, tb * 512:(tb + 1) * 512], in0=PB[0][:, :], scalar1=1.0 / n_feat,
                                                         scalar2=EPS, op0=ALU.mult, op1=ALU.add),
                 reads=[T_PB[0]], pwrites=[T_rbc])
        S.op("act", lambda e: e.activation(out=rbc[:], in_=rbc[:], func=AF.Sqrt), reads=[T_rbc], writes=[T_rbc])
        S.op("dve", lambda e: e.reciprocal(out=rbc[:], in_=rbc[:]), reads=[T_rbc], writes=[T_rbc])

    def rope_block(t_ps, T_t, sw_ps, T_sw, tb, dst_stage, T_dst_stage, first, tmpu, T_tmpu):
        sl = slice(tb * 512, (tb + 1) * 512)
        S.op("dve", lambda e: e.tensor_tensor(out=tmpu[:], in0=t_ps, in1=cos2[:, sl], op=ALU.mult),
             reads=[T_t, T_cs], writes=[T_tmpu])
        S.op("dve", lambda e: e.tensor_tensor(out=tmpv[:], in0=sw_ps, in1=sin2[:, sl], op=ALU.mult),
             reads=[T_sw, T_cs], writes=[T_tmpv])
        S.op("dve", lambda e: e.tensor_tensor(out=dst_stage[0:64, sl], in0=tmpu[:], in1=tmpv[:], op=ALU.add),
             reads=[T_tmpu, T_tmpv], writes=[T_dst_stage] if first else (), pwrites=() if first else [T_dst_stage])

    tmpv = sb("tmpv", [64, 512], F32, p34)
    T_tmpv = Tl()

    with ExitStack() as p3q:
        cq = sb("cq", [128, 6, SEQ], F32, p3q)
        cqn = sb("cqn", [128, 6, SEQ], BF16, p3q)
        rq = sb("rq", [128, SEQ], F32, p3q)
        wuq = sb("wuq", [128, 6, 3072], BF16, p3q)
        gq = sb("gq", [128, 6], F32, p3q)
        stg = [sb(f"q3s{i}", [128, SEQ], BF16, p3q) for i in range(2)]
        wsw = sb("wsw", [128, 6, 16, 64], BF16, p3q)
        tmpu = sb("tmpu", [64, 512], F32, p3q)
        T_cq = Tl(); T_cqn = [Tl() for _ in range(6)]; T_rq = Tl(); T_wuq = Tl(); T_gq = Tl()
        T_stg = [Tl(), Tl()]; T_tb16 = Tl(); T_tmpu = Tl()
        d_stg = [S.dsem("q3s0"), S.dsem("q3s1")]
        T_wsw = Tl()
        S.op("sp", lambda e: e.dma_start(out=cq[:], in_=cqT.rearrange("c p t -> p c t")), reads=[T_cqT], writes=[T_cq], dsem=T_cq)
        S.op("sp", lambda e: e.dma_start(out=gq[:], in_=W["mla_q_norm"].rearrange("(j p) -> p j", p=128), allow_slow_non_contiguous=True),
             writes=[T_gq], dsem=T_gq)
        for ch in range(6):
            S.op("pool", lambda e, ch=ch: e.dma_start(out=wuq[:, ch, :], in_=W["mla_w_uq"][ch * 128:(ch + 1) * 128, :], max_dma_last_dim=4096),
                 pwrites=[T_wuq], dsem=T_wuq)
        wr = wuq[:].rearrange("p c (h d) -> p c h d", d=192)
        for ch in range(6):
            S.op("dve", lambda e, ch=ch: e.tensor_scalar(out=wsw[:, ch, :, 0:32], in0=wr[:, ch, :, 160:192], scalar1=-1.0, scalar2=None, op0=ALU.mult),
                 reads=[T_wuq], pwrites=[T_wsw])
            S.op("dve", lambda e, ch=ch: e.tensor_copy(out=wsw[:, ch, :, 32:64], in_=wr[:, ch, :, 128:160]),
                 reads=[T_wuq], pwrites=[T_wsw])
        rms_bcast(cq, 6, 768.0, cqn, rq, T_cq, T_cqn, T_rq)
        for ch in range(6):
            S.op("dve", lambda e, ch=ch: e.scalar_tensor_tensor(out=cqn[:, ch, :], in0=cq[:, ch, :], scalar=gq[:, ch:ch + 1], in1=rq[:],
                                                                op0=ALU.mult, op1=ALU.mult),
                 reads=[T_cq, T_gq, T_rq], writes=[T_cqn[ch]])
        if "cqn_dbg" in debug_out:
            cqn_d = scratch("cqn_dbg", [128, 6, SEQ], BF16)
            rq_d = scratch("rq_dbg", [128, SEQ], F32)
            S.op("sp", lambda e: e.dma_start(out=cqn_d, in_=cqn[:]), reads=T_cqn, dsem=Tl())
            S.op("sp", lambda e: e.dma_start(out=rq_d, in_=rq[:]), reads=[T_rq], dsem=Tl())
        ns = 0
        for h in range(0 if "q_norm_only" not in debug_out else 16, 16):
            for part in range(2 if "q_nope_only" not in debug_out else 1):
                m = 128 if part == 0 else 64
                c0 = h * 192 + (0 if part == 0 else 128)
                st_i = ns % 2
                ns += 1
                for tb in range(4):
                    bank = 1 + (tb % 2) + 2 * part
                    for ch in range(6):
                        S.op("pe", lambda e, bank=bank, ch=ch, tb=tb, m=m, c0=c0: e.matmul(
                            PB[bank][0:m, :], lhsT=wuq[:, ch, c0:c0 + m], rhs=cqn[:, ch, tb * 512:(tb + 1) * 512],
                            start=(ch == 0), stop=(ch == 5)), reads=[T_wuq, T_cqn[ch]], writes=[T_PB[bank]])
                    if part == 0:
                        S.op("act", lambda e, bank=bank, tb=tb, st_i=st_i: e.copy(out=stg[st_i][:, tb * 512:(tb + 1) * 512], in_=PB[bank][:, :]),
                             reads=[T_PB[bank]], writes=[T_stg[st_i]] if tb == 0 else (), pwrites=() if tb == 0 else [T_stg[st_i]])
                    elif "rope_plain" in debug_out:
                        S.op("act", lambda e, bank=bank, tb=tb, st_i=st_i: e.copy(out=stg[st_i][0:64, tb * 512:(tb + 1) * 512], in_=PB[bank][0:64, :]),
                             reads=[T_PB[bank]], writes=[T_stg[st_i]] if tb == 0 else (), pwrites=() if tb == 0 else [T_stg[st_i]])
                    else:
                        bsw = 5 + (tb % 2)
                        for ch in range(6):
                            S.op("pe", lambda e, bsw=bsw, ch=ch, tb=tb, h=h: e.matmul(
                                PB[bsw][0:64, :], lhsT=wsw[:, ch, h, :], rhs=cqn[:, ch, tb * 512:(tb + 1) * 512],
                                start=(ch == 0), stop=(ch == 5)), reads=[T_wsw, T_cqn[ch]], writes=[T_PB[bsw]])
                        rope_block(PB[bank][0:64, :], T_PB[bank], PB[bsw][0:64, :], T_PB[bsw], tb, stg[st_i], T_stg[st_i], tb == 0, tmpu, T_tmpu)
                dst, T_dst = (qnT[h], T_qnT) if part == 0 else (qrT[h], T_qrT)
                S.op("sp", lambda e, dst=dst, st_i=st_i, m=m: e.dma_start(out=dst, in_=stg[st_i][0:m, :]),
                     reads=[T_stg[st_i]], pwrites=[T_dst], dsem=d_stg[st_i])
        S.barrier()
    if stage <= 3 and "q_only" in debug_out:
        S.emit(); es.close(); return nc

    with ExitStack() as p3k:
        ckv = sb("ckv", [128, 4, SEQ], F32, p3k)
        ckvn = sb("ckvn", [128, 4, SEQ], BF16, p3k)
        rkv = sb("rkv", [128, SEQ], F32, p3k)
        wukv = sb("wukv", [128, 4, 4096], BF16, p3k)
        gkv = sb("gkv", [128, 4], F32, p3k)
        kpe = sb("kpe", [64, SEQ], F32, p3k)
        kpesw = sb("kpesw", [64, SEQ], F32, p3k)
        kstg = [sb(f"k3s{i}", [128, SEQ], BF16, p3k) for i in range(2)]
        vst = [sb(f"v3s{i}", [128, NT, 512], BF16, p3k) for i in range(2)]
        ktmpu = sb("ktmpuk", [64, 512], F32, p3k)
        T_ckv = Tl(); T_ckvn = [Tl() for _ in range(4)]; T_rkv = Tl(); T_wukv = Tl(); T_gkv = Tl(); T_kpe = Tl()
        T_kkstg = [Tl(), Tl()]; T_vst = [Tl(), Tl()]; T_tb16 = Tl(); T_kktmpu = Tl()
        d_kkstg = [S.dsem("k3s0"), S.dsem("k3s1")]
        d_vst = [S.dsem("v3s0"), S.dsem("v3s1")]
        S.op("sp", lambda e: e.dma_start(out=ckv[:], in_=ckvT.rearrange("c p t -> p c t")), reads=[T_ckvT], writes=[T_ckv], dsem=T_ckv)
        S.op("sp", lambda e: e.dma_start(out=kpe[:], in_=kpeT), reads=[T_kpeT], writes=[T_kpe], dsem=T_kpe)
        S.op("sp", lambda e: e.dma_start(out=kpesw[:], in_=kpeswT), reads=[T_kpeT], pwrites=[T_kpe], dsem=T_kpe)
        S.op("sp", lambda e: e.dma_start(out=gkv[:], in_=W["mla_kv_norm"].rearrange("(j p) -> p j", p=128), allow_slow_non_contiguous=True),
             writes=[T_gkv], dsem=T_gkv)
        for ch in range(4):
            S.op("pool", lambda e, ch=ch: e.dma_start(out=wukv[:, ch, :], in_=W["mla_w_ukv"][ch * 128:(ch + 1) * 128, :], max_dma_last_dim=4096),
                 pwrites=[T_wukv], dsem=T_wukv)
        rms_bcast(ckv, 4, 512.0, ckvn, rkv, T_ckv, T_ckvn, T_rkv)
        for ch in range(4):
            S.op("dve", lambda e, ch=ch: e.scalar_tensor_tensor(out=ckvn[:, ch, :], in0=ckv[:, ch, :], scalar=gkv[:, ch:ch + 1], in1=rkv[:],
                                                                op0=ALU.mult, op1=ALU.mult),
                 reads=[T_ckv, T_gkv, T_rkv], writes=[T_ckvn[ch]])
        for tb in range(4):
            sl = slice(tb * 512, (tb + 1) * 512)
            S.op("dve", lambda e, sl=sl: e.tensor_tensor(out=ktmpu[:], in0=kpe[:, sl], in1=cos2[:, sl], op=ALU.mult),
                 reads=[T_kpe, T_cs], writes=[T_kktmpu])
            S.op("dve", lambda e, sl=sl: e.tensor_tensor(out=tmpv[:], in0=kpesw[:, sl], in1=sin2[:, sl], op=ALU.mult),
                 reads=[T_kpe, T_cs], writes=[T_tmpv])
            S.op("dve", lambda e, sl=sl, tb=tb: e.tensor_tensor(out=kstg[0][0:64, sl], in0=ktmpu[:], in1=tmpv[:], op=ALU.add),
                 reads=[T_kktmpu, T_tmpv], writes=[T_kkstg[0]] if tb == 0 else (), pwrites=() if tb == 0 else [T_kkstg[0]])
        S.op("sp", lambda e: e.dma_start(out=krT, in_=kstg[0][0:64, :]), reads=[T_kkstg[0]], writes=[T_krT], dsem=d_kkstg[0])
        ns = 1
        for h in range(16):
            st_i = ns % 2
            ns += 1
            for tb in range(4):
                bank = 1 + (tb % 2)
                for ch in range(4):
                    S.op("pe", lambda e, bank=bank, ch=ch, tb=tb, h=h: e.matmul(
                        PB[bank][:, :], lhsT=wukv[:, ch, h * 256:h * 256 + 128], rhs=ckvn[:, ch, tb * 512:(tb + 1) * 512],
                        start=(ch == 0), stop=(ch == 3)), reads=[T_wukv, T_ckvn[ch]], writes=[T_PB[bank]])
                S.op("act", lambda e, bank=bank, tb=tb, st_i=st_i: e.copy(out=kstg[st_i][:, tb * 512:(tb + 1) * 512], in_=PB[bank][:, :]),
                     reads=[T_PB[bank]], writes=[T_kkstg[st_i]] if tb == 0 else (), pwrites=() if tb == 0 else [T_kkstg[st_i]])
            S.op("sp", lambda e, h=h, st_i=st_i: e.dma_start(out=knT[h], in_=kstg[st_i][:, :]),
                 reads=[T_kkstg[st_i]], pwrites=[T_knT], dsem=d_kkstg[st_i])
        wv = wukv[:].rearrange("p c (h two d) -> p c h two d", two=2, d=128)
        for g4 in range(4):
            vi = g4 % 2
            for t in range(NT):
                bank = 3 + (t % 2)
                for ch in range(4):
                    S.op("pe", lambda e, bank=bank, ch=ch, t=t, g4=g4: e.matmul(
                        PB[bank][:, :], lhsT=ckvn[:, ch, t * 128:(t + 1) * 128], rhs=wv[:, ch, 4 * g4:4 * g4 + 4, 1, :],
                        start=(ch == 0), stop=(ch == 3)), reads=[T_wukv, T_ckvn[ch]], writes=[T_PB[bank]])
                S.op("dve", lambda e, bank=bank, t=t, vi=vi: e.tensor_copy(out=vst[vi][:, t, :], in_=PB[bank][:, :]),
                     reads=[T_PB[bank]], writes=[T_vst[vi]] if t == 0 else (), pwrites=() if t == 0 else [T_vst[vi]])
            S.op("sp", lambda e, g4=g4, vi=vi: e.dma_start(out=vm[:, g4 * 512:(g4 + 1) * 512].rearrange("(t p) c -> p t c", p=128), in_=vst[vi][:]),
                 reads=[T_vst[vi]], pwrites=[T_vm], dsem=d_vst[vi])
        S.barrier()
    if stage <= 3:
        S.emit()
        es.close()
        return nc
    oT = scratch("oT", [32, 128, SEQ], BF16)
    T_oT = Tl()

    def phase4():
        p4 = ExitStack()
        BIG = 1.0e9
        SC_D = 128.0 ** -0.5
        SC_M = 192.0 ** -0.5
        lamv = sb("lamv", [128, 512], F32, p4)
        lam2 = sb("lam2", [128, 4], F32, p4)
        gsub = sb("gsub", [128, 2], F32, p4)
        maskB = sb("maskB", [128, 4, 512], F32, p4)
        mki = sb("mki", [128, 512], I32, p4)
        posk_i = sb("posk_i", [128, NT], I32, p4)
        posk = sb("posk", [128, NT], F32, p4)
        posq_i = sb("posq_i", [128, 512], I32, p4)
        posq = sb("posq", [128, 512], F32, p4)
        dist = sb("dist", [128, NT, 512], F32, p4)
        T_lam = Tl(); T_gsub = Tl(); T_maskB = Tl(); T_mki = Tl(); T_posk = Tl(); T_posq = Tl()
        T_dist = [Tl() for _ in range(NT)]
        S.op("sp", lambda e: e.dma_start(out=lamv[:], in_=W["diff_lambda"].rearrange("a b -> (a b)").partition_broadcast(128)),
             writes=[T_lam], dsem=T_lam)
        S.op("dve", lambda e: e.tensor_tensor(out=lamv[:, 0:128], in0=lamv[:, 0:128], in1=lamv[:, 128:256], op=ALU.mult), reads=[T_lam], writes=[T_lam])
        S.op("dve", lambda e: e.tensor_tensor(out=lamv[:, 256:384], in0=lamv[:, 256:384], in1=lamv[:, 384:512], op=ALU.mult), reads=[T_lam], writes=[T_lam])
        S.op("dve", lambda e: e.reduce_sum(out=lam2[:, 0:1], in_=lamv[:, 0:128], axis=AX.X), reads=[T_lam], writes=[T_lam])
        S.op("dve", lambda e: e.reduce_sum(out=lam2[:, 1:2], in_=lamv[:, 256:384], axis=AX.X), reads=[T_lam], writes=[T_lam])
        S.op("act", lambda e: e.activation(out=lam2[:, 0:2], in_=lam2[:, 0:2], func=AF.Exp), reads=[T_lam], writes=[T_lam])
        S.op("dve", lambda e: e.tensor_tensor(out=lam2[:, 2:3], in0=lam2[:, 0:1], in1=lam2[:, 1:2], op=ALU.subtract), reads=[T_lam], writes=[T_lam])
        S.op("dve", lambda e: e.tensor_scalar(out=lam2[:, 2:3], in0=lam2[:, 2:3], scalar1=LAM_INIT, scalar2=None, op0=ALU.add), reads=[T_lam], writes=[T_lam])
        S.op("dve", lambda e: e.tensor_scalar(out=lam2[:, 3:4], in0=lam2[:, 2:3], scalar1=-1.0, scalar2=None, op0=ALU.mult), reads=[T_lam], writes=[T_lam])
        S.op("sp", lambda e: e.dma_start(out=gsub[:], in_=W["diff_subln"].rearrange("(j p) -> p j", p=128), allow_slow_non_contiguous=True),
             writes=[T_gsub], dsem=T_gsub)
        S.op("dve", lambda e: e.tensor_scalar(out=gsub[:], in0=gsub[:], scalar1=1.0 - LAM_INIT, scalar2=None, op0=ALU.mult), reads=[T_gsub], writes=[T_gsub])
        for j in range(4):
            S.op("pool", lambda e, j=j: e.iota(mki[:], pattern=[[1, 512]], base=-128 * j, channel_multiplier=-1), writes=[T_mki])
            S.op("dve", lambda e, j=j: e.tensor_copy(out=maskB[:, j, :], in_=mki[:]), reads=[T_mki], pwrites=[T_maskB])
            S.op("dve", lambda e, j=j: e.tensor_single_scalar(out=maskB[:, j, :], in_=maskB[:, j, :], scalar=0.0, op=ALU.is_lt),
                 reads=[T_maskB], pwrites=[T_maskB])
            S.op("dve", lambda e, j=j: e.tensor_scalar(out=maskB[:, j, :], in0=maskB[:, j, :], scalar1=BIG, scalar2=None, op0=ALU.mult),
                 reads=[T_maskB], pwrites=[T_maskB])
        S.op("sp", lambda e: e.dma_start(out=posk_i[:], in_=pos_in.rearrange("(t p) -> p t", p=128), allow_slow_non_contiguous=True),
             writes=[T_posk], dsem=T_posk)
        S.op("dve", lambda e: e.tensor_copy(out=posk[:], in_=posk_i[:]), reads=[T_posk], writes=[T_posk])
        qb_t = [sb(f"a_q{i}", [128, 2, 512], BF16, p4) for i in range(2)]
        kb_t = [sb(f"a_k{i}", [128, 2, SEQ], BF16, p4) for i in range(2)]
        vb_t = [sb(f"a_v{i}", [128, NT, 256], BF16, p4) for i in range(2)]
        qr_t = [sb(f"a_qr{i}", [128, 512], BF16, p4) for i in range(2)]
        kr_t = sb("a_kr", [128, SEQ], BF16, p4)
        T_q = [Tl(), Tl()]; T_k = [Tl(), Tl()]; T_v = [Tl(), Tl()]; T_qr = [Tl(), Tl()]; T_kr = Tl()
        d_q = [S.dsem("aq0"), S.dsem("aq1")]; d_k = [S.dsem("ak0"), S.dsem("ak1")]; d_v = [S.dsem("av0"), S.dsem("av1")]
        d_qr = [S.dsem("aqr0"), S.dsem("aqr1")]
        NPB = 3
        tmp_t = [sb(f"a_tmp{i}", [128, 512], F32, p4) for i in range(NPB)]
        p_t = [sb(f"a_p{i}", [128, 512], BF16, p4) for i in range(NPB)]
        T_tmp = [Tl() for _ in range(NPB)]; T_p = [Tl() for _ in range(NPB)]
        rcp = sb("a_rcp", [128, 512], F32, p4)
        res = sb("a_res", [128, 2, 2, 512], F32, p4)
        av = sb("a_av", [128, 2, 512], F32, p4)
        asq = sb("a_sq", [128, 2, 512], BF16, p4)
        rsd = sb("a_rsd", [128, 512], F32, p4)
        ost = [sb(f"a_o{i}", [128, 512], BF16, p4) for i in range(4)]
        T_rcp = Tl(); T_res = [[Tl(), Tl()], [Tl(), Tl()]]; T_av = [Tl(), Tl()]; T_asq = [Tl(), Tl()]; T_rsd = Tl()
        T_ost = [Tl() for _ in range(4)]
        d_ost = [S.dsem(f"ao{i}") for i in range(4)]
        S.op("dve", lambda e: e.memset(kr_t[:], 0.0), writes=[T_kr])
        for i in range(2):
            S.op("dve", lambda e, i=i: e.memset(qr_t[i][:], 0.0), writes=[T_qr[i]])
        S.op("sp", lambda e: e.dma_start(out=kr_t[0:64, :], in_=krT), reads=[T_krT], pwrites=[T_kr], dsem=T_kr)
        cnt = {"s": 0, "p": 0, "o": 0, "ld": 0}

        def run_head(qb, nkb, s_mms, diff_slope, pv_lhsT, acc_banks, T_srcs):
            for kb in range(nkb):
                sb_i = cnt["s"] % 2
                cnt["s"] += 1
                s_mms(PB[sb_i], T_PB[sb_i], kb)
                pi = cnt["p"] % NPB
                cnt["p"] += 1
                diag = kb >= 4 * qb
                if diff_slope is not None:
                    S.op("dve", lambda e, pi=pi, sb_i=sb_i, kb=kb: e.scalar_tensor_tensor(
                        out=tmp_t[pi][:], in0=dist[:, kb, :], scalar=-diff_slope / SC_D, in1=PB[sb_i][:, :], op0=ALU.mult, op1=ALU.add),
                        reads=[T_dist[kb], T_PB[sb_i]], writes=[T_tmp[pi]])
                    S.op("act", lambda e, pi=pi: e.activation(out=p_t[pi][:], in_=tmp_t[pi][:], func=AF.Exp, scale=SC_D),
                         reads=[T_tmp[pi]], writes=[T_p[pi]])
                elif diag:
                    j = kb - 4 * qb
                    S.op("dve", lambda e, pi=pi, sb_i=sb_i, j=j: e.scalar_tensor_tensor(
                        out=tmp_t[pi][:], in0=maskB[:, j, :], scalar=-1.0e-4, in1=PB[sb_i][:, :], op0=ALU.mult, op1=ALU.add),
                        reads=[T_maskB, T_PB[sb_i]], writes=[T_tmp[pi]])
                    S.op("act", lambda e, pi=pi: e.activation(out=p_t[pi][:], in_=tmp_t[pi][:], func=AF.Exp, scale=SC_M),
                         reads=[T_tmp[pi]], writes=[T_p[pi]])
                else:
                    S.op("act", lambda e, pi=pi, sb_i=sb_i: e.activation(out=p_t[pi][:], in_=PB[sb_i][:, :], func=AF.Exp, scale=SC_M),
                         reads=[T_PB[sb_i]], writes=[T_p[pi]])
                for ai, lh in enumerate(pv_lhsT(kb)):
                    bk = acc_banks[ai]
                    S.op("pe", lambda e, bk=bk, lh=lh, pi=pi, kb=kb: e.matmul(PB[bk][:, :], lhsT=lh, rhs=p_t[pi][:],
                                                                               start=(kb == 0), stop=(kb == nkb - 1)),
                         reads=[T_p[pi]] + T_srcs, writes=[T_PB[bk]])

        for qb in range(4):
            nkb = 4 * qb + 4
            S.op("sp", lambda e, qb=qb: e.dma_start(out=posq_i[:], in_=pos_in[qb * 512:(qb + 1) * 512].partition_broadcast(128)),
                 writes=[T_posq], dsem=T_posq)
            S.op("dve", lambda e: e.tensor_copy(out=posq[:], in_=posq_i[:]), reads=[T_posq], writes=[T_posq])
            for kb in range(nkb):
                S.op("dve", lambda e, kb=kb: e.tensor_scalar(out=dist[:, kb, :], in0=posq[:], scalar1=posk[:, kb:kb + 1], scalar2=None,
                                                             op0=ALU.subtract), reads=[T_posq, T_posk], writes=[T_dist[kb]])
                S.op("dve", lambda e, kb=kb: e.scalar_tensor_tensor(out=dist[:, kb, :], in0=dist[:, kb, :], scalar=-1.0, in1=dist[:, kb, :],
                                                                    op0=ALU.mult, op1=ALU.max), reads=[T_dist[kb]], writes=[T_dist[kb]])
                if kb >= 4 * qb:
                    S.op("dve", lambda e, kb=kb, qb=qb: e.tensor_tensor(out=dist[:, kb, :], in0=dist[:, kb, :], in1=maskB[:, kb - 4 * qb, :], op=ALU.add),
                         reads=[T_maskB], writes=[T_dist[kb]])
            qs = slice(qb * 512, (qb + 1) * 512)
            nk = nkb * 128
            for h in range(8):
                li = cnt["ld"] % 2
                cnt["ld"] += 1
                for mi in range(2):
                    S.op("sp", lambda e, li=li, mi=mi, h=h, qs=qs: e.dma_start(out=qb_t[li][:, mi, :], in_=qdT[2 * h + mi][:, qs]),
                         reads=[T_qdT], writes=[T_q[li]] if mi == 0 else (), pwrites=() if mi == 0 else [T_q[li]], dsem=d_q[li])
                    S.op("sp", lambda e, li=li, mi=mi, h=h, nk=nk: e.dma_start(out=kb_t[li][:, mi, 0:nk], in_=kdT[2 * h + mi][:, 0:nk]),
                         reads=[T_kdT], writes=[T_k[li]] if mi == 0 else (), pwrites=() if mi == 0 else [T_k[li]], dsem=d_k[li])
                S.op("sp", lambda e, li=li, h=h, nk=nk, nkb=nkb: e.dma_start(
                    out=vb_t[li][:, 0:nkb, :], in_=vd[0:nk, h * 256:(h + 1) * 256].rearrange("(t p) c -> p t c", p=128)),
                    reads=[T_vd], writes=[T_v[li]], dsem=d_v[li])
                for mi in range(2):
                    banks = [2, 3, 4] if mi == 0 else [5, 6, 7]

                    def s_mms(ps, T_ps, kb, li=li, mi=mi):
                        S.op("pe", lambda e: e.matmul(ps[:, :], lhsT=kb_t[li][:, mi, kb * 128:(kb + 1) * 128], rhs=qb_t[li][:, mi, :],
                                                      start=True, stop=True), reads=[T_k[li], T_q[li]], writes=[T_ps])

                    def pv(kb, li=li):
                        return [vb_t[li][:, kb, 0:128], vb_t[li][:, kb, 128:256], ones_b[:]]
                    run_head(qb, nkb, s_mms, 2.0 ** -(h + 1), pv, banks, [T_v[li], T_ones])
                    S.op("dve", lambda e, banks=banks: e.reciprocal(out=rcp[:], in_=PB[banks[2]][:, :]), reads=[T_PB[banks[2]]], writes=[T_rcp])
                    for c in range(2):
                        S.op("dve", lambda e, banks=banks, c=c, mi=mi: e.tensor_tensor(out=res[:, mi, c, :], in0=PB[banks[c]][:, :], in1=rcp[:], op=ALU.mult),
                             reads=[T_PB[banks[c]], T_rcp], writes=[T_res[mi][c]])
                for c in range(2):
                    S.op("dve", lambda e, c=c: e.scalar_tensor_tensor(out=av[:, c, :], in0=res[:, 1, c, :], scalar=lam2[:, 3:4], in1=res[:, 0, c, :],
                                                                      op0=ALU.mult, op1=ALU.add),
                         reads=[T_res[0][c], T_res[1][c], T_lam], writes=[T_av[c]])
                    S.op("act", lambda e, c=c: e.activation(out=asq[:, c, :], in_=av[:, c, :], func=AF.Square), reads=[T_av[c]], writes=[T_asq[c]])
                for c in range(2):
                    S.op("pe", lambda e, c=c: e.matmul(PB[0][:, :], lhsT=ones_b[:], rhs=asq[:, c, :], start=(c == 0), stop=(c == 1)),
                         reads=[T_asq[c], T_ones], writes=[T_PB[0]])
                cnt["s"] += 1 if cnt["s"] % 2 == 0 else 0
                S.op("dve", lambda e: e.tensor_scalar(out=rsd[:], in0=PB[0][:, :], scalar1=1.0 / 256.0, scalar2=EPS, op0=ALU.mult, op1=ALU.add),
                     reads=[T_PB[0]], writes=[T_rsd])
                S.op("act", lambda e: e.activation(out=rsd[:], in_=rsd[:], func=AF.Sqrt), reads=[T_rsd], writes=[T_rsd])
                S.op("dve", lambda e: e.reciprocal(out=rsd[:], in_=rsd[:]), reads=[T_rsd], writes=[T_rsd])
                for c in range(2):
                    oi = cnt["o"] % 4
                    cnt["o"] += 1
                    S.op("dve", lambda e, c=c, oi=oi: e.scalar_tensor_tensor(out=ost[oi][:], in0=av[:, c, :], scalar=gsub[:, c:c + 1], in1=rsd[:],
                                                                             op0=ALU.mult, op1=ALU.mult),
                         reads=[T_av[c], T_gsub, T_rsd], writes=[T_ost[oi]])
                    S.op("sp", lambda e, c=c, oi=oi, h=h, qs=qs: e.dma_start(out=oT[2 * h + c][:, qs], in_=ost[oi][:]),
                         reads=[T_ost[oi]], pwrites=[T_oT], dsem=d_ost[oi])
            for h in range(16):
                li = cnt["ld"] % 2
                cnt["ld"] += 1
                S.op("sp", lambda e, li=li, h=h, qs=qs: e.dma_start(out=qb_t[li][:, 0, :], in_=qnT[h][:, qs]),
                     reads=[T_qnT], writes=[T_q[li]], dsem=d_q[li])
                S.op("sp", lambda e, li=li, h=h, qs=qs: e.dma_start(out=qr_t[li][0:64, :], in_=qrT[h][:, qs]),
                     reads=[T_qrT], pwrites=[T_qr[li]], dsem=d_qr[li])
                S.op("sp", lambda e, li=li, h=h, nk=nk: e.dma_start(out=kb_t[li][:, 0, 0:nk], in_=knT[h][:, 0:nk]),
                     reads=[T_knT], writes=[T_k[li]], dsem=d_k[li])
                S.op("sp", lambda e, li=li, h=h, nk=nk, nkb=nkb: e.dma_start(
                    out=vb_t[li][:, 0:nkb, 0:128], in_=vm[0:nk, h * 128:(h + 1) * 128].rearrange("(t p) c -> p t c", p=128)),
                    reads=[T_vm], writes=[T_v[li]], dsem=d_v[li])
                banks = [2, 4] if h % 2 == 0 else [5, 7]

                def s_mms(ps, T_ps, kb, li=li):
                    S.op("pe", lambda e: e.matmul(ps[:, :], lhsT=kb_t[li][:, 0, kb * 128:(kb + 1) * 128], rhs=qb_t[li][:, 0, :],
                                                  start=True, stop=False), reads=[T_k[li], T_q[li]], writes=[T_ps])
                    S.op("pe", lambda e: e.matmul(ps[:, :], lhsT=kr_t[:, kb * 128:(kb + 1) * 128], rhs=qr_t[li][:, :],
                                                  start=False, stop=True), reads=[T_kr, T_qr[li]], writes=[T_ps])

                def pv(kb, li=li):
                    return [vb_t[li][:, kb, 0:128], ones_b[:]]
                run_head(qb, nkb, s_mms, None, pv, banks, [T_v[li], T_ones])
                oi = cnt["o"] % 4
                cnt["o"] += 1
                S.op("dve", lambda e, banks=banks: e.reciprocal(out=rcp[:], in_=PB[banks[1]][:, :]), reads=[T_PB[banks[1]]], writes=[T_rcp])
                S.op("dve", lambda e, banks=banks, oi=oi: e.tensor_tensor(out=ost[oi][:], in0=PB[banks[0]][:, :], in1=rcp[:], op=ALU.mult),
                     reads=[T_PB[banks[0]], T_rcp], writes=[T_ost[oi]])
                S.op("sp", lambda e, oi=oi, h=h, qs=qs: e.dma_start(out=oT[16 + h][:, qs], in_=ost[oi][:]),
                     reads=[T_ost[oi]], pwrites=[T_oT], dsem=d_ost[oi])
        S.barrier()
        p4.close()

    phase4()
    p34.close()
    if stage <= 4:
        S.emit()
        es.close()
        return nc
    x1d = scratch("x1d", [SEQ, D], F32)
    hfT = scratch("hfT", [NDC, 128, SEQ], BF16)
    T_x1d = Tl(); T_hfT = Tl()
    wts = sb("wts", [128, NT, NE], F32)
    T_wts = [Tl() for _ in range(NT)]

    def phase5():
        p5 = ExitStack()
        GT = 2
        GW = GT * 128
        oTg = [sb("e_o0", [128, NDC, GW], BF16, p5)] * 2
        wo = [sb(f"e_w{i}", [128, NDC, 512], BF16, p5) for i in range(2)]
        gt = [sb(f"e_g{i}", [128, GT, 2, 512], BF16, p5) for i in range(2)]
        xin = [sb(f"e_x{i}", [128, GT, 512], F32, p5) for i in range(2)]
        x1g = sb("e_x1g", [128, GT, D], F32, p5)
        xsb = sb("e_xs", [128, GT, D], BF16, p5)
        hfs = sb("e_hf", [128, NDC, GW], BF16, p5)
        gta = sb("e_gta", [128, D], F32, p5)
        t0 = [sb(f"e_t0{i}", [128, 512], F32, p5) for i in range(2)]
        t1 = [sb(f"e_t1{i}", [128, 512], F32, p5) for i in range(2)]
        st5 = sb("e_st", [128, NT], F32, p5)
        rw = sb("e_rw", [128, NDC, NE], BF16, p5)
        rbias = sb("e_rb", [128, NE], F32, p5)
        T_oTg = [Tl()] * 2; T_wo = [Tl(), Tl()]; T_gt = [Tl(), Tl()]; T_xin = [Tl(), Tl()]
        T_x1g = [Tl() for _ in range(GT)]; T_xsb = [Tl() for _ in range(GT)]; T_hfs = Tl(); T_gta = Tl()
        T_t0 = [Tl(), Tl()]; T_t1 = [Tl(), Tl()]; T_st5 = [Tl() for _ in range(NT)]; T_rw = Tl(); T_rb = Tl()
        d_oTg = [S.dsem("eo0")] * 2; d_wo = [S.dsem("ew0"), S.dsem("ew1")]
        d_gt = [S.dsem("eg0"), S.dsem("eg1")]; d_xin = [S.dsem("ex0"), S.dsem("ex1")]
        d_x1g = S.dsem("ex1g"); d_hfs = S.dsem("ehfs")
        S.op("sp", lambda e: e.dma_start(out=gta[:], in_=modv[2].partition_broadcast(128)), reads=[T_modv], writes=[T_gta], dsem=T_gta)
        S.op("pool", lambda e: e.dma_start(out=rw[:], in_=W["router_w"].rearrange("(j p) e -> p j e", p=128)), writes=[T_rw], dsem=T_rw)
        S.op("sp", lambda e: e.dma_start(out=rbias[:], in_=W["router_bias"].partition_broadcast(128)), writes=[T_rb], dsem=T_rb)
        k_sc = sb("k_sc", [128, NE], F32, p5); k_sel = sb("k_sel", [128, NE], F32, p5); k_cur = sb("k_cur", [128, NE], F32, p5)
        k_eq = sb("k_eq", [128, NE], F32, p5); k_m1 = sb("k_m1", [128, 8], F32, p5); k_m2 = sb("k_m2", [128, 8], F32, p5)
        k_gs = sb("k_gs", [128, 8], F32, p5); k_gc = sb("k_gc", [128, 8], F32, p5); k_ge = sb("k_ge", [128, 8], F32, p5)
        k_mx = sb("k_mx", [128, 1], F32, p5)
        T_k = Tl()
        BIGK = 1.0e4
        nw = 0
        tpi = 0
        for g in range(NT // GT):
            gi = g % 2
            gsl = slice(g * GW, (g + 1) * GW)
            S.op("sp", lambda e, gi=gi, gsl=gsl: e.dma_start(out=oTg[gi][:], in_=oT[:, :, gsl].rearrange("c p t -> p c t")),
                 reads=[T_oT], writes=[T_oTg[gi]], dsem=d_oTg[gi])
            for dmb in range(8):
                wi = nw % 2
                nw += 1
                dsl = slice(dmb * 512, (dmb + 1) * 512)
                S.op("pool", lambda e, wi=wi, dsl=dsl: e.dma_start(out=wo[wi][:], in_=W["w_out"][:, dsl].rearrange("(j p) c -> p j c", p=128)),
                     writes=[T_wo[wi]], dsem=d_wo[wi])
                for br in range(2):
                    S.op("sp", lambda e, wi=wi, dmb=dmb, gsl=gsl, br=br: e.dma_start(
                        out=gt[wi][:, :, br, :], in_=gates[gsl, br * D + dmb * 512:br * D + (dmb + 1) * 512].rearrange("(t p) c -> p t c", p=128)),
                        reads=[T_gates], writes=[T_gt[wi]] if br == 0 else (), pwrites=() if br == 0 else [T_gt[wi]], dsem=d_gt[wi])
                S.op("sp", lambda e, wi=wi, dsl=dsl, gsl=gsl: e.dma_start(out=xin[wi][:], in_=x_in[gsl, dsl].rearrange("(t p) c -> p t c", p=128)),
                     writes=[T_xin[wi]], dsem=d_xin[wi])
                for tt in range(GT):
                    ti = tpi % 2
                    tpi += 1
                    bd, bm = (0, 1) if ti == 0 else (2, 3)
                    for fc in range(16):
                        S.op("pe", lambda e, bd=bd, gi=gi, wi=wi, fc=fc, tt=tt: e.matmul(
                            PB[bd][:, :], lhsT=oTg[gi][:, fc, tt * 128:(tt + 1) * 128], rhs=wo[wi][:, fc, :], start=(fc == 0), stop=(fc == 15)),
                            reads=[T_oTg[gi], T_wo[wi]], writes=[T_PB[bd]])
                    for fc in range(16, 32):
                        S.op("pe", lambda e, bm=bm, gi=gi, wi=wi, fc=fc, tt=tt: e.matmul(
                            PB[bm][:, :], lhsT=oTg[gi][:, fc, tt * 128:(tt + 1) * 128], rhs=wo[wi][:, fc, :], start=(fc == 16), stop=(fc == 31)),
                            reads=[T_oTg[gi], T_wo[wi]], writes=[T_PB[bm]])
                    S.op("dve", lambda e, bd=bd, wi=wi, tt=tt, ti=ti: e.tensor_tensor(out=t0[ti][:], in0=PB[bd][:, :], in1=gt[wi][:, tt, 0, :], op=ALU.mult),
                         reads=[T_PB[bd], T_gt[wi]], writes=[T_t0[ti]])
                    S.op("dve", lambda e, bm=bm, wi=wi, tt=tt, ti=ti: e.tensor_tensor(out=t1[ti][:], in0=PB[bm][:, :], in1=gt[wi][:, tt, 1, :], op=ALU.mult),
                         reads=[T_PB[bm], T_gt[wi]], writes=[T_t1[ti]])
                    S.op("pool", lambda e, ti=ti: e.tensor_tensor(out=t0[ti][:], in0=t0[ti][:], in1=t1[ti][:], op=ALU.add),
                         reads=[T_t0[ti], T_t1[ti]], writes=[T_t0[ti]])
                    S.op("pool", lambda e, ti=ti, dsl=dsl: e.tensor_tensor(out=t0[ti][:], in0=t0[ti][:], in1=gta[:, dsl], op=ALU.mult),
                         reads=[T_t0[ti], T_gta], writes=[T_t0[ti]])
                    S.op("pool", lambda e, ti=ti, wi=wi, tt=tt, dsl=dsl: e.tensor_tensor(out=x1g[:, tt, dsl], in0=t0[ti][:], in1=xin[wi][:, tt, :], op=ALU.add),
                         reads=[T_t0[ti], T_xin[wi]], writes=[T_x1g[tt]] if dmb == 0 else (), pwrites=() if dmb == 0 else [T_x1g[tt]])
            S.op("sp", lambda e, gsl=gsl: e.dma_start(out=x1d[gsl, :].rearrange("(t p) c -> p t c", p=128), in_=x1g[:]),
                 reads=T_x1g, pwrites=[T_x1d], dsem=d_x1g)
            for tt in range(GT):
                t = g * GT + tt
                S.op("act", lambda e, tt=tt, t=t: e.activation(out=xsb[:, tt, :], in_=x1g[:, tt, :], func=AF.Square, accum_out=st5[:, t:t + 1]),
                     reads=[T_x1g[tt]], writes=[T_xsb[tt], T_st5[t]])
                S.op("dve", lambda e, t=t: e.tensor_scalar(out=st5[:, t:t + 1], in0=st5[:, t:t + 1], scalar1=1.0 / D, scalar2=EPS, op0=ALU.mult, op1=ALU.add),
                     reads=[T_st5[t]], writes=[T_st5[t]])
                S.op("act", lambda e, t=t: e.activation(out=st5[:, t:t + 1], in_=st5[:, t:t + 1], func=AF.Sqrt), reads=[T_st5[t]], writes=[T_st5[t]])
                S.op("dve", lambda e, t=t: e.reciprocal(out=st5[:, t:t + 1], in_=st5[:, t:t + 1]), reads=[T_st5[t]], writes=[T_st5[t]])
                S.op("act", lambda e, tt=tt, t=t: e.activation(out=xsb[:, tt, :], in_=x1g[:, tt, :], func=AF.Copy, scale=st5[:, t:t + 1]),
                     reads=[T_x1g[tt], T_st5[t]], writes=[T_xsb[tt]])
            for dc in range(NDC):
                kk = 4 + (dc % 2)
                tpv = PB[kk][:].bitcast(BF16)
                for tt in range(GT):
                    S.op("pe", lambda e, tpv=tpv, tt=tt, dc=dc: e.transpose(tpv[:, tt * 128:(tt + 1) * 128], xsb[:, tt, dc * 128:(dc + 1) * 128], ident_b[:]),
                         reads=[T_xsb[tt], T_ident], writes=[T_PB[kk]] if tt == 0 else (), pwrites=() if tt == 0 else [T_PB[kk]])
                if dc % 2 == 0:
                    S.op("dve", lambda e, tpv=tpv, dc=dc: e.tensor_scalar(out=hfs[:, dc, :], in0=tpv[:, 0:GW], scalar1=modT[:, 224 + dc:225 + dc],
                                                                          scalar2=modT[:, 96 + dc:97 + dc], op0=ALU.mult, op1=ALU.add),
                         reads=[T_PB[kk], T_modT], writes=[T_hfs] if dc == 0 else (), pwrites=() if dc == 0 else [T_hfs])
                else:
                    S.op("act", lambda e, tpv=tpv, dc=dc: e.activation(out=hfs[:, dc, :], in_=tpv[:, 0:GW], func=AF.Identity,
                                                                       scale=modT[:, 224 + dc:225 + dc], bias=modT[:, 96 + dc:97 + dc]),
                         reads=[T_PB[kk], T_modT], pwrites=[T_hfs])
            S.op("sp", lambda e, gsl=gsl: e.dma_start(out=hfT[:, :, gsl].rearrange("c p t -> p c t"), in_=hfs[:]),
                 reads=[T_hfs], pwrites=[T_hfT], dsem=d_hfs)
            for tt in range(GT):
                t = g * GT + tt
                for dc in range(NDC):
                    S.op("pe", lambda e, dc=dc, tt=tt: e.matmul(PB[6][:, 0:NE], lhsT=hfs[:, dc, tt * 128:(tt + 1) * 128], rhs=rw[:, dc, :],
                                                                start=(dc == 0), stop=(dc == NDC - 1)), reads=[T_hfs, T_rw], writes=[T_PB[6]])
                S.op("act", lambda e: e.activation(out=k_sc[:], in_=PB[6][:, 0:NE], func=AF.Sigmoid), reads=[T_PB[6]], writes=[T_k])
                K = dict(reads=[T_k], writes=[T_k])
                v3 = lambda a: a[:].rearrange("p (g c) -> p g c", c=8)
                b3 = lambda a: a[:].unsqueeze(2).to_broadcast([128, 8, 8])
                S.op("dve", lambda e: e.tensor_tensor(out=k_sel[:], in0=k_sc[:], in1=rbias[:], op=ALU.add), reads=[T_k, T_rb], writes=[T_k])
                S.op("dve", lambda e: e.reduce_max(out=k_m1[:], in_=v3(k_sel), axis=AX.X), **K)
                S.op("dve", lambda e: e.tensor_tensor(out=v3(k_eq), in0=v3(k_sel), in1=b3(k_m1), op=ALU.is_equal), **K)
                S.op("dve", lambda e: e.scalar_tensor_tensor(out=k_cur[:], in0=k_eq[:], scalar=-BIGK, in1=k_sel[:], op0=ALU.mult, op1=ALU.add), **K)
                S.op("dve", lambda e: e.reduce_max(out=k_m2[:], in_=v3(k_cur), axis=AX.X), **K)
                S.op("dve", lambda e: e.tensor_tensor(out=k_gs[:], in0=k_m1[:], in1=k_m2[:], op=ALU.add), **K)
                S.op("dve", lambda e: e.tensor_copy(out=k_gc[:], in_=k_gs[:]), **K)
                for _ in range(3):
                    S.op("dve", lambda e: e.reduce_max(out=k_mx[:], in_=k_gc[:], axis=AX.X), **K)
                    S.op("dve", lambda e: e.tensor_scalar(out=k_ge[:], in0=k_gc[:], scalar1=k_mx[:, 0:1], scalar2=None, op0=ALU.is_equal), **K)
                    S.op("dve", lambda e: e.scalar_tensor_tensor(out=k_gc[:], in0=k_ge[:], scalar=-BIGK, in1=k_gc[:], op0=ALU.mult, op1=ALU.add), **K)
                S.op("dve", lambda e: e.reduce_max(out=k_mx[:], in_=k_gc[:], axis=AX.X), **K)
                S.op("dve", lambda e: e.tensor_scalar(out=k_ge[:], in0=k_gs[:], scalar1=k_mx[:, 0:1], scalar2=None, op0=ALU.is_ge), **K)
                S.op("dve", lambda e: e.tensor_scalar(out=k_ge[:], in0=k_ge[:], scalar1=-1.0, scalar2=BIGK, op0=ALU.add, op1=ALU.mult), **K)
                S.op("dve", lambda e: e.tensor_tensor(out=v3(k_sel), in0=v3(k_sel), in1=b3(k_ge), op=ALU.add), **K)
                S.op("dve", lambda e: e.tensor_copy(out=k_cur[:], in_=k_sel[:]), **K)
                for _ in range(5):
                    S.op("dve", lambda e: e.reduce_max(out=k_mx[:], in_=k_cur[:], axis=AX.X), **K)
                    S.op("dve", lambda e: e.tensor_scalar(out=k_eq[:], in0=k_cur[:], scalar1=k_mx[:, 0:1], scalar2=None, op0=ALU.is_equal), **K)
                    S.op("dve", lambda e: e.scalar_tensor_tensor(out=k_cur[:], in0=k_eq[:], scalar=-BIGK, in1=k_cur[:], op0=ALU.mult, op1=ALU.add), **K)
                S.op("dve", lambda e: e.reduce_max(out=k_mx[:], in_=k_cur[:], axis=AX.X), **K)
                S.op("dve", lambda e: e.tensor_scalar(out=k_eq[:], in0=k_sel[:], scalar1=k_mx[:, 0:1], scalar2=None, op0=ALU.is_ge), **K)
                S.op("dve", lambda e: e.tensor_tensor(out=k_sc[:], in0=k_sc[:], in1=k_eq[:], op=ALU.mult), **K)
                S.op("dve", lambda e: e.reduce_sum(out=k_mx[:], in_=k_sc[:], axis=AX.X), **K)
                S.op("dve", lambda e: e.reciprocal(out=k_mx[:], in_=k_mx[:]), **K)
                S.op("dve", lambda e, t=t: e.tensor_scalar(out=wts[:, t, :], in0=k_sc[:], scalar1=k_mx[:, 0:1], scalar2=2.5, op0=ALU.mult, op1=ALU.mult),
                     reads=[T_k], writes=[T_wts[t]])
        S.barrier()
        p5.close()

    phase5()
    if stage <= 5:
        if "wts_dbg" in debug_out:
            wd = scratch("wts_dbg", [128, NT, NE], F32)
            S.op("sp", lambda e: e.dma_start(out=wd, in_=wts[:]), reads=T_wts, dsem=Tl())
            S.barrier()
        S.emit()
        es.close()
        return nc
    accd = scratch("accd", [SEQ, D], F32)
    T_acc = [[Tl(), Tl()] for _ in range(NT)]

    def phase6():
        p6 = ExitStack()
        hfb = [sb(f"m_h{i}", [128, NDC, 512], BF16, p6) for i in range(2)]
        w1 = sb("m_w1", [128, NDC, 512], BF16, p6)
        w3 = sb("m_w3", [128, NDC, 512], BF16, p6)
        w2 = sb("m_w2", [128, 4, D], BF16, p6)
        aT = [sb(f"m_a{i}", [128, 4, 512], BF16, p6) for i in range(2)]
        sil = [sb(f"m_s{i}", [128, 512], F32, p6) for i in range(2)]
        yst = [sb(f"m_y{i}", [128, 2048], F32, p6) for i in range(2)]
        T_hfb = [Tl(), Tl()]; T_w1 = Tl(); T_w3 = Tl(); T_w2 = Tl(); T_aT = [[Tl() for _ in range(4)] for _ in range(2)]
        T_sil = [Tl(), Tl()]; T_yst = [Tl(), Tl()]
        d_hfb = [S.dsem("mh0"), S.dsem("mh1")]; d_w1 = S.dsem("mw1"); d_w3 = S.dsem("mw3"); d_w2 = S.dsem("mw2")
        d_yst = [S.dsem("my0"), S.dsem("my1")]
        NEX = NE + 1
        c = {"hb": 0, "s1": 0, "sl": 0, "s2": 0, "y": 0, "ev": 0}
        for ex in range(NEX):
            if ex < NE:
                s1, s3, s2 = W["exp_w1"][ex], W["exp_w3"][ex], W["exp_w2"][ex]
            else:
                s1, s3, s2 = W["shared_w1"], W["shared_w3"], W["shared_w2"]
            S.op("pool", lambda e, s1=s1: e.dma_start(out=w1[:], in_=s1.rearrange("(j p) f -> p j f", p=128)), writes=[T_w1], dsem=d_w1)
            S.op("pool", lambda e, s3=s3: e.dma_start(out=w3[:], in_=s3.rearrange("(j p) f -> p j f", p=128)), writes=[T_w3], dsem=d_w3)
            for j in range(4):
                S.op("pool", lambda e, s2=s2, j=j: e.dma_start(out=w2[:, j, :], in_=s2[j * 128:(j + 1) * 128, :], max_dma_last_dim=8192),
                     writes=[T_w2] if j == 0 else (), pwrites=() if j == 0 else [T_w2], dsem=d_w2)
            for tb in range(4):
                hi = c["hb"] % 2
                c["hb"] += 1
                S.op("sp", lambda e, hi=hi, tb=tb: e.dma_start(out=hfb[hi][:], in_=hfT[:, :, tb * 512:(tb + 1) * 512].rearrange("c p t -> p c t")),
                     reads=[T_hfT], writes=[T_hfb[hi]], dsem=d_hfb[hi])
                ai = (ex * 4 + tb) % 2
                for fc in range(4):
                    k1 = c["s1"] % 2
                    c["s1"] += 1
                    b1, b3 = (0, 1) if k1 == 0 else (2, 3)
                    for (bk, wt_, T_w) in ((b1, w1, T_w1), (b3, w3, T_w3)):
                        for dc in range(NDC):
                            S.op("pe", lambda e, bk=bk, wt_=wt_, dc=dc, fc=fc, hi=hi: e.matmul(
                                PB[bk][:, :], lhsT=wt_[:, dc, fc * 128:(fc + 1) * 128], rhs=hfb[hi][:, dc, :], start=(dc == 0), stop=(dc == NDC - 1)),
                                reads=[T_w, T_hfb[hi]], writes=[T_PB[bk]])
                    si = c["sl"] % 2
                    c["sl"] += 1
                    S.op("act", lambda e, si=si, b1=b1: e.activation(out=sil[si][:], in_=PB[b1][:, :], func=AF.Silu), reads=[T_PB[b1]], writes=[T_sil[si]])
                    S.op("dve", lambda e, si=si, b3=b3, ai=ai, fc=fc: e.tensor_tensor(out=aT[ai][:, fc, :], in0=sil[si][:], in1=PB[b3][:, :], op=ALU.mult),
                         reads=[T_sil[si], T_PB[b3]], writes=[T_aT[ai][fc]])
                for tt in range(4):
                    t = tb * 4 + tt
                    for half in range(2):
                        yi = c["y"] % 2
                        c["y"] += 1
                        for q4 in range(4):
                            dmb = half * 4 + q4
                            bk = 4 + (c["s2"] % 4)
                            c["s2"] += 1
                            for fc in range(4):
                                S.op("pe", lambda e, bk=bk, ai=ai, fc=fc, tt=tt, dmb=dmb: e.matmul(
                                    PB[bk][:, :], lhsT=aT[ai][:, fc, tt * 128:(tt + 1) * 128], rhs=w2[:, fc, dmb * 512:(dmb + 1) * 512],
                                    start=(fc == 0), stop=(fc == 3)), reads=[T_aT[ai][fc], T_w2], writes=[T_PB[bk]])
                            wr = [T_yst[yi]] if q4 == 0 else []
                            pw = [] if q4 == 0 else [T_yst[yi]]
                            use_act = (c["ev"] % 2 == 0)
                            c["ev"] += 1
                            if ex < NE:
                                if use_act:
                                    S.op("act", lambda e, bk=bk, yi=yi, q4=q4, t=t, ex=ex: e.activation(
                                        out=yst[yi][:, q4 * 512:(q4 + 1) * 512], in_=PB[bk][:, :], func=AF.Copy, scale=wts[:, t, ex:ex + 1]),
                                        reads=[T_PB[bk], T_wts[t]], writes=wr, pwrites=pw)
                                else:
                                    S.op("dve", lambda e, bk=bk, yi=yi, q4=q4, t=t, ex=ex: e.tensor_scalar(
                                        out=yst[yi][:, q4 * 512:(q4 + 1) * 512], in0=PB[bk][:, :], scalar1=wts[:, t, ex:ex + 1], scalar2=None, op0=ALU.mult),
                                        reads=[T_PB[bk], T_wts[t]], writes=wr, pwrites=pw)
                            else:
                                if use_act:
                                    S.op("act", lambda e, bk=bk, yi=yi, q4=q4: e.copy(out=yst[yi][:, q4 * 512:(q4 + 1) * 512], in_=PB[bk][:, :]),
                                         reads=[T_PB[bk]], writes=wr, pwrites=pw)
                                else:
                                    S.op("dve", lambda e, bk=bk, yi=yi, q4=q4: e.tensor_copy(out=yst[yi][:, q4 * 512:(q4 + 1) * 512], in_=PB[bk][:, :]),
                                         reads=[T_PB[bk]], writes=wr, pwrites=pw)
                        dst = accd[t * 128:(t + 1) * 128, half * 2048:(half + 1) * 2048]
                        if ex == 0:
                            S.op("pool", lambda e, dst=dst, yi=yi: e.dma_start(out=dst, in_=yst[yi][:]),
                                 reads=[T_yst[yi]], writes=[T_acc[t][half]], dsem=d_yst[yi])
                        else:
                            S.op("pool", lambda e, dst=dst, yi=yi: e.dma_start(out=dst, in_=yst[yi][:], accum_op=ALU.add),
                                 reads=[T_yst[yi]], writes=[T_acc[t][half]], dsem=d_yst[yi])
        S.barrier()
        p6.close()

    phase6()
    if stage <= 6:
        S.emit()
        es.close()
        return nc

    def phase7():
        p7 = ExitStack()
        gtf = sb("f_gtf", [128, D], F32, p7)
        fnb = sb("f_fn", [128, D], F32, p7)
        xa = [sb(f"f_x{i}", [128, D], F32, p7) for i in range(2)]
        ac = [sb(f"f_a{i}", [128, D], F32, p7) for i in range(2)]
        jk = sb("f_jk", [128, D], BF16, p7)
        st7 = sb("f_st", [128, NT], F32, p7)
        T_gtf = Tl(); T_fnb = Tl(); T_xa = [Tl(), Tl()]; T_ac = [Tl(), Tl()]; T_jk = Tl(); T_st7 = [Tl() for _ in range(NT)]
        d_xa = [S.dsem("fx0"), S.dsem("fx1")]; d_ac = [S.dsem("fa0"), S.dsem("fa1")]
        T_out = Tl()
        S.op("sp", lambda e: e.dma_start(out=gtf[:], in_=modv[5].partition_broadcast(128)), reads=[T_modv], writes=[T_gtf], dsem=T_gtf)
        S.op("sp", lambda e: e.dma_start(out=fnb[:], in_=W["final_norm"].partition_broadcast(128)), writes=[T_fnb], dsem=T_fnb)
        for t in range(NT):
            i = t % 2
            rows = slice(t * 128, (t + 1) * 128)
            S.op("sp", lambda e, i=i, rows=rows: e.dma_start(out=xa[i][:], in_=x1d[rows, :]), reads=[T_x1d], writes=[T_xa[i]], dsem=d_xa[i])
            S.op("sp", lambda e, i=i, rows=rows: e.dma_start(out=ac[i][:], in_=accd[rows, :]), reads=T_acc[t], writes=[T_ac[i]], dsem=d_ac[i])
            S.op("dve", lambda e, i=i: e.tensor_tensor(out=ac[i][:], in0=ac[i][:], in1=gtf[:], op=ALU.mult), reads=[T_ac[i], T_gtf], writes=[T_ac[i]])
            S.op("pool", lambda e, i=i: e.tensor_tensor(out=xa[i][:], in0=xa[i][:], in1=ac[i][:], op=ALU.add), reads=[T_xa[i], T_ac[i]], writes=[T_xa[i]])
            S.op("act", lambda e, i=i, t=t: e.activation(out=jk[:], in_=xa[i][:], func=AF.Square, accum_out=st7[:, t:t + 1]),
                 reads=[T_xa[i]], writes=[T_jk, T_st7[t]])
            S.op("dve", lambda e, t=t: e.tensor_scalar(out=st7[:, t:t + 1], in0=st7[:, t:t + 1], scalar1=1.0 / D, scalar2=EPS, op0=ALU.mult, op1=ALU.add),
                 reads=[T_st7[t]], writes=[T_st7[t]])
            S.op("act", lambda e, t=t: e.activation(out=st7[:, t:t + 1], in_=st7[:, t:t + 1], func=AF.Sqrt), reads=[T_st7[t]], writes=[T_st7[t]])
            S.op("dve", lambda e, t=t: e.reciprocal(out=st7[:, t:t + 1], in_=st7[:, t:t + 1]), reads=[T_st7[t]], writes=[T_st7[t]])
            S.op("act", lambda e, i=i, t=t: e.activation(out=ac[i][:], in_=xa[i][:], func=AF.Copy, scale=st7[:, t:t + 1]),
                 reads=[T_xa[i], T_st7[t]], writes=[T_ac[i]])
            S.op("dve", lambda e, i=i: e.tensor_tensor(out=ac[i][:], in0=ac[i][:], in1=fnb[:], op=ALU.mult), reads=[T_ac[i], T_fnb], writes=[T_ac[i]])
            S.op("sp", lambda e, i=i, rows=rows: e.dma_start(out=out_d[rows, :], in_=ac[i][:]), reads=[T_ac[i]], pwrites=[T_out], dsem=d_ac[i])
        S.barrier()
        p7.close()

    phase7()
    S.emit()
    es.close()
    return nc


RMAT = np.zeros((128, 64), np.float32)
for _m in range(32):
    RMAT[_m + 32, _m] = -1.0
    RMAT[_m, _m + 32] = 1.0

IMPLEMENTED_STAGE = 99
STAGE_WEIGHTS = ["w_ada", "b_ada", "norm_attn", "norm_ffn", "w_in"]


def make_in_maps(inputs, names=None):
    names = WEIGHT_NAMES if names is None else names
    maps = []
    ws = {n: np.ascontiguousarray(np.asarray(inputs[n])[0] if n != "final_norm" else np.asarray(inputs[n]), dtype=np.float32)
          for n in names}
    x = np.asarray(inputs["x"], dtype=np.float32)
    c = np.asarray(inputs["c"], dtype=np.float32)
    pos = np.asarray(inputs["positions"], dtype=np.int32)
    ident = np.eye(128, dtype=np.float32)
    for b in range(8):
        m = dict(ws)
        m["x"] = np.ascontiguousarray(x[b])
        m["c"] = np.ascontiguousarray(c[b])
        m["pos"] = np.ascontiguousarray(pos[b])
        m["ident"] = ident
        m["rmat"] = RMAT
        maps.append(m)
    return maps


def kernel(**inputs):
    nc = build(stage=IMPLEMENTED_STAGE)
    names = stage_weights(IMPLEMENTED_STAGE)
    res = run_bass_kernel_spmd(nc, make_in_maps(inputs, names), core_ids=list(range(8)))
    return np.stack([np.asarray(r["out"]).reshape(SEQ, D) for r in res.results], axis=0).astype(np.float32)
```

```python
import numpy as np
import ml_dtypes
from contextlib import ExitStack
import concourse.bass as bass
import concourse.mybir as mybir
from concourse.bass_utils import run_bass_kernel_spmd

F32 = mybir.dt.float32
BF16 = mybir.dt.bfloat16
I32 = mybir.dt.int32
AF = mybir.ActivationFunctionType
ALU = mybir.AluOpType
AX = mybir.AxisListType

ENG_NAMES = ("pe", "act", "dve", "pool", "sp")
EPOCH_MAX = 30000


class Tl:
    __slots__ = ("name", "w", "r", "fw", "ds")

    def __init__(self, name=""):
        self.name = name
        self.w = {}
        self.r = {}
        self.fw = {}
        self.ds = None


class DSem:
    __slots__ = ("name", "count", "sems", "nops")

    def __init__(self, name):
        self.name = name
        self.nops = 0


class Op:
    __slots__ = ("eng", "fn", "waits", "idx", "needed", "dsem", "didx", "ticket")


class Sched:
    def __init__(self, nc, same_eng_wait=True):
        self.nc = nc
        self.ops = {e: [] for e in ENG_NAMES}
        self.waited = {e: {} for e in ENG_NAMES}
        self.same_eng_wait = same_eng_wait
        self.dsems = []
        self.last = {}
        self.free_ds = []
        self.tile_ds = []

    def dsem(self, name):
        d = DSem(name)
        self.dsems.append(d)
        return d

    def op(self, eng, fn, reads=(), writes=(), pwrites=(), dsem=None):
        if isinstance(dsem, Tl):
            t = dsem
            if t.ds is None:
                t.ds = self.free_ds.pop() if self.free_ds else self.dsem(f"t{len(self.dsems)}")
                self.tile_ds.append(t)
            dsem = t.ds
        o = Op()
        o.eng = eng
        o.fn = fn
        o.needed = False
        o.dsem = dsem
        o.idx = len(self.ops[eng])
        deps = {}

        def add(d):
            for k, ent in d.items():
                cur = deps.get(k)
                if cur is None or cur[0] < ent[0]:
                    deps[k] = ent

        for t in reads:
            add(t.w)
        for t in writes:
            add(t.w)
            add(t.r)
        for t in pwrites:
            add(t.r)
            add(t.fw)
        waits = []
        wd = self.waited[eng]
        for k, (order, dop) in deps.items():
            if k[0] == "e" and k[1] == eng:
                if eng == "pe" or not self.same_eng_wait:
                    continue
            if wd.get(k, -1) >= order:
                continue
            wd[k] = order
            dop.needed = True
            waits.append(dop)
        o.waits = waits
        if dsem is not None:
            o.didx = dsem.nops
            dsem.nops += 1
            key = ("d", id(dsem))
            order = o.didx
        else:
            key = ("e", eng)
            order = o.idx
        ent = (order, o)
        self.last[key] = ent
        for t in writes:
            t.w = {key: ent}
            t.fw = {key: ent}
            t.r = {}
        for t in pwrites:
            t.w[key] = ent
        for t in reads:
            t.r[key] = ent
        self.ops[eng].append(o)
        return o

    def barrier(self):
        t = Tl("bar")
        t.w = dict(self.last)
        for e in ENG_NAMES:
            self.op(e, None, reads=[t])
        for tt in self.tile_ds:
            self.free_ds.append(tt.ds)
            tt.ds = None
        self.tile_ds = []

    def emit(self):
        nc = self.nc
        with ExitStack() as es:
            for e in ENG_NAMES:
                n = 0
                epoch = 0
                sems = [es.enter_context(nc.semaphore(f"c_{e}_0"))]
                for o in self.ops[e]:
                    if o.dsem is None and o.needed:
                        if o.fn is None:
                            o.fn = lambda eng: eng.nop()
                        if n >= EPOCH_MAX:
                            epoch += 1
                            n = 0
                            sems.append(es.enter_context(nc.semaphore(f"c_{e}_{epoch}")))
                        n += 1
                        o.ticket = (sems[epoch], n)
            per = {}
            for e in ENG_NAMES:
                for o in self.ops[e]:
                    if o.dsem is not None:
                        per.setdefault(id(o.dsem), []).append(o)
            for d in self.dsems:
                lst = sorted(per.get(id(d), []), key=lambda o: o.didx)
                if not lst:
                    continue
                d.sems = [es.enter_context(nc.semaphore(f"d_{d.name}_0"))]
                cnt = 0
                ep = 0
                for o in lst:
                    if cnt + 16 > EPOCH_MAX:
                        ep += 1
                        cnt = 0
                        d.sems.append(es.enter_context(nc.semaphore(f"d_{d.name}_{ep}")))
                    cnt += 16
                    o.ticket = (d.sems[ep], cnt)
            engs = {"pe": "tensor", "act": "scalar", "dve": "vector", "pool": "gpsimd", "sp": "sync"}
            with nc.Block() as block:
                def mk(e):
                    ops = self.ops[e]

                    def body(engine):
                        for o in ops:
                            for dop in o.waits:
                                s, v = dop.ticket
                                engine.wait_ge(s, v)
                            if o.fn is None:
                                continue
                            ins = o.fn(engine)
                            if o.dsem is not None:
                                ins.then_inc(o.ticket[0], 16)
                            elif o.needed:
                                ins.then_inc(o.ticket[0], 1)
                    return body
                for e in ENG_NAMES:
                    getattr(block, engs[e])(mk(e))
        return nc


D = 4096
SEQ = 2048
NT = SEQ // 128
NDC = D // 128
IN_COLS = 15680
OFF_Q, OFF_K, OFF_V, OFF_CQ, OFF_CKV, OFF_KPE, OFF_G = 0, 2048, 4096, 6144, 6912, 7424, 7488
EPS = 1e-6
LAM_INIT = 0.8 - 0.6 * 1.0
NE = 64
CAP = 1024

WEIGHT_NAMES = ["w_ada", "b_ada", "norm_attn", "w_in", "diff_lambda", "diff_subln", "mla_q_norm",
                "mla_w_uq", "mla_kv_norm", "mla_w_ukv", "w_out", "norm_ffn", "router_w", "router_bias",
                "exp_w1", "exp_w3", "exp_w2", "shared_w1", "shared_w3", "shared_w2", "final_norm"]
WEIGHT_SHAPES = {
    "w_ada": [D, 6 * D], "b_ada": [6 * D], "norm_attn": [D], "w_in": [D, IN_COLS], "diff_lambda": [4, 128],
    "diff_subln": [256], "mla_q_norm": [768], "mla_w_uq": [768, 3072], "mla_kv_norm": [512],
    "mla_w_ukv": [512, 4096], "w_out": [D, D], "norm_ffn": [D], "router_w": [D, NE], "router_bias": [NE],
    "exp_w1": [NE, D, 512], "exp_w3": [NE, D, 512], "exp_w2": [NE, 512, D], "shared_w1": [D, 512],
    "shared_w3": [D, 512], "shared_w2": [512, D], "final_norm": [D],
}


def stage_weights(stage):
    base = ["w_ada", "b_ada", "norm_attn", "norm_ffn", "w_in"]
    if stage <= 2:
        return base
    base = base + ["diff_lambda", "diff_subln", "mla_q_norm", "mla_w_uq", "mla_kv_norm", "mla_w_ukv"]
    if stage <= 4:
        return base
    base = base + ["w_out", "router_w", "router_bias"]
    if stage <= 5:
        return base
    return WEIGHT_NAMES


def build(stage=99, debug_out=()):
    nc = bass.Bass("TRN2", target_bir_lowering=False)
    S = Sched(nc)
    es = ExitStack()
    W = {}
    wnames = stage_weights(stage)
    for n in wnames:
        W[n] = nc.dram_tensor(n, WEIGHT_SHAPES[n], F32, kind="ExternalInput").ap()
    x_in = nc.dram_tensor("x", [SEQ, D], F32, kind="ExternalInput").ap()
    c_in = nc.dram_tensor("c", [D], F32, kind="ExternalInput").ap()
    pos_in = nc.dram_tensor("pos", [SEQ], I32, kind="ExternalInput").ap()
    ident_in = nc.dram_tensor("ident", [128, 128], F32, kind="ExternalInput").ap()
    rmat_in = nc.dram_tensor("rmat", [128, 64], F32, kind="ExternalInput").ap()
    out_d = nc.dram_tensor("out", [SEQ, D], F32, kind="ExternalOutput").ap()

    def scratch(name, shape, dt):
        kind = "ExternalOutput" if name in debug_out else "Internal"
        return nc.dram_tensor(name, shape, dt, kind=kind).ap()

    def sb(name, shape, dt, stack=es):
        return stack.enter_context(nc.sbuf_tensor(name, shape, dt))

    def ps(name, shape, dt, stack=es):
        return stack.enter_context(nc.psum_tensor(name, shape, dt))

    modv = scratch("modv", [8, D], F32)
    qdT = scratch("qdT", [16, 128, SEQ], BF16)
    kdT = scratch("kdT", [16, 128, SEQ], BF16)
    vd = scratch("vd", [SEQ, 2048], BF16)
    cqT = scratch("cqT", [6, 128, SEQ], F32)
    ckvT = scratch("ckvT", [4, 128, SEQ], F32)
    kpeT = scratch("kpeT", [64, SEQ], F32)
    kpeswT = scratch("kpeswT", [64, SEQ], F32)
    gates = scratch("gates", [SEQ, 2 * D], BF16)
    T_modv = Tl(); T_qdT = Tl(); T_kdT = Tl(); T_vd = Tl(); T_cqT = Tl(); T_ckvT = Tl(); T_kpeT = Tl(); T_gates = Tl()

    ident_f = sb("ident_f", [128, 128], F32)
    ident_b = sb("ident_b", [128, 128], BF16)
    modT = sb("modT", [128, 256], F32)
    T_ident = Tl(); T_modT = Tl()
    d_misc = S.dsem("misc")
    S.op("sp", lambda e: e.dma_start(out=ident_f[:], in_=ident_in), writes=[T_ident], dsem=T_ident)
    S.op("dve", lambda e: e.tensor_copy(out=ident_b[:], in_=ident_f[:]), reads=[T_ident], writes=[T_ident])

    PB = [ps(f"pb{i}", [128, 512], F32) for i in range(8)]
    T_PB = [Tl(f"pb{i}") for i in range(8)]

    with ExitStack() as p0:
        cT = sb("cT", [128, 32], F32, p0)
        bT = sb("bT", [128, 192], F32, p0)
        nT = sb("nT", [128, 64], F32, p0)
        wa = [sb(f"wa{i}", [128, 32, 256], F32, p0) for i in range(2)]
        T_cT = Tl(); T_bT = Tl(); T_nT = Tl(); T_wa = [Tl(), Tl()]
        d_wa = [S.dsem("wa0"), S.dsem("wa1")]

        def small_T(eng, dst, src_vec, n, tl):
            S.op(eng, lambda e: e.dma_start(out=dst, in_=src_vec.rearrange("(j p) -> p j", p=128),
                                            allow_slow_non_contiguous=True), writes=[tl], dsem=tl)
        small_T("sp", cT[:], c_in, 32, T_cT)
        S.op("sp", lambda e: e.dma_start(out=bT[:], in_=W["b_ada"].rearrange("(j p) -> p j", p=128),
                                         allow_slow_non_contiguous=True), writes=[T_bT], dsem=T_bT)
        S.op("sp", lambda e: e.dma_start(out=nT[:, 0:32], in_=W["norm_attn"].rearrange("(j p) -> p j", p=128),
                                         allow_slow_non_contiguous=True), pwrites=[T_nT], dsem=T_nT)
        S.op("sp", lambda e: e.dma_start(out=nT[:, 32:64], in_=W["norm_ffn"].rearrange("(j p) -> p j", p=128),
                                         allow_slow_non_contiguous=True), pwrites=[T_nT], dsem=T_nT)
        S.op("act", lambda e: e.activation(out=cT[:], in_=cT[:], func=AF.Silu), reads=[T_cT], writes=[T_cT])
        NB0 = 96
        pm = PB[0]

        def load_wa(eb):
            i = eb % 2
            S.op("sp", lambda e: e.dma_start(out=wa[i][:], in_=W["w_ada"][:, eb * 256:(eb + 1) * 256]
                                             .rearrange("(j p) c -> p j c", p=128)),
                 writes=[T_wa[i]], dsem=d_wa[i])
        load_wa(0)
        for eb in range(NB0):
            if eb + 1 < NB0:
                load_wa(eb + 1)
            i = eb % 2
            for half in range(2):
                col = eb * 2 + half
                for j in range(32):
                    S.op("pe", lambda e, i=i, j=j, half=half, col=col: e.matmul(
                        pm[:, col:col + 1], lhsT=wa[i][:, j, half * 128:(half + 1) * 128], rhs=cT[:, j:j + 1],
                        start=(j == 0), stop=(j == 31)),
                        reads=[T_wa[i], T_cT], writes=[T_PB[0]])
        S.op("dve", lambda e: e.tensor_tensor(out=modT[:, 0:192], in0=pm[:, 0:192], in1=bT[:], op=ALU.add),
             reads=[T_PB[0], T_bT], writes=[T_modT])
        S.op("dve", lambda e: e.scalar_tensor_tensor(out=modT[:, 192:224], in0=modT[:, 32:64], scalar=1.0, in1=nT[:, 0:32],
                                                     op0=ALU.add, op1=ALU.mult), reads=[T_modT, T_nT], writes=[T_modT])
        S.op("dve", lambda e: e.scalar_tensor_tensor(out=modT[:, 224:256], in0=modT[:, 128:160], scalar=1.0, in1=nT[:, 32:64],
                                                     op0=ALU.add, op1=ALU.mult), reads=[T_modT, T_nT], writes=[T_modT])
        S.op("sp", lambda e: e.dma_start(out=modv.rearrange("i (j p) -> p i j", p=128), in_=modT[:].rearrange("p (i j) -> p i j", j=32),
                                         allow_slow_non_contiguous=True), reads=[T_modT], writes=[T_modv], dsem=T_modv)
        S.barrier()
    if stage <= 0:
        S.emit()
        es.close()
        return nc

    hstack = ExitStack()
    hT = sb("hT", [128, NDC, SEQ], BF16, hstack)
    T_hT = [Tl(f"hT{g}") for g in range(4)]
    with ExitStack() as p1:
        xt = [sb(f"xt{i}", [128, D], F32, p1) for i in range(2)]
        junk = sb("junk", [128, D], BF16, p1)
        xs = sb("xs", [128, 4, D], BF16, p1)
        st = sb("st", [128, NT], F32, p1)
        T_xt = [Tl(), Tl()]; T_junk = Tl(); T_xs = [Tl() for _ in range(4)]; T_st = [Tl() for _ in range(NT)]
        d_xt = [S.dsem("xt0"), S.dsem("xt1")]
        tpb = [PB[1], PB[2]]
        T_tp = [T_PB[1], T_PB[2]]
        ev = 0
        for g in range(4):
            for tt in range(4):
                t = 4 * g + tt
                i = t % 2
                S.op("sp", lambda e, i=i, t=t: e.dma_start(out=xt[i][:], in_=x_in[t * 128:(t + 1) * 128, :]),
                     writes=[T_xt[i]], dsem=d_xt[i])
                S.op("act", lambda e, i=i, t=t: e.activation(out=junk[:], in_=xt[i][:], func=AF.Square, accum_out=st[:, t:t + 1]),
                     reads=[T_xt[i]], writes=[T_junk, T_st[t]])
                S.op("dve", lambda e, t=t: e.tensor_scalar(out=st[:, t:t + 1], in0=st[:, t:t + 1], scalar1=1.0 / D, scalar2=EPS,
                                                           op0=ALU.mult, op1=ALU.add), reads=[T_st[t]], writes=[T_st[t]])
                S.op("act", lambda e, t=t: e.activation(out=st[:, t:t + 1], in_=st[:, t:t + 1], func=AF.Sqrt),
                     reads=[T_st[t]], writes=[T_st[t]])
                S.op("dve", lambda e, t=t: e.reciprocal(out=st[:, t:t + 1], in_=st[:, t:t + 1]), reads=[T_st[t]], writes=[T_st[t]])
                S.op("act", lambda e, i=i, t=t, tt=tt: e.activation(out=xs[:, tt, :], in_=xt[i][:], func=AF.Copy, scale=st[:, t:t + 1]),
                     reads=[T_xt[i], T_st[t]], writes=[T_xs[tt]])
            for dc in range(NDC):
                k = dc % 2
                tpv = tpb[k][:].bitcast(BF16)
                for tt in range(4):
                    S.op("pe", lambda e, tpv=tpv, tt=tt, dc=dc: e.transpose(tpv[:, tt * 128:(tt + 1) * 128],
                                                                           xs[:, tt, dc * 128:(dc + 1) * 128], ident_b[:]),
                         reads=[T_xs[tt], T_ident], writes=[T_tp[k]] if tt == 0 else (), pwrites=() if tt == 0 else [T_tp[k]])
                if ev % 2 == 0:
                    S.op("dve", lambda e, tpv=tpv, dc=dc, g=g: e.tensor_scalar(
                        out=hT[:, dc, g * 512:(g + 1) * 512], in0=tpv[:, 0:512], scalar1=modT[:, 192 + dc:193 + dc],
                        scalar2=modT[:, dc:dc + 1], op0=ALU.mult, op1=ALU.add),
                        reads=[T_tp[k], T_modT], pwrites=[T_hT[g]])
                else:
                    S.op("act", lambda e, tpv=tpv, dc=dc, g=g: e.activation(
                        out=hT[:, dc, g * 512:(g + 1) * 512], in_=tpv[:, 0:512], func=AF.Identity,
                        scale=modT[:, 192 + dc:193 + dc], bias=modT[:, dc:dc + 1]),
                        reads=[T_tp[k], T_modT], pwrites=[T_hT[g]])
                ev += 1
        S.barrier()
    if stage <= 1:
        if "hT_dbg" in debug_out:
            hdbg = scratch("hT_dbg", [128, NDC, SEQ], BF16)
            S.op("sp", lambda e: e.dma_start(out=hdbg, in_=hT[:]), reads=T_hT, dsem=Tl())
            S.barrier()
        S.emit()
        es.close()
        return nc

    with ExitStack() as p2:
        CB = 256
        wb = [sb(f"wb{i}", [128, NDC, CB], BF16, p2) for i in range(2)]
        T_wb = [Tl(), Tl()]
        d_wb = [S.dsem("wb0"), S.dsem("wb1")]
        fstage_b = [sb(f"fsb{i}", [128, SEQ], BF16, p2) for i in range(2)]
        fstage_f = [sb(f"fsf{i}", [128, SEQ], F32, p2) for i in range(2)]
        tstage = [sb(f"tst{i}", [128, NT, CB], BF16, p2) for i in range(2)]
        T_fsb = [Tl(), Tl()]; T_fsf = [Tl(), Tl()]; T_tst = [Tl(), Tl()]
        d_fsb = [S.dsem("fsb0"), S.dsem("fsb1")]
        d_fsf = [S.dsem("fsf0"), S.dsem("fsf1")]
        d_tst = [S.dsem("tst0"), S.dsem("tst1")]
        blocks = []
        for c0 in range(0, 2048, CB):
            blocks.append((OFF_Q + c0, CB, "fb", (qdT, T_qdT, c0 // 128)))
        for c0 in range(0, 2048, CB):
            blocks.append((OFF_K + c0, CB, "fb", (kdT, T_kdT, c0 // 128)))
        for c0 in range(0, 768, CB):
            blocks.append((OFF_CQ + c0, CB, "ff", (cqT, T_cqT, c0 // 128)))
        for c0 in range(0, 512, CB):
            blocks.append((OFF_CKV + c0, CB, "ff", (ckvT, T_ckvT, c0 // 128)))
        blocks.append((OFF_KPE, 64, "kpe", None))
        blocks.append((OFF_KPE, 64, "kpesw", None))
        for c0 in range(0, 2048, CB):
            blocks.append((OFF_V + c0, CB, "tv", c0))
        for c0 in range(0, 2 * D, CB):
            blocks.append((OFF_G + c0, CB, "tg", c0))

        def load_wb(bi):
            col0, ncols, kind, info = blocks[bi]
            i = bi % 2
            S.op("pool", lambda e: e.dma_start(out=wb[i][:, :, 0:ncols], in_=W["w_in"][:, col0:col0 + ncols]
                                               .rearrange("(j p) c -> p j c", p=128)),
                 writes=[T_wb[i]], dsem=d_wb[i])
        load_wb(0)
        pbi = 0
        nfb = 0; nff = 0; ntst = 0
        evc = 0
        for bi in range(len(blocks)):
            if bi + 1 < len(blocks):
                load_wb(bi + 1)
            col0, ncols, kind, info = blocks[bi]
            i = bi % 2
            if kind == "kpesw":
                S.op("dve", lambda e, i=i: e.tensor_scalar(out=wb[i][:, :, 64:96], in0=wb[i][:, :, 32:64], scalar1=-1.0, scalar2=None, op0=ALU.mult),
                     reads=[T_wb[i]], writes=[T_wb[i]])
                S.op("dve", lambda e, i=i: e.tensor_copy(out=wb[i][:, :, 96:128], in_=wb[i][:, :, 0:32]),
                     reads=[T_wb[i]], writes=[T_wb[i]])
            if kind in ("fb", "ff", "kpe", "kpesw"):
                nch = 1 if kind in ("kpe", "kpesw") else ncols // 128
                woff = 64 if kind == "kpesw" else 0
                for ch in range(nch):
                    m = 64 if kind in ("kpe", "kpesw") else 128
                    if kind == "fb":
                        stg, T_stg, d_stg = fstage_b[nfb % 2], T_fsb[nfb % 2], d_fsb[nfb % 2]
                        nfb += 1
                    else:
                        stg, T_stg, d_stg = fstage_f[nff % 2], T_fsf[nff % 2], d_fsf[nff % 2]
                        nff += 1
                    for tb in range(4):
                        bank = pbi % 8
                        pbi += 1
                        for dc in range(NDC):
                            S.op("pe", lambda e, bank=bank, i=i, dc=dc, ch=ch, tb=tb, m=m, woff=woff: e.matmul(
                                PB[bank][0:m, :], lhsT=wb[i][:, dc, woff + ch * 128:woff + ch * 128 + m], rhs=hT[:, dc, tb * 512:(tb + 1) * 512],
                                start=(dc == 0), stop=(dc == NDC - 1)),
                                reads=[T_wb[i], T_hT[tb]], writes=[T_PB[bank]])
                        eng = "dve" if evc % 2 == 0 else "act"
                        evc += 1
                        if eng == "dve":
                            S.op("dve", lambda e, bank=bank, stg=stg, tb=tb, m=m: e.tensor_copy(
                                out=stg[0:m, tb * 512:(tb + 1) * 512], in_=PB[bank][0:m, :]),
                                reads=[T_PB[bank]], writes=[T_stg] if tb == 0 else (), pwrites=() if tb == 0 else [T_stg])
                        else:
                            S.op("act", lambda e, bank=bank, stg=stg, tb=tb, m=m: e.copy(
                                out=stg[0:m, tb * 512:(tb + 1) * 512], in_=PB[bank][0:m, :]),
                                reads=[T_PB[bank]], writes=[T_stg] if tb == 0 else (), pwrites=() if tb == 0 else [T_stg])
                    if kind == "kpe":
                        dst, T_dst = kpeT, T_kpeT
                    elif kind == "kpesw":
                        dst, T_dst = kpeswT, T_kpeT
                    else:
                        dst, T_dst = info[0][info[2] + ch], info[1]
                    S.op("sp", lambda e, dst=dst, stg=stg, m=m: e.dma_start(out=dst, in_=stg[0:m, :]),
                         reads=[T_stg], pwrites=[T_dst], dsem=d_stg)
            else:
                stg, T_stg, d_stg = tstage[ntst % 2], T_tst[ntst % 2], d_tst[ntst % 2]
                ntst += 1
                for t in range(NT):
                    bank = pbi % 8
                    pbi += 1
                    for dc in range(NDC):
                        S.op("pe", lambda e, bank=bank, i=i, dc=dc, t=t: e.matmul(
                            PB[bank][:, 0:CB], lhsT=hT[:, dc, t * 128:(t + 1) * 128], rhs=wb[i][:, dc, :],
                            start=(dc == 0), stop=(dc == NDC - 1)),
                            reads=[T_wb[i], T_hT[t // 4]], writes=[T_PB[bank]])
                    if kind == "tg":
                        S.op("act", lambda e, bank=bank, stg=stg, t=t: e.activation(out=stg[:, t, :], in_=PB[bank][:, 0:CB], func=AF.Sigmoid),
                             reads=[T_PB[bank]], writes=[T_stg] if t == 0 else (), pwrites=() if t == 0 else [T_stg])
                    else:
                        S.op("dve", lambda e, bank=bank, stg=stg, t=t: e.tensor_copy(out=stg[:, t, :], in_=PB[bank][:, 0:CB]),
                             reads=[T_PB[bank]], writes=[T_stg] if t == 0 else (), pwrites=() if t == 0 else [T_stg])
                if kind == "tv":
                    dst, T_dst = vd[:, info:info + CB], T_vd
                else:
                    dst, T_dst = gates[:, info:info + CB], T_gates
                S.op("sp", lambda e, dst=dst, stg=stg: e.dma_start(out=dst.rearrange("(t p) c -> p t c", p=128), in_=stg[:]),
                     reads=[T_stg], pwrites=[T_dst], dsem=d_stg)
        S.barrier()
    hstack.close()
    if stage <= 2:
        S.emit()
        es.close()
        return nc
    qnT = scratch("qnT", [16, 128, SEQ], BF16)
    qrT = scratch("qrT", [16, 64, SEQ], BF16)
    knT = scratch("knT", [16, 128, SEQ], BF16)
    krT = scratch("krT", [64, SEQ], BF16)
    vm = scratch("vm", [SEQ, 2048], BF16)
    T_qnT = Tl(); T_qrT = Tl(); T_knT = Tl(); T_krT = Tl(); T_vm = Tl()
    ones_b = sb("ones_b", [128, 128], BF16)
    p34 = ExitStack()
    rm_f = sb("rm_f", [128, 64], F32, p34)
    rm_b = sb("rm_b", [128, 64], BF16, p34)
    cos2 = sb("cos2", [64, SEQ], F32, p34)
    sin2 = sb("sin2", [64, SEQ], F32, p34)
    T_ones = Tl(); T_rm = Tl(); T_cs = Tl()
    S.op("dve", lambda e: e.memset(ones_b[:], 1.0), writes=[T_ones])
    S.op("sp", lambda e: e.dma_start(out=rm_f[:], in_=rmat_in), writes=[T_rm], dsem=T_rm)
    S.op("dve", lambda e: e.tensor_copy(out=rm_b[:], in_=rm_f[:]), reads=[T_rm], writes=[T_rm])
    PI = float(np.pi)
    with ExitStack() as p3a:
        posi = sb("posi", [64, SEQ], I32, p3a)
        ang = sb("ang", [64, SEQ], F32, p3a)
        tmpa = sb("tmpa", [64, SEQ], F32, p3a)
        pidx_i = sb("pidx_i", [64, 1], I32, p3a)
        invf = sb("invf", [64, 1], F32, p3a)
        frac = sb("frac", [64, SEQ], F32, p3a)
        T_posi = Tl(); T_ang = Tl(); T_tmpa = Tl(); T_invf = Tl(); T_frac = Tl()
        S.op("sp", lambda e: e.dma_start(out=posi[:], in_=pos_in.partition_broadcast(64)), writes=[T_posi], dsem=T_posi)
        S.op("pool", lambda e: e.iota(pidx_i[0:32, :], pattern=[[0, 1]], base=0, channel_multiplier=1), pwrites=[T_invf])
        S.op("pool", lambda e: e.iota(pidx_i[32:64, :], pattern=[[0, 1]], base=0, channel_multiplier=1), pwrites=[T_invf])
        S.op("dve", lambda e: e.tensor_copy(out=invf[:], in_=pidx_i[:]), reads=[T_invf], writes=[T_invf])
        S.op("act", lambda e: e.activation(out=invf[:], in_=invf[:], func=AF.Exp, scale=-float(np.log(10000.0)) / 32.0),
             reads=[T_invf], writes=[T_invf])
        S.op("dve", lambda e: e.tensor_scalar(out=invf[:], in0=invf[:], scalar1=1.0 / (2.0 * PI), scalar2=None, op0=ALU.mult),
             reads=[T_invf], writes=[T_invf])
        S.op("dve", lambda e: e.tensor_copy(out=ang[:], in_=posi[:]), reads=[T_posi], writes=[T_ang])
        S.op("dve", lambda e: e.tensor_scalar(out=ang[:], in0=ang[:], scalar1=invf[:, 0:1], scalar2=None, op0=ALU.mult),
             reads=[T_ang, T_invf], writes=[T_ang])
        ki = posi
        for (dst, shift) in ((sin2, 0.0), (cos2, 0.25)):
            S.op("dve", lambda e, shift=shift: e.tensor_scalar(out=tmpa[:], in0=ang[:], scalar1=shift, scalar2=None, op0=ALU.add),
                 reads=[T_ang], writes=[T_tmpa])
            S.op("dve", lambda e: e.tensor_copy(out=ki[:], in_=tmpa[:]), reads=[T_tmpa], writes=[T_posi])
            S.op("dve", lambda e: e.tensor_copy(out=frac[:], in_=ki[:]), reads=[T_posi], writes=[T_frac])
            S.op("dve", lambda e: e.tensor_tensor(out=tmpa[:], in0=tmpa[:], in1=frac[:], op=ALU.subtract),
                 reads=[T_tmpa, T_frac], writes=[T_tmpa])
            S.op("dve", lambda e: e.tensor_single_scalar(out=frac[:], in_=tmpa[:], scalar=0.5, op=ALU.is_gt),
                 reads=[T_tmpa], writes=[T_frac])
            S.op("dve", lambda e: e.tensor_tensor(out=tmpa[:], in0=tmpa[:], in1=frac[:], op=ALU.subtract),
                 reads=[T_tmpa, T_frac], writes=[T_tmpa])
            S.op("dve", lambda e: e.tensor_single_scalar(out=frac[:], in_=tmpa[:], scalar=-0.5, op=ALU.is_lt),
                 reads=[T_tmpa], writes=[T_frac])
            S.op("dve", lambda e: e.tensor_tensor(out=tmpa[:], in0=tmpa[:], in1=frac[:], op=ALU.add),
                 reads=[T_tmpa, T_frac], writes=[T_tmpa])
            S.op("act", lambda e, dst=dst: e.activation(out=dst[:], in_=tmpa[:], func=AF.Sin, scale=2.0 * PI), reads=[T_tmpa], pwrites=[T_cs])
        S.barrier()
    if "cs_dbg" in debug_out:
        csd = scratch("cs_dbg", [2, 64, SEQ], F32)
        S.op("sp", lambda e: e.dma_start(out=csd[0], in_=cos2[:]), reads=[T_cs], dsem=Tl())
        S.op("sp", lambda e: e.dma_start(out=csd[1], in_=sin2[:]), reads=[T_cs], dsem=Tl())
        S.barrier()
        S.emit(); es.close(); return nc

    def rms_bcast(src, nch, n_feat, scr, rbc, T_src, T_scr, T_rbc):
        for ch in range(nch):
            S.op("act", lambda e, ch=ch: e.activation(out=scr[:, ch, :], in_=src[:, ch, :], func=AF.Square),
                 reads=[T_src], writes=[T_scr[ch]])
        for tb in range(4):
            for ch in range(nch):
                S.op("pe", lambda e, ch=ch, tb=tb: e.matmul(PB[0][:, :], lhsT=ones_b[:], rhs=scr[:, ch, tb * 512:(tb + 1) * 512],
                                                            start=(ch == 0), stop=(ch == nch - 1)),
                     reads=[T_ones, T_scr[ch]], writes=[T_PB[0]])
            S.op("dve", lambda e, tb=tb: e.tensor_scalar(out=rbc[:, tb * 512:(tb + 1) * 512], in0=PB[0][:, :], scalar1=1.0 / n_feat,
                                                         scalar2=EPS, op0=ALU.mult, op1=ALU.add),
                 reads=[T_PB[0]], pwrites=[T_rbc])
        S.op("act", lambda e: e.activation(out=rbc[:], in_=rbc[:], func=AF.Sqrt), reads=[T_rbc], writes=[T_rbc])
        S.op("dve", lambda e: e.reciprocal(out=rbc[:], in_=rbc[:]), reads=[T_rbc], writes=[T_rbc])

    def rope_block(t_ps, T_t, sw_ps, T_sw, tb, dst_stage, T_dst_stage, first, tmpu, T_tmpu):
        sl = slice(tb * 512, (tb + 1) * 512)
        S.op("dve", lambda e: e.tensor_tensor(out=tmpu[:], in0=t_ps, in1=cos2[:, sl], op=ALU.mult),
             reads=[T_t, T_cs], writes=[T_tmpu])
        S.op("dve", lambda e: e.tensor_tensor(out=tmpv[:], in0=sw_ps, in1=sin2[:, sl], op=ALU.mult),
             reads=[T_sw, T_cs], writes=[T_tmpv])
        S.op("dve", lambda e: e.tensor_tensor(out=dst_stage[0:64, sl], in0=tmpu[:], in1=tmpv[:], op=ALU.add),
             reads=[T_tmpu, T_tmpv], writes=[T_dst_stage] if first else (), pwrites=() if first else [T_dst_stage])

    tmpv = sb("tmpv", [64, 512], F32, p34)
    T_tmpv = Tl()

    with ExitStack() as p3q:
        cq = sb("cq", [128, 6, SEQ], F32, p3q)
        cqn = sb("cqn", [128, 6, SEQ], BF16, p3q)
        rq = sb("rq", [128, SEQ], F32, p3q)
        wuq = sb("wuq", [128, 6, 3072], BF16, p3q)
        gq = sb("gq", [128, 6], F32, p3q)
        stg = [sb(f"q3s{i}", [128, SEQ], BF16, p3q) for i in range(2)]
        wsw = sb("wsw", [128, 6, 16, 64], BF16, p3q)
        tmpu = sb("tmpu", [64, 512], F32, p3q)
        T_cq = Tl(); T_cqn = [Tl() for _ in range(6)]; T_rq = Tl(); T_wuq = Tl(); T_gq = Tl()
        T_stg = [Tl(), Tl()]; T_tb16 = Tl(); T_tmpu = Tl()
        d_stg = [S.dsem("q3s0"), S.dsem("q3s1")]
        T_wsw = Tl()
        S.op("sp", lambda e: e.dma_start(out=cq[:], in_=cqT.rearrange("c p t -> p c t")), reads=[T_cqT], writes=[T_cq], dsem=T_cq)
        S.op("sp", lambda e: e.dma_start(out=gq[:], in_=W["mla_q_norm"].rearrange("(j p) -> p j", p=128), allow_slow_non_contiguous=True),
             writes=[T_gq], dsem=T_gq)
        for ch in range(6):
            S.op("pool", lambda e, ch=ch: e.dma_start(out=wuq[:, ch, :], in_=W["mla_w_uq"][ch * 128:(ch + 1) * 128, :], max_dma_last_dim=4096),
                 pwrites=[T_wuq], dsem=T_wuq)
        wr = wuq[:].rearrange("p c (h d) -> p c h d", d=192)
        for ch in range(6):
            S.op("dve", lambda e, ch=ch: e.tensor_scalar(out=wsw[:, ch, :, 0:32], in0=wr[:, ch, :, 160:192], scalar1=-1.0, scalar2=None, op0=ALU.mult),
                 reads=[T_wuq], pwrites=[T_wsw])
            S.op("dve", lambda e, ch=ch: e.tensor_copy(out=wsw[:, ch, :, 32:64], in_=wr[:, ch, :, 128:160]),
                 reads=[T_wuq], pwrites=[T_wsw])
        rms_bcast(cq, 6, 768.0, cqn, rq, T_cq, T_cqn, T_rq)
        for ch in range(6):
            S.op("dve", lambda e, ch=ch: e.scalar_tensor_tensor(out=cqn[:, ch, :], in0=cq[:, ch, :], scalar=gq[:, ch:ch + 1], in1=rq[:],
                                                                op0=ALU.mult, op1=ALU.mult),
                 reads=[T_cq, T_gq, T_rq], writes=[T_cqn[ch]])
        if "cqn_dbg" in debug_out:
            cqn_d = scratch("cqn_dbg", [128, 6, SEQ], BF16)
            rq_d = scratch("rq_dbg", [128, SEQ], F32)
            S.op("sp", lambda e: e.dma_start(out=cqn_d, in_=cqn[:]), reads=T_cqn, dsem=Tl())
            S.op("sp", lambda e: e.dma_start(out=rq_d, in_=rq[:]), reads=[T_rq], dsem=Tl())
        ns = 0
        for h in range(0 if "q_norm_only" not in debug_out else 16, 16):
            for part in range(2 if "q_nope_only" not in debug_out else 1):
                m = 128 if part == 0 else 64
                c0 = h * 192 + (0 if part == 0 else 128)
                st_i = ns % 2
                ns += 1
                for tb in range(4):
                    bank = 1 + (tb % 2) + 2 * part
                    for ch in range(6):
                        S.op("pe", lambda e, bank=bank, ch=ch, tb=tb, m=m, c0=c0: e.matmul(
                            PB[bank][0:m, :], lhsT=wuq[:, ch, c0:c0 + m], rhs=cqn[:, ch, tb * 512:(tb + 1) * 512],
                            start=(ch == 0), stop=(ch == 5)), reads=[T_wuq, T_cqn[ch]], writes=[T_PB[bank]])
                    if part == 0:
                        S.op("act", lambda e, bank=bank, tb=tb, st_i=st_i: e.copy(out=stg[st_i][:, tb * 512:(tb + 1) * 512], in_=PB[bank][:, :]),
                             reads=[T_PB[bank]], writes=[T_stg[st_i]] if tb == 0 else (), pwrites=() if tb == 0 else [T_stg[st_i]])
                    elif "rope_plain" in debug_out:
                        S.op("act", lambda e, bank=bank, tb=tb, st_i=st_i: e.copy(out=stg[st_i][0:64, tb * 512:(tb + 1) * 512], in_=PB[bank][0:64, :]),
                             reads=[T_PB[bank]], writes=[T_stg[st_i]] if tb == 0 else (), pwrites=() if tb == 0 else [T_stg[st_i]])
                    else:
                        bsw = 5 + (tb % 2)
                        for ch in range(6):
                            S.op("pe", lambda e, bsw=bsw, ch=ch, tb=tb, h=h: e.matmul(
                                PB[bsw][0:64, :], lhsT=wsw[:, ch, h, :], rhs=cqn[:, ch, tb * 512:(tb + 1) * 512],
                                start=(ch == 0), stop=(ch == 5)), reads=[T_wsw, T_cqn[ch]], writes=[T_PB[bsw]])
                        rope_block(PB[bank][0:64, :], T_PB[bank], PB[bsw][0:64, :], T_PB[bsw], tb, stg[st_i], T_stg[st_i], tb == 0, tmpu, T_tmpu)
                dst, T_dst = (qnT[h], T_qnT) if part == 0 else (qrT[h], T_qrT)
                S.op("sp", lambda e, dst=dst, st_i=st_i, m=m: e.dma_start(out=dst, in_=stg[st_i][0:m, :]),
                     reads=[T_stg[st_i]], pwrites=[T_dst], dsem=d_stg[st_i])
        S.barrier()
    if stage <= 3 and "q_only" in debug_out:
        S.emit(); es.close(); return nc

    with ExitStack() as p3k:
        ckv = sb("ckv", [128, 4, SEQ], F32, p3k)
        ckvn = sb("ckvn", [128, 4, SEQ], BF16, p3k)
        rkv = sb("rkv", [128, SEQ], F32, p3k)
        wukv = sb("wukv", [128, 4, 4096], BF16, p3k)
        gkv = sb("gkv", [128, 4], F32, p3k)
        kpe = sb("kpe", [64, SEQ], F32, p3k)
        kpesw = sb("kpesw", [64, SEQ], F32, p3k)
        kstg = [sb(f"k3s{i}", [128, SEQ], BF16, p3k) for i in range(2)]
        vst = [sb(f"v3s{i}", [128, NT, 512], BF16, p3k) for i in range(2)]
        ktmpu = sb("ktmpuk", [64, 512], F32, p3k)
        T_ckv = Tl(); T_ckvn = [Tl() for _ in range(4)]; T_rkv = Tl(); T_wukv = Tl(); T_gkv = Tl(); T_kpe = Tl()
        T_kkstg = [Tl(), Tl()]; T_vst = [Tl(), Tl()]; T_tb16 = Tl(); T_kktmpu = Tl()
        d_kkstg = [S.dsem("k3s0"), S.dsem("k3s1")]
        d_vst = [S.dsem("v3s0"), S.dsem("v3s1")]
        S.op("sp", lambda e: e.dma_start(out=ckv[:], in_=ckvT.rearrange("c p t -> p c t")), reads=[T_ckvT], writes=[T_ckv], dsem=T_ckv)
        S.op("sp", lambda e: e.dma_start(out=kpe[:], in_=kpeT), reads=[T_kpeT], writes=[T_kpe], dsem=T_kpe)
        S.op("sp", lambda e: e.dma_start(out=kpesw[:], in_=kpeswT), reads=[T_kpeT], pwrites=[T_kpe], dsem=T_kpe)
        S.op("sp", lambda e: e.dma_start(out=gkv[:], in_=W["mla_kv_norm"].rearrange("(j p) -> p j", p=128), allow_slow_non_contiguous=True),
             writes=[T_gkv], dsem=T_gkv)
        for ch in range(4):
            S.op("pool", lambda e, ch=ch: e.dma_start(out=wukv[:, ch, :], in_=W["mla_w_ukv"][ch * 128:(ch + 1) * 128, :], max_dma_last_dim=4096),
                 pwrites=[T_wukv], dsem=T_wukv)
        rms_bcast(ckv, 4, 512.0, ckvn, rkv, T_ckv, T_ckvn, T_rkv)
        for ch in range(4):
            S.op("dve", lambda e, ch=ch: e.scalar_tensor_tensor(out=ckvn[:, ch, :], in0=ckv[:, ch, :], scalar=gkv[:, ch:ch + 1], in1=rkv[:],
                                                                op0=ALU.mult, op1=ALU.mult),
                 reads=[T_ckv, T_gkv, T_rkv], writes=[T_ckvn[ch]])
        for tb in range(4):
            sl = slice(tb * 512, (tb + 1) * 512)
            S.op("dve", lambda e, sl=sl: e.tensor_tensor(out=ktmpu[:], in0=kpe[:, sl], in1=cos2[:, sl], op=ALU.mult),
                 reads=[T_kpe, T_cs], writes=[T_kktmpu])
            S.op("dve", lambda e, sl=sl: e.tensor_tensor(out=tmpv[:], in0=kpesw[:, sl], in1=sin2[:, sl], op=ALU.mult),
                 reads=[T_kpe, T_cs], writes=[T_tmpv])
            S.op("dve", lambda e, sl=sl, tb=tb: e.tensor_tensor(out=kstg[0][0:64, sl], in0=ktmpu[:], in1=tmpv[:], op=ALU.add),
                 reads=[T_kktmpu, T_tmpv], writes=[T_kkstg[0]] if tb == 0 else (), pwrites=() if tb == 0 else [T_kkstg[0]])
        S.op("sp", lambda e: e.dma_start(out=krT, in_=kstg[0][0:64, :]), reads=[T_kkstg[0]], writes=[T_krT], dsem=d_kkstg[0])
        ns = 1
        for h in range(16):
            st_i = ns % 2
            ns += 1
            for tb in range(4):
                bank = 1 + (tb % 2)
                for ch in range(4):
                    S.op("pe", lambda e, bank=bank, ch=ch, tb=tb, h=h: e.matmul(
                        PB[bank][:, :], lhsT=wukv[:, ch, h * 256:h * 256 + 128], rhs=ckvn[:, ch, tb * 512:(tb + 1) * 512],
                        start=(ch == 0), stop=(ch == 3)), reads=[T_wukv, T_ckvn[ch]], writes=[T_PB[bank]])
                S.op("act", lambda e, bank=bank, tb=tb, st_i=st_i: e.copy(out=kstg[st_i][:, tb * 512:(tb + 1) * 512], in_=PB[bank][:, :]),
                     reads=[T_PB[bank]], writes=[T_kkstg[st_i]] if tb == 0 else (), pwrites=() if tb == 0 else [T_kkstg[st_i]])
            S.op("sp", lambda e, h=h, st_i=st_i: e.dma_start(out=knT[h], in_=kstg[st_i][:, :]),
                 reads=[T_kkstg[st_i]], pwrites=[T_knT], dsem=d_kkstg[st_i])
        wv = wukv[:].rearrange("p c (h two d) -> p c h two d", two=2, d=128)
        for g4 in range(4):
            vi = g4 % 2
            for t in range(NT):
                bank = 3 + (t % 2)
                for ch in range(4):
                    S.op("pe", lambda e, bank=bank, ch=ch, t=t, g4=g4: e.matmul(
                        PB[bank][:, :], lhsT=ckvn[:, ch, t * 128:(t + 1) * 128], rhs=wv[:, ch, 4 * g4:4 * g4 + 4, 1, :],
                        start=(ch == 0), stop=(ch == 3)), reads=[T_wukv, T_ckvn[ch]], writes=[T_PB[bank]])
                S.op("dve", lambda e, bank=bank, t=t, vi=vi: e.tensor_copy(out=vst[vi][:, t, :], in_=PB[bank][:, :]),
                     reads=[T_PB[bank]], writes=[T_vst[vi]] if t == 0 else (), pwrites=() if t == 0 else [T_vst[vi]])
            S.op("sp", lambda e, g4=g4, vi=vi: e.dma_start(out=vm[:, g4 * 512:(g4 + 1) * 512].rearrange("(t p) c -> p t c", p=128), in_=vst[vi][:]),
                 reads=[T_vst[vi]], pwrites=[T_vm], dsem=d_vst[vi])
        S.barrier()
    if stage <= 3:
        S.emit()
        es.close()
        return nc
    oT = scratch("oT", [32, 128, SEQ], BF16)
    T_oT = Tl()

    def phase4():
        p4 = ExitStack()
        BIG = 1.0e9
        SC_D = 128.0 ** -0.5
        SC_M = 192.0 ** -0.5
        lamv = sb("lamv", [128, 512], F32, p4)
        lam2 = sb("lam2", [128, 4], F32, p4)
        gsub = sb("gsub", [128, 2], F32, p4)
        maskB = sb("maskB", [128, 4, 512], F32, p4)
        mki = sb("mki", [128, 512], I32, p4)
        posk_i = sb("posk_i", [128, NT], I32, p4)
        posk = sb("posk", [128, NT], F32, p4)
        posq_i = sb("posq_i", [128, 512], I32, p4)
        posq = sb("posq", [128, 512], F32, p4)
        dist = sb("dist", [128, NT, 512], F32, p4)
        T_lam = Tl(); T_gsub = Tl(); T_maskB = Tl(); T_mki = Tl(); T_posk = Tl(); T_posq = Tl()
        T_dist = [Tl() for _ in range(NT)]
        S.op("sp", lambda e: e.dma_start(out=lamv[:], in_=W["diff_lambda"].rearrange("a b -> (a b)").partition_broadcast(128)),
             writes=[T_lam], dsem=T_lam)
        S.op("dve", lambda e: e.tensor_tensor(out=lamv[:, 0:128], in0=lamv[:, 0:128], in1=lamv[:, 128:256], op=ALU.mult), reads=[T_lam], writes=[T_lam])
        S.op("dve", lambda e: e.tensor_tensor(out=lamv[:, 256:384], in0=lamv[:, 256:384], in1=lamv[:, 384:512], op=ALU.mult), reads=[T_lam], writes=[T_lam])
        S.op("dve", lambda e: e.reduce_sum(out=lam2[:, 0:1], in_=lamv[:, 0:128], axis=AX.X), reads=[T_lam], writes=[T_lam])
        S.op("dve", lambda e: e.reduce_sum(out=lam2[:, 1:2], in_=lamv[:, 256:384], axis=AX.X), reads=[T_lam], writes=[T_lam])
        S.op("act", lambda e: e.activation(out=lam2[:, 0:2], in_=lam2[:, 0:2], func=AF.Exp), reads=[T_lam], writes=[T_lam])
        S.op("dve", lambda e: e.tensor_tensor(out=lam2[:, 2:3], in0=lam2[:, 0:1], in1=lam2[:, 1:2], op=ALU.subtract), reads=[T_lam], writes=[T_lam])
        S.op("dve", lambda e: e.tensor_scalar(out=lam2[:, 2:3], in0=lam2[:, 2:3], scalar1=LAM_INIT, scalar2=None, op0=ALU.add), reads=[T_lam], writes=[T_lam])
        S.op("dve", lambda e: e.tensor_scalar(out=lam2[:, 3:4], in0=lam2[:, 2:3], scalar1=-1.0, scalar2=None, op0=ALU.mult), reads=[T_lam], writes=[T_lam])
        S.op("sp", lambda e: e.dma_start(out=gsub[:], in_=W["diff_subln"].rearrange("(j p) -> p j", p=128), allow_slow_non_contiguous=True),
             writes=[T_gsub], dsem=T_gsub)
        S.op("dve", lambda e: e.tensor_scalar(out=gsub[:], in0=gsub[:], scalar1=1.0 - LAM_INIT, scalar2=None, op0=ALU.mult), reads=[T_gsub], writes=[T_gsub])
        for j in range(4):
            S.op("pool", lambda e, j=j: e.iota(mki[:], pattern=[[1, 512]], base=-128 * j, channel_multiplier=-1), writes=[T_mki])
            S.op("dve", lambda e, j=j: e.tensor_copy(out=maskB[:, j, :], in_=mki[:]), reads=[T_mki], pwrites=[T_maskB])
            S.op("dve", lambda e, j=j: e.tensor_single_scalar(out=maskB[:, j, :], in_=maskB[:, j, :], scalar=0.0, op=ALU.is_lt),
                 reads=[T_maskB], pwrites=[T_maskB])
            S.op("dve", lambda e, j=j: e.tensor_scalar(out=maskB[:, j, :], in0=maskB[:, j, :], scalar1=BIG, scalar2=None, op0=ALU.mult),
                 reads=[T_maskB], pwrites=[T_maskB])
        S.op("sp", lambda e: e.dma_start(out=posk_i[:], in_=pos_in.rearrange("(t p) -> p t", p=128), allow_slow_non_contiguous=True),
             writes=[T_posk], dsem=T_posk)
        S.op("dve", lambda e: e.tensor_copy(out=posk[:], in_=posk_i[:]), reads=[T_posk], writes=[T_posk])
        qb_t = [sb(f"a_q{i}", [128, 2, 512], BF16, p4) for i in range(2)]
        kb_t = [sb(f"a_k{i}", [128, 2, SEQ], BF16, p4) for i in range(2)]
        vb_t = [sb(f"a_v{i}", [128, NT, 256], BF16, p4) for i in range(2)]
        qr_t = [sb(f"a_qr{i}", [128, 512], BF16, p4) for i in range(2)]
        kr_t = sb("a_kr", [128, SEQ], BF16, p4)
        T_q = [Tl(), Tl()]; T_k = [Tl(), Tl()]; T_v = [Tl(), Tl()]; T_qr = [Tl(), Tl()]; T_kr = Tl()
        d_q = [S.dsem("aq0"), S.dsem("aq1")]; d_k = [S.dsem("ak0"), S.dsem("ak1")]; d_v = [S.dsem("av0"), S.dsem("av1")]
        d_qr = [S.dsem("aqr0"), S.dsem("aqr1")]
        NPB = 3
        tmp_t = [sb(f"a_tmp{i}", [128, 512], F32, p4) for i in range(NPB)]
        p_t = [sb(f"a_p{i}", [128, 512], BF16, p4) for i in range(NPB)]
        T_tmp = [Tl() for _ in range(NPB)]; T_p = [Tl() for _ in range(NPB)]
        rcp = sb("a_rcp", [128, 512], F32, p4)
        res = sb("a_res", [128, 2, 2, 512], F32, p4)
        av = sb("a_av", [128, 2, 512], F32, p4)
        asq = sb("a_sq", [128, 2, 512], BF16, p4)
        rsd = sb("a_rsd", [128, 512], F32, p4)
        ost = [sb(f"a_o{i}", [128, 512], BF16, p4) for i in range(4)]
        T_rcp = Tl(); T_res = [[Tl(), Tl()], [Tl(), Tl()]]; T_av = [Tl(), Tl()]; T_asq = [Tl(), Tl()]; T_rsd = Tl()
        T_ost = [Tl() for _ in range(4)]
        d_ost = [S.dsem(f"ao{i}") for i in range(4)]
        S.op("dve", lambda e: e.memset(kr_t[:], 0.0), writes=[T_kr])
        for i in range(2):
            S.op("dve", lambda e, i=i: e.memset(qr_t[i][:], 0.0), writes=[T_qr[i]])
        S.op("sp", lambda e: e.dma_start(out=kr_t[0:64, :], in_=krT), reads=[T_krT], pwrites=[T_kr], dsem=T_kr)
        cnt = {"s": 0, "p": 0, "o": 0, "ld": 0}

        def run_head(qb, nkb, s_mms, diff_slope, pv_lhsT, acc_banks, T_srcs):
            for kb in range(nkb):
                sb_i = cnt["s"] % 2
                cnt["s"] += 1
                s_mms(PB[sb_i], T_PB[sb_i], kb)
                pi = cnt["p"] % NPB
                cnt["p"] += 1
                diag = kb >= 4 * qb
                if diff_slope is not None:
                    S.op("dve", lambda e, pi=pi, sb_i=sb_i, kb=kb: e.scalar_tensor_tensor(
                        out=tmp_t[pi][:], in0=dist[:, kb, :], scalar=-diff_slope / SC_D, in1=PB[sb_i][:, :], op0=ALU.mult, op1=ALU.add),
                        reads=[T_dist[kb], T_PB[sb_i]], writes=[T_tmp[pi]])
                    S.op("act", lambda e, pi=pi: e.activation(out=p_t[pi][:], in_=tmp_t[pi][:], func=AF.Exp, scale=SC_D),
                         reads=[T_tmp[pi]], writes=[T_p[pi]])
                elif diag:
                    j = kb - 4 * qb
                    S.op("dve", lambda e, pi=pi, sb_i=sb_i, j=j: e.scalar_tensor_tensor(
                        out=tmp_t[pi][:], in0=maskB[:, j, :], scalar=-1.0e-4, in1=PB[sb_i][:, :], op0=ALU.mult, op1=ALU.add),
                        reads=[T_maskB, T_PB[sb_i]], writes=[T_tmp[pi]])
                    S.op("act", lambda e, pi=pi: e.activation(out=p_t[pi][:], in_=tmp_t[pi][:], func=AF.Exp, scale=SC_M),
                         reads=[T_tmp[pi]], writes=[T_p[pi]])
                else:
                    S.op("act", lambda e, pi=pi, sb_i=sb_i: e.activation(out=p_t[pi][:], in_=PB[sb_i][:, :], func=AF.Exp, scale=SC_M),
                         reads=[T_PB[sb_i]], writes=[T_p[pi]])
                for ai, lh in enumerate(pv_lhsT(kb)):
                    bk = acc_banks[ai]
                    S.op("pe", lambda e, bk=bk, lh=lh, pi=pi, kb=kb: e.matmul(PB[bk][:, :], lhsT=lh, rhs=p_t[pi][:],
                                                                               start=(kb == 0), stop=(kb == nkb - 1)),
                         reads=[T_p[pi]] + T_srcs, writes=[T_PB[bk]])

        for qb in range(4):
            nkb = 4 * qb + 4
            S.op("sp", lambda e, qb=qb: e.dma_start(out=posq_i[:], in_=pos_in[qb * 512:(qb + 1) * 512].partition_broadcast(128)),
                 writes=[T_posq], dsem=T_posq)
            S.op("dve", lambda e: e.tensor_copy(out=posq[:], in_=posq_i[:]), reads=[T_posq], writes=[T_posq])
            for kb in range(nkb):
                S.op("dve", lambda e, kb=kb: e.tensor_scalar(out=dist[:, kb, :], in0=posq[:], scalar1=posk[:, kb:kb + 1], scalar2=None,
                                                             op0=ALU.subtract), reads=[T_posq, T_posk], writes=[T_dist[kb]])
                S.op("dve", lambda e, kb=kb: e.scalar_tensor_tensor(out=dist[:, kb, :], in0=dist[:, kb, :], scalar=-1.0, in1=dist[:, kb, :],
                                                                    op0=ALU.mult, op1=ALU.max), reads=[T_dist[kb]], writes=[T_dist[kb]])
                if kb >= 4 * qb:
                    S.op("dve", lambda e, kb=kb, qb=qb: e.tensor_tensor(out=dist[:, kb, :], in0=dist[:, kb, :], in1=maskB[:, kb - 4 * qb, :], op=ALU.add),
                         reads=[T_maskB], writes=[T_dist[kb]])
            qs = slice(qb * 512, (qb + 1) * 512)
            nk = nkb * 128
            for h in range(8):
                li = cnt["ld"] % 2
                cnt["ld"] += 1
                for mi in range(2):
                    S.op("sp", lambda e, li=li, mi=mi, h=h, qs=qs: e.dma_start(out=qb_t[li][:, mi, :], in_=qdT[2 * h + mi][:, qs]),
                         reads=[T_qdT], writes=[T_q[li]] if mi == 0 else (), pwrites=() if mi == 0 else [T_q[li]], dsem=d_q[li])
                    S.op("sp", lambda e, li=li, mi=mi, h=h, nk=nk: e.dma_start(out=kb_t[li][:, mi, 0:nk], in_=kdT[2 * h + mi][:, 0:nk]),
                         reads=[T_kdT], writes=[T_k[li]] if mi == 0 else (), pwrites=() if mi == 0 else [T_k[li]], dsem=d_k[li])
                S.op("sp", lambda e, li=li, h=h, nk=nk, nkb=nkb: e.dma_start(
                    out=vb_t[li][:, 0:nkb, :], in_=vd[0:nk, h * 256:(h + 1) * 256].rearrange("(t p) c -> p t c", p=128)),
                    reads=[T_vd], writes=[T_v[li]], dsem=d_v[li])
                for mi in range(2):
                    banks = [2, 3, 4] if mi == 0 else [5, 6, 7]

                    def s_mms(ps, T_ps, kb, li=li, mi=mi):
                        S.op("pe", lambda e: e.matmul(ps[:, :], lhsT=kb_t[li][:, mi, kb * 128:(kb + 1) * 128], rhs=qb_t[li][:, mi, :],
                                                      start=True, stop=True), reads=[T_k[li], T_q[li]], writes=[T_ps])

                    def pv(kb, li=li):
                        return [vb_t[li][:, kb, 0:128], vb_t[li][:, kb, 128:256], ones_b[:]]
                    run_head(qb, nkb, s_mms, 2.0 ** -(h + 1), pv, banks, [T_v[li], T_ones])
                    S.op("dve", lambda e, banks=banks: e.reciprocal(out=rcp[:], in_=PB[banks[2]][:, :]), reads=[T_PB[banks[2]]], writes=[T_rcp])
                    for c in range(2):
                        S.op("dve", lambda e, banks=banks, c=c, mi=mi: e.tensor_tensor(out=res[:, mi, c, :], in0=PB[banks[c]][:, :], in1=rcp[:], op=ALU.mult),
                             reads=[T_PB[banks[c]], T_rcp], writes=[T_res[mi][c]])
                for c in range(2):
                    S.op("dve", lambda e, c=c: e.scalar_tensor_tensor(out=av[:, c, :], in0=res[:, 1, c, :], scalar=lam2[:, 3:4], in1=res[:, 0, c, :],
                                                                      op0=ALU.mult, op1=ALU.add),
                         reads=[T_res[0][c], T_res[1][c], T_lam], writes=[T_av[c]])
                    S.op("act", lambda e, c=c: e.activation(out=asq[:, c, :], in_=av[:, c, :], func=AF.Square), reads=[T_av[c]], writes=[T_asq[c]])
                for c in range(2):
                    S.op("pe", lambda e, c=c: e.matmul(PB[0][:, :], lhsT=ones_b[:], rhs=asq[:, c, :], start=(c == 0), stop=(c == 1)),
                         reads=[T_asq[c], T_ones], writes=[T_PB[0]])
                cnt["s"] += 1 if cnt["s"] % 2 == 0 else 0
                S.op("dve", lambda e: e.tensor_scalar(out=rsd[:], in0=PB[0][:, :], scalar1=1.0 / 256.0, scalar2=EPS, op0=ALU.mult, op1=ALU.add),
                     reads=[T_PB[0]], writes=[T_rsd])
                S.op("act", lambda e: e.activation(out=rsd[:], in_=rsd[:], func=AF.Sqrt), reads=[T_rsd], writes=[T_rsd])
                S.op("dve", lambda e: e.reciprocal(out=rsd[:], in_=rsd[:]), reads=[T_rsd], writes=[T_rsd])
                for c in range(2):
                    oi = cnt["o"] % 4
                    cnt["o"] += 1
                    S.op("dve", lambda e, c=c, oi=oi: e.scalar_tensor_tensor(out=ost[oi][:], in0=av[:, c, :], scalar=gsub[:, c:c + 1], in1=rsd[:],
                                                                             op0=ALU.mult, op1=ALU.mult),
                         reads=[T_av[c], T_gsub, T_rsd], writes=[T_ost[oi]])
                    S.op("sp", lambda e, c=c, oi=oi, h=h, qs=qs: e.dma_start(out=oT[2 * h + c][:, qs], in_=ost[oi][:]),
                         reads=[T_ost[oi]], pwrites=[T_oT], dsem=d_ost[oi])
            for h in range(16):
                li = cnt["ld"] % 2
                cnt["ld"] += 1
                S.op("sp", lambda e, li=li, h=h, qs=qs: e.dma_start(out=qb_t[li][:, 0, :], in_=qnT[h][:, qs]),
                     reads=[T_qnT], writes=[T_q[li]], dsem=d_q[li])
                S.op("sp", lambda e, li=li, h=h, qs=qs: e.dma_start(out=qr_t[li][0:64, :], in_=qrT[h][:, qs]),
                     reads=[T_qrT], pwrites=[T_qr[li]], dsem=d_qr[li])
                S.op("sp", lambda e, li=li, h=h, nk=nk: e.dma_start(out=kb_t[li][:, 0, 0:nk], in_=knT[h][:, 0:nk]),
                     reads=[T_knT], writes=[T_k[li]], dsem=d_k[li])
                S.op("sp", lambda e, li=li, h=h, nk=nk, nkb=nkb: e.dma_start(
                    out=vb_t[li][:, 0:nkb, 0:128], in_=vm[0:nk, h * 128:(h + 1) * 128].rearrange("(t p) c -> p t c", p=128)),
                    reads=[T_vm], writes=[T_v[li]], dsem=d_v[li])
                banks = [2, 4] if h % 2 == 0 else [5, 7]

                def s_mms(ps, T_ps, kb, li=li):
                    S.op("pe", lambda e: e.matmul(ps[:, :], lhsT=kb_t[li][:, 0, kb * 128:(kb + 1) * 128], rhs=qb_t[li][:, 0, :],
                                                  start=True, stop=False), reads=[T_k[li], T_q[li]], writes=[T_ps])
                    S.op("pe", lambda e: e.matmul(ps[:, :], lhsT=kr_t[:, kb * 128:(kb + 1) * 128], rhs=qr_t[li][:, :],
                                                  start=False, stop=True), reads=[T_kr, T_qr[li]], writes=[T_ps])

                def pv(kb, li=li):
                    return [vb_t[li][:, kb, 0:128], ones_b[:]]
                run_head(qb, nkb, s_mms, None, pv, banks, [T_v[li], T_ones])
                oi = cnt["o"] % 4
                cnt["o"] += 1
                S.op("dve", lambda e, banks=banks: e.reciprocal(out=rcp[:], in_=PB[banks[1]][:, :]), reads=[T_PB[banks[1]]], writes=[T_rcp])
                S.op("dve", lambda e, banks=banks, oi=oi: e.tensor_tensor(out=ost[oi][:], in0=PB[banks[0]][:, :], in1=rcp[:], op=ALU.mult),
                     reads=[T_PB[banks[0]], T_rcp], writes=[T_ost[oi]])
                S.op("sp", lambda e, oi=oi, h=h, qs=qs: e.dma_start(out=oT[16 + h][:, qs], in_=ost[oi][:]),
                     reads=[T_ost[oi]], pwrites=[T_oT], dsem=d_ost[oi])
        S.barrier()
        p4.close()

    phase4()
    p34.close()
    if stage <= 4:
        S.emit()
        es.close()
        return nc
    x1d = scratch("x1d", [SEQ, D], F32)
    hfT = scratch("hfT", [NDC, 128, SEQ], BF16)
    T_x1d = Tl(); T_hfT = Tl()
    wts = sb("wts", [128, NT, NE], F32)
    T_wts = [Tl() for _ in range(NT)]

    def phase5():
        p5 = ExitStack()
        GT = 2
        GW = GT * 128
        oTg = [sb("e_o0", [128, NDC, GW], BF16, p5)] * 2
        wo = [sb(f"e_w{i}", [128, NDC, 512], BF16, p5) for i in range(2)]
        gt = [sb(f"e_g{i}", [128, GT, 2, 512], BF16, p5) for i in range(2)]
        xin = [sb(f"e_x{i}", [128, GT, 512], F32, p5) for i in range(2)]
        x1g = sb("e_x1g", [128, GT, D], F32, p5)
        xsb = sb("e_xs", [128, GT, D], BF16, p5)
        hfs = sb("e_hf", [128, NDC, GW], BF16, p5)
        gta = sb("e_gta", [128, D], F32, p5)
        t0 = [sb(f"e_t0{i}", [128, 512], F32, p5) for i in range(2)]
        t1 = [sb(f"e_t1{i}", [128, 512], F32, p5) for i in range(2)]
        st5 = sb("e_st", [128, NT], F32, p5)
        rw = sb("e_rw", [128, NDC, NE], BF16, p5)
        rbias = sb("e_rb", [128, NE], F32, p5)
        T_oTg = [Tl()] * 2; T_wo = [Tl(), Tl()]; T_gt = [Tl(), Tl()]; T_xin = [Tl(), Tl()]
        T_x1g = [Tl() for _ in range(GT)]; T_xsb = [Tl() for _ in range(GT)]; T_hfs = Tl(); T_gta = Tl()
        T_t0 = [Tl(), Tl()]; T_t1 = [Tl(), Tl()]; T_st5 = [Tl() for _ in range(NT)]; T_rw = Tl(); T_rb = Tl()
        d_oTg = [S.dsem("eo0")] * 2; d_wo = [S.dsem("ew0"), S.dsem("ew1")]
        d_gt = [S.dsem("eg0"), S.dsem("eg1")]; d_xin = [S.dsem("ex0"), S.dsem("ex1")]
        d_x1g = S.dsem("ex1g"); d_hfs = S.dsem("ehfs")
        S.op("sp", lambda e: e.dma_start(out=gta[:], in_=modv[2].partition_broadcast(128)), reads=[T_modv], writes=[T_gta], dsem=T_gta)
        S.op("pool", lambda e: e.dma_start(out=rw[:], in_=W["router_w"].rearrange("(j p) e -> p j e", p=128)), writes=[T_rw], dsem=T_rw)
        S.op("sp", lambda e: e.dma_start(out=rbias[:], in_=W["router_bias"].partition_broadcast(128)), writes=[T_rb], dsem=T_rb)
        k_sc = sb("k_sc", [128, NE], F32, p5); k_sel = sb("k_sel", [128, NE], F32, p5); k_cur = sb("k_cur", [128, NE], F32, p5)
        k_eq = sb("k_eq", [128, NE], F32, p5); k_m1 = sb("k_m1", [128, 8], F32, p5); k_m2 = sb("k_m2", [128, 8], F32, p5)
        k_gs = sb("k_gs", [128, 8], F32, p5); k_gc = sb("k_gc", [128, 8], F32, p5); k_ge = sb("k_ge", [128, 8], F32, p5)
        k_mx = sb("k_mx", [128, 1], F32, p5)
        T_k = Tl()
        BIGK = 1.0e4
        nw = 0
        tpi = 0
        for g in range(NT // GT):
            gi = g % 2
            gsl = slice(g * GW, (g + 1) * GW)
            S.op("sp", lambda e, gi=gi, gsl=gsl: e.dma_start(out=oTg[gi][:], in_=oT[:, :, gsl].rearrange("c p t -> p c t")),
                 reads=[T_oT], writes=[T_oTg[gi]], dsem=d_oTg[gi])
            for dmb in range(8):
                wi = nw % 2
                nw += 1
                dsl = slice(dmb * 512, (dmb + 1) * 512)
                S.op("pool", lambda e, wi=wi, dsl=dsl: e.dma_start(out=wo[wi][:], in_=W["w_out"][:, dsl].rearrange("(j p) c -> p j c", p=128)),
                     writes=[T_wo[wi]], dsem=d_wo[wi])
                for br in range(2):
                    S.op("sp", lambda e, wi=wi, dmb=dmb, gsl=gsl, br=br: e.dma_start(
                        out=gt[wi][:, :, br, :], in_=gates[gsl, br * D + dmb * 512:br * D + (dmb + 1) * 512].rearrange("(t p) c -> p t c", p=128)),
                        reads=[T_gates], writes=[T_gt[wi]] if br == 0 else (), pwrites=() if br == 0 else [T_gt[wi]], dsem=d_gt[wi])
                S.op("sp", lambda e, wi=wi, dsl=dsl, gsl=gsl: e.dma_start(out=xin[wi][:], in_=x_in[gsl, dsl].rearrange("(t p) c -> p t c", p=128)),
                     writes=[T_xin[wi]], dsem=d_xin[wi])
                for tt in range(GT):
                    ti = tpi % 2
                    tpi += 1
                    bd, bm = (0, 1) if ti == 0 else (2, 3)
                    for fc in range(16):
                        S.op("pe", lambda e, bd=bd, gi=gi, wi=wi, fc=fc, tt=tt: e.matmul(
                            PB[bd][:, :], lhsT=oTg[gi][:, fc, tt * 128:(tt + 1) * 128], rhs=wo[wi][:, fc, :], start=(fc == 0), stop=(fc == 15)),
                            reads=[T_oTg[gi], T_wo[wi]], writes=[T_PB[bd]])
                    for fc in range(16, 32):
                        S.op("pe", lambda e, bm=bm, gi=gi, wi=wi, fc=fc, tt=tt: e.matmul(
                            PB[bm][:, :], lhsT=oTg[gi][:, fc, tt * 128:(tt + 1) * 128], rhs=wo[wi][:, fc, :], start=(fc == 16), stop=(fc == 31)),
                            reads=[T_oTg[gi], T_wo[wi]], writes=[T_PB[bm]])
                    S.op("dve", lambda e, bd=bd, wi=wi, tt=tt, ti=ti: e.tensor_tensor(out=t0[ti][:], in0=PB[bd][:, :], in1=gt[wi][:, tt, 0, :], op=ALU.mult),
                         reads=[T_PB[bd], T_gt[wi]], writes=[T_t0[ti]])
                    S.op("dve", lambda e, bm=bm, wi=wi, tt=tt, ti=ti: e.tensor_tensor(out=t1[ti][:], in0=PB[bm][:, :], in1=gt[wi][:, tt, 1, :], op=ALU.mult),
                         reads=[T_PB[bm], T_gt[wi]], writes=[T_t1[ti]])
                    S.op("pool", lambda e, ti=ti: e.tensor_tensor(out=t0[ti][:], in0=t0[ti][:], in1=t1[ti][:], op=ALU.add),
                         reads=[T_t0[ti], T_t1[ti]], writes=[T_t0[ti]])
                    S.op("pool", lambda e, ti=ti, dsl=dsl: e.tensor_tensor(out=t0[ti][:], in0=t0[ti][:], in1=gta[:, dsl], op=ALU.mult),
                         reads=[T_t0[ti], T_gta], writes=[T_t0[ti]])
                    S.op("pool", lambda e, ti=ti, wi=wi, tt=tt, dsl=dsl: e.tensor_tensor(out=x1g[:, tt, dsl], in0=t0[ti][:], in1=xin[wi][:, tt, :], op=ALU.add),
                         reads=[T_t0[ti], T_xin[wi]], writes=[T_x1g[tt]] if dmb == 0 else (), pwrites=() if dmb == 0 else [T_x1g[tt]])
            S.op("sp", lambda e, gsl=gsl: e.dma_start(out=x1d[gsl, :].rearrange("(t p) c -> p t c", p=128), in_=x1g[:]),
                 reads=T_x1g, pwrites=[T_x1d], dsem=d_x1g)
            for tt in range(GT):
                t = g * GT + tt
                S.op("act", lambda e, tt=tt, t=t: e.activation(out=xsb[:, tt, :], in_=x1g[:, tt, :], func=AF.Square, accum_out=st5[:, t:t + 1]),
                     reads=[T_x1g[tt]], writes=[T_xsb[tt], T_st5[t]])
                S.op("dve", lambda e, t=t: e.tensor_scalar(out=st5[:, t:t + 1], in0=st5[:, t:t + 1], scalar1=1.0 / D, scalar2=EPS, op0=ALU.mult, op1=ALU.add),
                     reads=[T_st5[t]], writes=[T_st5[t]])
                S.op("act", lambda e, t=t: e.activation(out=st5[:, t:t + 1], in_=st5[:, t:t + 1], func=AF.Sqrt), reads=[T_st5[t]], writes=[T_st5[t]])
                S.op("dve", lambda e, t=t: e.reciprocal(out=st5[:, t:t + 1], in_=st5[:, t:t + 1]), reads=[T_st5[t]], writes=[T_st5[t]])
                S.op("act", lambda e, tt=tt, t=t: e.activation(out=xsb[:, tt, :], in_=x1g[:, tt, :], func=AF.Copy, scale=st5[:, t:t + 1]),
                     reads=[T_x1g[tt], T_st5[t]], writes=[T_xsb[tt]])
            for dc in range(NDC):
                kk = 4 + (dc % 2)
                tpv = PB[kk][:].bitcast(BF16)
                for tt in range(GT):
                    S.op("pe", lambda e, tpv=tpv, tt=tt, dc=dc: e.transpose(tpv[:, tt * 128:(tt + 1) * 128], xsb[:, tt, dc * 128:(dc + 1) * 128], ident_b[:]),
                         reads=[T_xsb[tt], T_ident], writes=[T_PB[kk]] if tt == 0 else (), pwrites=() if tt == 0 else [T_PB[kk]])
                if dc % 2 == 0:
                    S.op("dve", lambda e, tpv=tpv, dc=dc: e.tensor_scalar(out=hfs[:, dc, :], in0=tpv[:, 0:GW], scalar1=modT[:, 224 + dc:225 + dc],
                                                                          scalar2=modT[:, 96 + dc:97 + dc], op0=ALU.mult, op1=ALU.add),
                         reads=[T_PB[kk], T_modT], writes=[T_hfs] if dc == 0 else (), pwrites=() if dc == 0 else [T_hfs])
                else:
                    S.op("act", lambda e, tpv=tpv, dc=dc: e.activation(out=hfs[:, dc, :], in_=tpv[:, 0:GW], func=AF.Identity,
                                                                       scale=modT[:, 224 + dc:225 + dc], bias=modT[:, 96 + dc:97 + dc]),
                         reads=[T_PB[kk], T_modT], pwrites=[T_hfs])
            S.op("sp", lambda e, gsl=gsl: e.dma_start(out=hfT[:, :, gsl].rearrange("c p t -> p c t"), in_=hfs[:]),
                 reads=[T_hfs], pwrites=[T_hfT], dsem=d_hfs)
            for tt in range(GT):
                t = g * GT + tt
                for dc in range(NDC):
                    S.op("pe", lambda e, dc=dc, tt=tt: e.matmul(PB[6][:, 0:NE], lhsT=hfs[:, dc, tt * 128:(tt + 1) * 128], rhs=rw[:, dc, :],
                                                                start=(dc == 0), stop=(dc == NDC - 1)), reads=[T_hfs, T_rw], writes=[T_PB[6]])
                S.op("act", lambda e: e.activation(out=k_sc[:], in_=PB[6][:, 0:NE], func=AF.Sigmoid), reads=[T_PB[6]], writes=[T_k])
                K = dict(reads=[T_k], writes=[T_k])
                v3 = lambda a: a[:].rearrange("p (g c) -> p g c", c=8)
                b3 = lambda a: a[:].unsqueeze(2).to_broadcast([128, 8, 8])
                S.op("dve", lambda e: e.tensor_tensor(out=k_sel[:], in0=k_sc[:], in1=rbias[:], op=ALU.add), reads=[T_k, T_rb], writes=[T_k])
                S.op("dve", lambda e: e.reduce_max(out=k_m1[:], in_=v3(k_sel), axis=AX.X), **K)
                S.op("dve", lambda e: e.tensor_tensor(out=v3(k_eq), in0=v3(k_sel), in1=b3(k_m1), op=ALU.is_equal), **K)
                S.op("dve", lambda e: e.scalar_tensor_tensor(out=k_cur[:], in0=k_eq[:], scalar=-BIGK, in1=k_sel[:], op0=ALU.mult, op1=ALU.add), **K)
                S.op("dve", lambda e: e.reduce_max(out=k_m2[:], in_=v3(k_cur), axis=AX.X), **K)
                S.op("dve", lambda e: e.tensor_tensor(out=k_gs[:], in0=k_m1[:], in1=k_m2[:], op=ALU.add), **K)
                S.op("dve", lambda e: e.tensor_copy(out=k_gc[:], in_=k_gs[:]), **K)
                for _ in range(3):
                    S.op("dve", lambda e: e.reduce_max(out=k_mx[:], in_=k_gc[:], axis=AX.X), **K)
                    S.op("dve", lambda e: e.tensor_scalar(out=k_ge[:], in0=k_gc[:], scalar1=k_mx[:, 0:1], scalar2=None, op0=ALU.is_equal), **K)
                    S.op("dve", lambda e: e.scalar_tensor_tensor(out=k_gc[:], in0=k_ge[:], scalar=-BIGK, in1=k_gc[:], op0=ALU.mult, op1=ALU.add), **K)
                S.op("dve", lambda e: e.reduce_max(out=k_mx[:], in_=k_gc[:], axis=AX.X), **K)
                S.op("dve", lambda e: e.tensor_scalar(out=k_ge[:], in0=k_gs[:], scalar1=k_mx[:, 0:1], scalar2=None, op0=ALU.is_ge), **K)
                S.op("dve", lambda e: e.tensor_scalar(out=k_ge[:], in0=k_ge[:], scalar1=-1.0, scalar2=BIGK, op0=ALU.add, op1=ALU.mult), **K)
                S.op("dve", lambda e: e.tensor_tensor(out=v3(k_sel), in0=v3(k_sel), in1=b3(k_ge), op=ALU.add), **K)
                S.op("dve", lambda e: e.tensor_copy(out=k_cur[:], in_=k_sel[:]), **K)
                for _ in range(5):
                    S.op("dve", lambda e: e.reduce_max(out=k_mx[:], in_=k_cur[:], axis=AX.X), **K)
                    S.op("dve", lambda e: e.tensor_scalar(out=k_eq[:], in0=k_cur[:], scalar1=k_mx[:, 0:1], scalar2=None, op0=ALU.is_equal), **K)
                    S.op("dve", lambda e: e.scalar_tensor_tensor(out=k_cur[:], in0=k_eq[:], scalar=-BIGK, in1=k_cur[:], op0=ALU.mult, op1=ALU.add), **K)
                S.op("dve", lambda e: e.reduce_max(out=k_mx[:], in_=k_cur[:], axis=AX.X), **K)
                S.op("dve", lambda e: e.tensor_scalar(out=k_eq[:], in0=k_sel[:], scalar1=k_mx[:, 0:1], scalar2=None, op0=ALU.is_ge), **K)
                S.op("dve", lambda e: e.tensor_tensor(out=k_sc[:], in0=k_sc[:], in1=k_eq[:], op=ALU.mult), **K)
                S.op("dve", lambda e: e.reduce_sum(out=k_mx[:], in_=k_sc[:], axis=AX.X), **K)
                S.op("dve", lambda e: e.reciprocal(out=k_mx[:], in_=k_mx[:]), **K)
                S.op("dve", lambda e, t=t: e.tensor_scalar(out=wts[:, t, :], in0=k_sc[:], scalar1=k_mx[:, 0:1], scalar2=2.5, op0=ALU.mult, op1=ALU.mult),
                     reads=[T_k], writes=[T_wts[t]])
        S.barrier()
        p5.close()

    phase5()
    if stage <= 5:
        if "wts_dbg" in debug_out:
            wd = scratch("wts_dbg", [128, NT, NE], F32)
            S.op("sp", lambda e: e.dma_start(out=wd, in_=wts[:]), reads=T_wts, dsem=Tl())
            S.barrier()
        S.emit()
        es.close()
        return nc
    accd = scratch("accd", [SEQ, D], F32)
    T_acc = [[Tl(), Tl()] for _ in range(NT)]

    def phase6():
        p6 = ExitStack()
        hfb = [sb(f"m_h{i}", [128, NDC, 512], BF16, p6) for i in range(2)]
        w1h = [sb(f"m_w1{i}", [128, NDC, 256], BF16, p6) for i in range(2)]
        w3h = [sb(f"m_w3{i}", [128, NDC, 256], BF16, p6) for i in range(2)]
        w2h = [sb(f"m_w2{i}", [128, 4, 2048], BF16, p6) for i in range(2)]
        aT = sb("m_a", [128, 4, SEQ], BF16, p6)
        sil = [sb(f"m_s{i}", [128, 512], F32, p6) for i in range(2)]
        yst = [sb(f"m_y{i}", [128, 2048], F32, p6) for i in range(2)]
        T_hfb = [Tl(), Tl()]; T_w1h = [Tl(), Tl()]; T_w3h = [Tl(), Tl()]; T_w2h = [Tl(), Tl()]
        T_aT = [[Tl() for _ in range(4)] for _ in range(4)]
        T_sil = [Tl(), Tl()]; T_yst = [Tl(), Tl()]
        d_hfb = [S.dsem("mh0"), S.dsem("mh1")]
        d_w1h = [S.dsem("mw10"), S.dsem("mw11")]; d_w3h = [S.dsem("mw30"), S.dsem("mw31")]; d_w2h = [S.dsem("mw20"), S.dsem("mw21")]
        d_yst = [S.dsem("my0"), S.dsem("my1")]
        NEX = NE + 1
        c = {"s1": 0, "sl": 0, "s2": 0, "y": 0, "ev": 0}
        if "sbuf_report" in debug_out:
            print("phase6 sbuf remaining", nc.sbuf_bytes_remaining)

        def wsrc(ex_):
            if ex_ < NE:
                return W["exp_w1"][ex_], W["exp_w3"][ex_], W["exp_w2"][ex_]
            return W["shared_w1"], W["shared_w3"], W["shared_w2"]

        def load_w13(ex_, hh):
            s1_, s3_, _ = wsrc(ex_)
            cs = slice(hh * 256, (hh + 1) * 256)
            S.op("pool", lambda e: e.dma_start(out=w1h[hh][:], in_=s1_[:, cs].rearrange("(j p) f -> p j f", p=128)), writes=[T_w1h[hh]], dsem=d_w1h[hh])
            S.op("pool", lambda e: e.dma_start(out=w3h[hh][:], in_=s3_[:, cs].rearrange("(j p) f -> p j f", p=128)), writes=[T_w3h[hh]], dsem=d_w3h[hh])

        def load_w2(ex_, hh):
            _, _, s2_ = wsrc(ex_)
            for j in range(4):
                S.op("pool", lambda e, j=j: e.dma_start(out=w2h[hh][:, j, :], in_=s2_[j * 128:(j + 1) * 128, hh * 2048:(hh + 1) * 2048], max_dma_last_dim=8192),
                     writes=[T_w2h[hh]] if j == 0 else (), pwrites=() if j == 0 else [T_w2h[hh]], dsem=d_w2h[hh])

        def load_hfb(k):
            tb = k % 4
            hi = k % 2
            S.op("sp", lambda e: e.dma_start(out=hfb[hi][:], in_=hfT[:, :, tb * 512:(tb + 1) * 512].rearrange("c p t -> p c t")),
                 reads=[T_hfT], writes=[T_hfb[hi]], dsem=d_hfb[hi])

        def s1_groups(k):
            tb = k % 4
            hi = k % 2
            out = []
            for fc in range(4):
                hh, co = fc // 2, (fc % 2) * 128
                k1 = c["s1"] % 2
                c["s1"] += 1
                b1, b3 = (0, 1) if k1 == 0 else (2, 3)

                def g1(fc=fc, hh=hh, co=co, b1=b1):
                    for dc in range(NDC):
                        S.op("pe", lambda e, dc=dc: e.matmul(PB[b1][:, :], lhsT=w1h[hh][:, dc, co:co + 128], rhs=hfb[hi][:, dc, :],
                                                             start=(dc == 0), stop=(dc == NDC - 1)),
                             reads=[T_w1h[hh], T_hfb[hi]], writes=[T_PB[b1]])

                def g3(fc=fc, hh=hh, co=co, b1=b1, b3=b3):
                    for dc in range(NDC):
                        S.op("pe", lambda e, dc=dc: e.matmul(PB[b3][:, :], lhsT=w3h[hh][:, dc, co:co + 128], rhs=hfb[hi][:, dc, :],
                                                             start=(dc == 0), stop=(dc == NDC - 1)),
                             reads=[T_w3h[hh], T_hfb[hi]], writes=[T_PB[b3]])
                    si = c["sl"] % 2
                    c["sl"] += 1
                    S.op("act", lambda e: e.activation(out=sil[si][:], in_=PB[b1][:, :], func=AF.Silu), reads=[T_PB[b1]], writes=[T_sil[si]])
                    S.op("dve", lambda e: e.tensor_tensor(out=aT[:, fc, tb * 512:(tb + 1) * 512], in0=sil[si][:], in1=PB[b3][:, :], op=ALU.mult),
                         reads=[T_sil[si], T_PB[b3]], writes=[T_aT[tb][fc]])
                out += [g1, g3]
            return out

        def s2_fills(k):
            ex, tb = k // 4, k % 4
            out = []
            for half in range(2):
                for tt in range(4):
                    def f(half=half, tt=tt):
                        t = tb * 4 + tt
                        yi = c["y"] % 2
                        c["y"] += 1
                        for q4 in range(4):
                            bk = 4 + (c["s2"] % 4)
                            c["s2"] += 1
                            for fc in range(4):
                                S.op("pe", lambda e, bk=bk, fc=fc, q4=q4: e.matmul(
                                    PB[bk][:, :], lhsT=aT[:, fc, tb * 512 + tt * 128:tb * 512 + (tt + 1) * 128], rhs=w2h[half][:, fc, q4 * 512:(q4 + 1) * 512],
                                    start=(fc == 0), stop=(fc == 3)), reads=[T_aT[tb][fc], T_w2h[half]], writes=[T_PB[bk]])
                            wr = [T_yst[yi]] if q4 == 0 else []
                            pw = [] if q4 == 0 else [T_yst[yi]]
                            use_act = (c["ev"] % 2 == 0)
                            c["ev"] += 1
                            osl = slice(q4 * 512, (q4 + 1) * 512)
                            if ex < NE:
                                if use_act:
                                    S.op("act", lambda e, bk=bk, osl=osl: e.activation(out=yst[yi][:, osl], in_=PB[bk][:, :], func=AF.Copy, scale=wts[:, t, ex:ex + 1]),
                                         reads=[T_PB[bk], T_wts[t]], writes=wr, pwrites=pw)
                                else:
                                    S.op("dve", lambda e, bk=bk, osl=osl: e.tensor_scalar(out=yst[yi][:, osl], in0=PB[bk][:, :], scalar1=wts[:, t, ex:ex + 1], scalar2=None, op0=ALU.mult),
                                         reads=[T_PB[bk], T_wts[t]], writes=wr, pwrites=pw)
                            else:
                                if use_act:
                                    S.op("act", lambda e, bk=bk, osl=osl: e.copy(out=yst[yi][:, osl], in_=PB[bk][:, :]), reads=[T_PB[bk]], writes=wr, pwrites=pw)
                                else:
                                    S.op("dve", lambda e, bk=bk, osl=osl: e.tensor_copy(out=yst[yi][:, osl], in_=PB[bk][:, :]), reads=[T_PB[bk]], writes=wr, pwrites=pw)
                        dst = accd[t * 128:(t + 1) * 128, half * 2048:(half + 1) * 2048]
                        if ex == 0:
                            S.op("pool", lambda e: e.dma_start(out=dst, in_=yst[yi][:]), reads=[T_yst[yi]], writes=[T_acc[t][half]], dsem=d_yst[yi])
                        else:
                            S.op("pool", lambda e: e.dma_start(out=dst, in_=yst[yi][:], accum_op=ALU.add), reads=[T_yst[yi]], writes=[T_acc[t][half]], dsem=d_yst[yi])
                    out.append(f)
            return out

        nslots = NEX * 4
        load_w13(0, 0); load_w13(0, 1); load_w2(0, 0); load_w2(0, 1)
        load_hfb(0)
        prev = None
        for k in range(nslots):
            ex, tb = k // 4, k % 4
            if k + 1 < nslots:
                load_hfb(k + 1)
            g = s1_groups(k)
            f2 = s2_fills(prev) if prev is not None else []
            for i in range(8):
                g[i]()
                if tb == 3 and ex + 1 < NEX and i == 3:
                    load_w13(ex + 1, 0)
                if f2:
                    f2[i]()
                    pex, ptb = prev // 4, prev % 4
                    if ptb == 3 and pex + 1 < NEX and i == 3:
                        load_w2(pex + 1, 0)
                    if ptb == 3 and pex + 1 < NEX and i == 7:
                        load_w2(pex + 1, 1)
            if tb == 3 and ex + 1 < NEX:
                load_w13(ex + 1, 1)
            prev = k
        for f in s2_fills(prev):
            f()
        S.barrier()
        p6.close()

    phase6()
    if stage <= 6:
        S.emit()
        es.close()
        return nc

    def phase7():
        p7 = ExitStack()
        gtf = sb("f_gtf", [128, D], F32, p7)
        fnb = sb("f_fn", [128, D], F32, p7)
        xa = [sb(f"f_x{i}", [128, D], F32, p7) for i in range(2)]
        ac = [sb(f"f_a{i}", [128, D], F32, p7) for i in range(2)]
        jk = sb("f_jk", [128, D], BF16, p7)
        st7 = sb("f_st", [128, NT], F32, p7)
        T_gtf = Tl(); T_fnb = Tl(); T_xa = [Tl(), Tl()]; T_ac = [Tl(), Tl()]; T_jk = Tl(); T_st7 = [Tl() for _ in range(NT)]
        d_xa = [S.dsem("fx0"), S.dsem("fx1")]; d_ac = [S.dsem("fa0"), S.dsem("fa1")]
        T_out = Tl()
        S.op("sp", lambda e: e.dma_start(out=gtf[:], in_=modv[5].partition_broadcast(128)), reads=[T_modv], writes=[T_gtf], dsem=T_gtf)
        S.op("sp", lambda e: e.dma_start(out=fnb[:], in_=W["final_norm"].partition_broadcast(128)), writes=[T_fnb], dsem=T_fnb)
        for t in range(NT):
            i = t % 2
            rows = slice(t * 128, (t + 1) * 128)
            S.op("sp", lambda e, i=i, rows=rows: e.dma_start(out=xa[i][:], in_=x1d[rows, :]), reads=[T_x1d], writes=[T_xa[i]], dsem=d_xa[i])
            S.op("sp", lambda e, i=i, rows=rows: e.dma_start(out=ac[i][:], in_=accd[rows, :]), reads=T_acc[t], writes=[T_ac[i]], dsem=d_ac[i])
            S.op("dve", lambda e, i=i: e.tensor_tensor(out=ac[i][:], in0=ac[i][:], in1=gtf[:], op=ALU.mult), reads=[T_ac[i], T_gtf], writes=[T_ac[i]])
            S.op("pool", lambda e, i=i: e.tensor_tensor(out=xa[i][:], in0=xa[i][:], in1=ac[i][:], op=ALU.add), reads=[T_xa[i], T_ac[i]], writes=[T_xa[i]])
            S.op("act", lambda e, i=i, t=t: e.activation(out=jk[:], in_=xa[i][:], func=AF.Square, accum_out=st7[:, t:t + 1]),
                 reads=[T_xa[i]], writes=[T_jk, T_st7[t]])
            S.op("dve", lambda e, t=t: e.tensor_scalar(out=st7[:, t:t + 1], in0=st7[:, t:t + 1], scalar1=1.0 / D, scalar2=EPS, op0=ALU.mult, op1=ALU.add),
                 reads=[T_st7[t]], writes=[T_st7[t]])
            S.op("act", lambda e, t=t: e.activation(out=st7[:, t:t + 1], in_=st7[:, t:t + 1], func=AF.Sqrt), reads=[T_st7[t]], writes=[T_st7[t]])
            S.op("dve", lambda e, t=t: e.reciprocal(out=st7[:, t:t + 1], in_=st7[:, t:t + 1]), reads=[T_st7[t]], writes=[T_st7[t]])
            S.op("act", lambda e, i=i, t=t: e.activation(out=ac[i][:], in_=xa[i][:], func=AF.Copy, scale=st7[:, t:t + 1]),
                 reads=[T_xa[i], T_st7[t]], writes=[T_ac[i]])
            S.op("dve", lambda e, i=i: e.tensor_tensor(out=ac[i][:], in0=ac[i][:], in1=fnb[:], op=ALU.mult), reads=[T_ac[i], T_fnb], writes=[T_ac[i]])
            S.op("sp", lambda e, i=i, rows=rows: e.dma_start(out=out_d[rows, :], in_=ac[i][:]), reads=[T_ac[i]], pwrites=[T_out], dsem=d_ac[i])
        S.barrier()
        p7.close()

    phase7()
    S.emit()
    es.close()
    return nc


RMAT = np.zeros((128, 64), np.float32)
for _m in range(32):
    RMAT[_m + 32, _m] = -1.0
    RMAT[_m, _m + 32] = 1.0

IMPLEMENTED_STAGE = 99
STAGE_WEIGHTS = ["w_ada", "b_ada", "norm_attn", "norm_ffn", "w_in"]


def make_in_maps(inputs, names=None):
    names = WEIGHT_NAMES if names is None else names
    maps = []
    ws = {n: np.ascontiguousarray(np.asarray(inputs[n])[0] if n != "final_norm" else np.asarray(inputs[n]), dtype=np.float32)
          for n in names}
    x = np.asarray(inputs["x"], dtype=np.float32)
    c = np.asarray(inputs["c"], dtype=np.float32)
    pos = np.asarray(inputs["positions"], dtype=np.int32)
    ident = np.eye(128, dtype=np.float32)
    for b in range(8):
        m = dict(ws)
        m["x"] = np.ascontiguousarray(x[b])
        m["c"] = np.ascontiguousarray(c[b])
        m["pos"] = np.ascontiguousarray(pos[b])
        m["ident"] = ident
        m["rmat"] = RMAT
        maps.append(m)
    return maps


def kernel(**inputs):
    nc = build(stage=IMPLEMENTED_STAGE)
    names = stage_weights(IMPLEMENTED_STAGE)
    res = run_bass_kernel_spmd(nc, make_in_maps(inputs, names), core_ids=list(range(8)))
    return np.stack([np.asarray(r["out"]).reshape(SEQ, D) for r in res.results], axis=0).astype(np.float32)
```

```python
import numpy as np
import ml_dtypes
from contextlib import ExitStack
import concourse.bass as bass
import concourse.mybir as mybir
from concourse.bass_utils import run_bass_kernel_spmd

F32 = mybir.dt.float32
BF16 = mybir.dt.bfloat16
I32 = mybir.dt.int32
AF = mybir.ActivationFunctionType
ALU = mybir.AluOpType
AX = mybir.AxisListType

ENG_NAMES = ("pe", "act", "dve", "pool", "sp")
EPOCH_MAX = 30000


class Tl:
    __slots__ = ("name", "w", "r", "fw", "ds")

    def __init__(self, name=""):
        self.name = name
        self.w = {}
        self.r = {}
        self.fw = {}
        self.ds = None


class DSem:
    __slots__ = ("name", "count", "sems", "nops")

    def __init__(self, name):
        self.name = name
        self.nops = 0


class Op:
    __slots__ = ("eng", "fn", "waits", "idx", "needed", "dsem", "didx", "ticket")


class Sched:
    def __init__(self, nc, same_eng_wait=True):
        self.nc = nc
        self.ops = {e: [] for e in ENG_NAMES}
        self.waited = {e: {} for e in ENG_NAMES}
        self.same_eng_wait = same_eng_wait
        self.dsems = []
        self.last = {}
        self.free_ds = []
        self.tile_ds = []

    def dsem(self, name):
        d = DSem(name)
        self.dsems.append(d)
        return d

    def op(self, eng, fn, reads=(), writes=(), pwrites=(), dsem=None):
        if isinstance(dsem, Tl):
            t = dsem
            if t.ds is None:
                t.ds = self.free_ds.pop() if self.free_ds else self.dsem(f"t{len(self.dsems)}")
                self.tile_ds.append(t)
            dsem = t.ds
        o = Op()
        o.eng = eng
        o.fn = fn
        o.needed = False
        o.dsem = dsem
        o.idx = len(self.ops[eng])
        deps = {}

        def add(d):
            for k, ent in d.items():
                cur = deps.get(k)
                if cur is None or cur[0] < ent[0]:
                    deps[k] = ent

        for t in reads:
            add(t.w)
        for t in writes:
            add(t.w)
            add(t.r)
        for t in pwrites:
            add(t.r)
            add(t.fw)
        waits = []
        wd = self.waited[eng]
        for k, (order, dop) in deps.items():
            if k[0] == "e" and k[1] == eng:
                if eng == "pe" or not self.same_eng_wait:
                    continue
            if wd.get(k, -1) >= order:
                continue
            wd[k] = order
            dop.needed = True
            waits.append(dop)
        o.waits = waits
        if dsem is not None:
            o.didx = dsem.nops
            dsem.nops += 1
            key = ("d", id(dsem))
            order = o.didx
        else:
            key = ("e", eng)
            order = o.idx
        ent = (order, o)
        self.last[key] = ent
        for t in writes:
            t.w = {key: ent}
            t.fw = {key: ent}
            t.r = {}
        for t in pwrites:
            t.w[key] = ent
        for t in reads:
            t.r[key] = ent
        self.ops[eng].append(o)
        return o

    def barrier(self):
        t = Tl("bar")
        t.w = dict(self.last)
        for e in ENG_NAMES:
            self.op(e, None, reads=[t])
        for tt in self.tile_ds:
            self.free_ds.append(tt.ds)
            tt.ds = None
        self.tile_ds = []

    def emit(self):
        nc = self.nc
        with ExitStack() as es:
            for e in ENG_NAMES:
                n = 0
                epoch = 0
                sems = [es.enter_context(nc.semaphore(f"c_{e}_0"))]
                for o in self.ops[e]:
                    if o.dsem is None and o.needed:
                        if o.fn is None:
                            o.fn = lambda eng: eng.nop()
                        if n >= EPOCH_MAX:
                            epoch += 1
                            n = 0
                            sems.append(es.enter_context(nc.semaphore(f"c_{e}_{epoch}")))
                        n += 1
                        o.ticket = (sems[epoch], n)
            per = {}
            for e in ENG_NAMES:
                for o in self.ops[e]:
                    if o.dsem is not None:
                        per.setdefault(id(o.dsem), []).append(o)
            for d in self.dsems:
                lst = sorted(per.get(id(d), []), key=lambda o: o.didx)
                if not lst:
                    continue
                d.sems = [es.enter_context(nc.semaphore(f"d_{d.name}_0"))]
                cnt = 0
                ep = 0
                for o in lst:
                    if cnt + 16 > EPOCH_MAX:
                        ep += 1
                        cnt = 0
                        d.sems.append(es.enter_context(nc.semaphore(f"d_{d.name}_{ep}")))
                    cnt += 16
                    o.ticket = (d.sems[ep], cnt)
            engs = {"pe": "tensor", "act": "scalar", "dve": "vector", "pool": "gpsimd", "sp": "sync"}
            with nc.Block() as block:
                def mk(e):
                    ops = self.ops[e]

                    def body(engine):
                        for o in ops:
                            for dop in o.waits:
                                s, v = dop.ticket
                                engine.wait_ge(s, v)
                            if o.fn is None:
                                continue
                            ins = o.fn(engine)
                            if o.dsem is not None:
                                ins.then_inc(o.ticket[0], 16)
                            elif o.needed:
                                ins.then_inc(o.ticket[0], 1)
                    return body
                for e in ENG_NAMES:
                    getattr(block, engs[e])(mk(e))
        return nc


D = 4096
SEQ = 2048
NT = SEQ // 128
NDC = D // 128
IN_COLS = 15680
OFF_Q, OFF_K, OFF_V, OFF_CQ, OFF_CKV, OFF_KPE, OFF_G = 0, 2048, 4096, 6144, 6912, 7424, 7488
EPS = 1e-6
LAM_INIT = 0.8 - 0.6 * 1.0
NE = 64
CAP = 1024

WEIGHT_NAMES = ["w_ada", "b_ada", "norm_attn", "w_in", "diff_lambda", "diff_subln", "mla_q_norm",
                "mla_w_uq", "mla_kv_norm", "mla_w_ukv", "w_out", "norm_ffn", "router_w", "router_bias",
                "exp_w1", "exp_w3", "exp_w2", "shared_w1", "shared_w3", "shared_w2", "final_norm"]
WEIGHT_SHAPES = {
    "w_ada": [D, 6 * D], "b_ada": [6 * D], "norm_attn": [D], "w_in": [D, IN_COLS], "diff_lambda": [4, 128],
    "diff_subln": [256], "mla_q_norm": [768], "mla_w_uq": [768, 3072], "mla_kv_norm": [512],
    "mla_w_ukv": [512, 4096], "w_out": [D, D], "norm_ffn": [D], "router_w": [D, NE], "router_bias": [NE],
    "exp_w1": [NE, D, 512], "exp_w3": [NE, D, 512], "exp_w2": [NE, 512, D], "shared_w1": [D, 512],
    "shared_w3": [D, 512], "shared_w2": [512, D], "final_norm": [D],
}


def stage_weights(stage):
    base = ["w_ada", "b_ada", "norm_attn", "norm_ffn", "w_in"]
    if stage <= 2:
        return base
    base = base + ["diff_lambda", "diff_subln", "mla_q_norm", "mla_w_uq", "mla_kv_norm", "mla_w_ukv"]
    if stage <= 4:
        return base
    base = base + ["w_out", "router_w", "router_bias"]
    if stage <= 5:
        return base
    return WEIGHT_NAMES


def build(stage=99, debug_out=()):
    nc = bass.Bass("TRN2", target_bir_lowering=False)
    S = Sched(nc)
    es = ExitStack()
    W = {}
    wnames = stage_weights(stage)
    for n in wnames:
        W[n] = nc.dram_tensor(n, WEIGHT_SHAPES[n], F32, kind="ExternalInput").ap()
    x_in = nc.dram_tensor("x", [SEQ, D], F32, kind="ExternalInput").ap()
    c_in = nc.dram_tensor("c", [D], F32, kind="ExternalInput").ap()
    pos_in = nc.dram_tensor("pos", [SEQ], I32, kind="ExternalInput").ap()
    ident_in = nc.dram_tensor("ident", [128, 128], F32, kind="ExternalInput").ap()
    rmat_in = nc.dram_tensor("rmat", [128, 64], F32, kind="ExternalInput").ap()
    out_d = nc.dram_tensor("out", [SEQ, D], F32, kind="ExternalOutput").ap()

    def scratch(name, shape, dt):
        kind = "ExternalOutput" if name in debug_out else "Internal"
        return nc.dram_tensor(name, shape, dt, kind=kind).ap()

    def sb(name, shape, dt, stack=es):
        return stack.enter_context(nc.sbuf_tensor(name, shape, dt))

    def ps(name, shape, dt, stack=es):
        return stack.enter_context(nc.psum_tensor(name, shape, dt))

    modv = scratch("modv", [8, D], F32)
    qdT = scratch("qdT", [16, 128, SEQ], BF16)
    kdT = scratch("kdT", [16, 128, SEQ], BF16)
    vd = scratch("vd", [SEQ, 2048], BF16)
    cqT = scratch("cqT", [6, 128, SEQ], F32)
    ckvT = scratch("ckvT", [4, 128, SEQ], F32)
    kpeT = scratch("kpeT", [64, SEQ], F32)
    kpeswT = scratch("kpeswT", [64, SEQ], F32)
    gates = scratch("gates", [SEQ, 2 * D], BF16)
    T_modv = Tl(); T_qdT = Tl(); T_kdT = Tl(); T_vd = Tl(); T_cqT = Tl(); T_ckvT = Tl(); T_kpeT = Tl(); T_gates = Tl()

    ident_f = sb("ident_f", [128, 128], F32)
    ident_b = sb("ident_b", [128, 128], BF16)
    modT = sb("modT", [128, 256], F32)
    T_ident = Tl(); T_modT = Tl()
    d_misc = S.dsem("misc")
    S.op("sp", lambda e: e.dma_start(out=ident_f[:], in_=ident_in), writes=[T_ident], dsem=T_ident)
    S.op("dve", lambda e: e.tensor_copy(out=ident_b[:], in_=ident_f[:]), reads=[T_ident], writes=[T_ident])

    PB = [ps(f"pb{i}", [128, 512], F32) for i in range(8)]
    T_PB = [Tl(f"pb{i}") for i in range(8)]

    with ExitStack() as p0:
        cT = sb("cT", [128, 32], F32, p0)
        bT = sb("bT", [128, 192], F32, p0)
        nT = sb("nT", [128, 64], F32, p0)
        wa = [sb(f"wa{i}", [128, 32, 256], F32, p0) for i in range(2)]
        T_cT = Tl(); T_bT = Tl(); T_nT = Tl(); T_wa = [Tl(), Tl()]
        d_wa = [S.dsem("wa0"), S.dsem("wa1")]

        def small_T(eng, dst, src_vec, n, tl):
            S.op(eng, lambda e: e.dma_start(out=dst, in_=src_vec.rearrange("(j p) -> p j", p=128),
                                            allow_slow_non_contiguous=True), writes=[tl], dsem=tl)
        small_T("sp", cT[:], c_in, 32, T_cT)
        S.op("sp", lambda e: e.dma_start(out=bT[:], in_=W["b_ada"].rearrange("(j p) -> p j", p=128),
                                         allow_slow_non_contiguous=True), writes=[T_bT], dsem=T_bT)
        S.op("sp", lambda e: e.dma_start(out=nT[:, 0:32], in_=W["norm_attn"].rearrange("(j p) -> p j", p=128),
                                         allow_slow_non_contiguous=True), pwrites=[T_nT], dsem=T_nT)
        S.op("sp", lambda e: e.dma_start(out=nT[:, 32:64], in_=W["norm_ffn"].rearrange("(j p) -> p j", p=128),
                                         allow_slow_non_contiguous=True), pwrites=[T_nT], dsem=T_nT)
        S.op("act", lambda e: e.activation(out=cT[:], in_=cT[:], func=AF.Silu), reads=[T_cT], writes=[T_cT])
        NB0 = 96
        pm = PB[0]

        def load_wa(eb):
            i = eb % 2
            S.op("sp", lambda e: e.dma_start(out=wa[i][:], in_=W["w_ada"][:, eb * 256:(eb + 1) * 256]
                                             .rearrange("(j p) c -> p j c", p=128)),
                 writes=[T_wa[i]], dsem=d_wa[i])
        load_wa(0)
        for eb in range(NB0):
            if eb + 1 < NB0:
                load_wa(eb + 1)
            i = eb % 2
            for half in range(2):
                col = eb * 2 + half
                for j in range(32):
                    S.op("pe", lambda e, i=i, j=j, half=half, col=col: e.matmul(
                        pm[:, col:col + 1], lhsT=wa[i][:, j, half * 128:(half + 1) * 128], rhs=cT[:, j:j + 1],
                        start=(j == 0), stop=(j == 31)),
                        reads=[T_wa[i], T_cT], writes=[T_PB[0]])
        S.op("dve", lambda e: e.tensor_tensor(out=modT[:, 0:192], in0=pm[:, 0:192], in1=bT[:], op=ALU.add),
             reads=[T_PB[0], T_bT], writes=[T_modT])
        S.op("dve", lambda e: e.scalar_tensor_tensor(out=modT[:, 192:224], in0=modT[:, 32:64], scalar=1.0, in1=nT[:, 0:32],
                                                     op0=ALU.add, op1=ALU.mult), reads=[T_modT, T_nT], writes=[T_modT])
        S.op("dve", lambda e: e.scalar_tensor_tensor(out=modT[:, 224:256], in0=modT[:, 128:160], scalar=1.0, in1=nT[:, 32:64],
                                                     op0=ALU.add, op1=ALU.mult), reads=[T_modT, T_nT], writes=[T_modT])
        S.op("sp", lambda e: e.dma_start(out=modv.rearrange("i (j p) -> p i j", p=128), in_=modT[:].rearrange("p (i j) -> p i j", j=32),
                                         allow_slow_non_contiguous=True), reads=[T_modT], writes=[T_modv], dsem=T_modv)
        S.barrier()
    if stage <= 0:
        S.emit()
        es.close()
        return nc

    hstack = ExitStack()
    hT = sb("hT", [128, NDC, SEQ], BF16, hstack)
    T_hT = [Tl(f"hT{g}") for g in range(4)]
    with ExitStack() as p1:
        xt = [sb(f"xt{i}", [128, D], F32, p1) for i in range(2)]
        junk = sb("junk", [128, D], BF16, p1)
        xs = sb("xs", [128, 4, D], BF16, p1)
        st = sb("st", [128, NT], F32, p1)
        T_xt = [Tl(), Tl()]; T_junk = Tl(); T_xs = [Tl() for _ in range(4)]; T_st = [Tl() for _ in range(NT)]
        d_xt = [S.dsem("xt0"), S.dsem("xt1")]
        tpb = [PB[1], PB[2]]
        T_tp = [T_PB[1], T_PB[2]]
        ev = 0
        for g in range(4):
            for tt in range(4):
                t = 4 * g + tt
                i = t % 2
                S.op("sp", lambda e, i=i, t=t: e.dma_start(out=xt[i][:], in_=x_in[t * 128:(t + 1) * 128, :]),
                     writes=[T_xt[i]], dsem=d_xt[i])
                S.op("act", lambda e, i=i, t=t: e.activation(out=junk[:], in_=xt[i][:], func=AF.Square, accum_out=st[:, t:t + 1]),
                     reads=[T_xt[i]], writes=[T_junk, T_st[t]])
                S.op("dve", lambda e, t=t: e.tensor_scalar(out=st[:, t:t + 1], in0=st[:, t:t + 1], scalar1=1.0 / D, scalar2=EPS,
                                                           op0=ALU.mult, op1=ALU.add), reads=[T_st[t]], writes=[T_st[t]])
                S.op("act", lambda e, t=t: e.activation(out=st[:, t:t + 1], in_=st[:, t:t + 1], func=AF.Sqrt),
                     reads=[T_st[t]], writes=[T_st[t]])
                S.op("dve", lambda e, t=t: e.reciprocal(out=st[:, t:t + 1], in_=st[:, t:t + 1]), reads=[T_st[t]], writes=[T_st[t]])
                S.op("act", lambda e, i=i, t=t, tt=tt: e.activation(out=xs[:, tt, :], in_=xt[i][:], func=AF.Copy, scale=st[:, t:t + 1]),
                     reads=[T_xt[i], T_st[t]], writes=[T_xs[tt]])
            for dc in range(NDC):
                k = dc % 2
                tpv = tpb[k][:].bitcast(BF16)
                for tt in range(4):
                    S.op("pe", lambda e, tpv=tpv, tt=tt, dc=dc: e.transpose(tpv[:, tt * 128:(tt + 1) * 128],
                                                                           xs[:, tt, dc * 128:(dc + 1) * 128], ident_b[:]),
                         reads=[T_xs[tt], T_ident], writes=[T_tp[k]] if tt == 0 else (), pwrites=() if tt == 0 else [T_tp[k]])
                if ev % 2 == 0:
                    S.op("dve", lambda e, tpv=tpv, dc=dc, g=g: e.tensor_scalar(
                        out=hT[:, dc, g * 512:(g + 1) * 512], in0=tpv[:, 0:512], scalar1=modT[:, 192 + dc:193 + dc],
                        scalar2=modT[:, dc:dc + 1], op0=ALU.mult, op1=ALU.add),
                        reads=[T_tp[k], T_modT], pwrites=[T_hT[g]])
                else:
                    S.op("act", lambda e, tpv=tpv, dc=dc, g=g: e.activation(
                        out=hT[:, dc, g * 512:(g + 1) * 512], in_=tpv[:, 0:512], func=AF.Identity,
                        scale=modT[:, 192 + dc:193 + dc], bias=modT[:, dc:dc + 1]),
                        reads=[T_tp[k], T_modT], pwrites=[T_hT[g]])
                ev += 1
        S.barrier()
    if stage <= 1:
        if "hT_dbg" in debug_out:
            hdbg = scratch("hT_dbg", [128, NDC, SEQ], BF16)
            S.op("sp", lambda e: e.dma_start(out=hdbg, in_=hT[:]), reads=T_hT, dsem=Tl())
            S.barrier()
        S.emit()
        es.close()
        return nc

    with ExitStack() as p2:
        CB = 256
        wb = [sb(f"wb{i}", [128, NDC, CB], BF16, p2) for i in range(2)]
        T_wb = [Tl(), Tl()]
        d_wb = [S.dsem("wb0"), S.dsem("wb1")]
        fstage_b = [sb(f"fsb{i}", [128, SEQ], BF16, p2) for i in range(2)]
        fstage_f = [sb(f"fsf{i}", [128, SEQ], F32, p2) for i in range(2)]
        tstage = [sb(f"tst{i}", [128, NT, CB], BF16, p2) for i in range(2)]
        T_fsb = [Tl(), Tl()]; T_fsf = [Tl(), Tl()]; T_tst = [Tl(), Tl()]
        d_fsb = [S.dsem("fsb0"), S.dsem("fsb1")]
        d_fsf = [S.dsem("fsf0"), S.dsem("fsf1")]
        d_tst = [S.dsem("tst0"), S.dsem("tst1")]
        blocks = []
        for c0 in range(0, 2048, CB):
            blocks.append((OFF_Q + c0, CB, "fb", (qdT, T_qdT, c0 // 128)))
        for c0 in range(0, 2048, CB):
            blocks.append((OFF_K + c0, CB, "fb", (kdT, T_kdT, c0 // 128)))
        for c0 in range(0, 768, CB):
            blocks.append((OFF_CQ + c0, CB, "ff", (cqT, T_cqT, c0 // 128)))
        for c0 in range(0, 512, CB):
            blocks.append((OFF_CKV + c0, CB, "ff", (ckvT, T_ckvT, c0 // 128)))
        blocks.append((OFF_KPE, 64, "kpe", None))
        blocks.append((OFF_KPE, 64, "kpesw", None))
        for c0 in range(0, 2048, CB):
            blocks.append((OFF_V + c0, CB, "tv", c0))
        for c0 in range(0, 2 * D, CB):
            blocks.append((OFF_G + c0, CB, "tg", c0))

        def load_wb(bi):
            col0, ncols, kind, info = blocks[bi]
            i = bi % 2
            S.op("pool", lambda e: e.dma_start(out=wb[i][:, :, 0:ncols], in_=W["w_in"][:, col0:col0 + ncols]
                                               .rearrange("(j p) c -> p j c", p=128)),
                 writes=[T_wb[i]], dsem=d_wb[i])
        load_wb(0)
        pbi = 0
        nfb = 0; nff = 0; ntst = 0
        evc = 0
        for bi in range(len(blocks)):
            if bi + 1 < len(blocks):
                load_wb(bi + 1)
            col0, ncols, kind, info = blocks[bi]
            i = bi % 2
            if kind == "kpesw":
                S.op("dve", lambda e, i=i: e.tensor_scalar(out=wb[i][:, :, 64:96], in0=wb[i][:, :, 32:64], scalar1=-1.0, scalar2=None, op0=ALU.mult),
                     reads=[T_wb[i]], writes=[T_wb[i]])
                S.op("dve", lambda e, i=i: e.tensor_copy(out=wb[i][:, :, 96:128], in_=wb[i][:, :, 0:32]),
                     reads=[T_wb[i]], writes=[T_wb[i]])
            if kind in ("fb", "ff", "kpe", "kpesw"):
                nch = 1 if kind in ("kpe", "kpesw") else ncols // 128
                woff = 64 if kind == "kpesw" else 0
                for ch in range(nch):
                    m = 64 if kind in ("kpe", "kpesw") else 128
                    if kind == "fb":
                        stg, T_stg, d_stg = fstage_b[nfb % 2], T_fsb[nfb % 2], d_fsb[nfb % 2]
                        nfb += 1
                    else:
                        stg, T_stg, d_stg = fstage_f[nff % 2], T_fsf[nff % 2], d_fsf[nff % 2]
                        nff += 1
                    for tb in range(4):
                        bank = pbi % 8
                        pbi += 1
                        for dc in range(NDC):
                            S.op("pe", lambda e, bank=bank, i=i, dc=dc, ch=ch, tb=tb, m=m, woff=woff: e.matmul(
                                PB[bank][0:m, :], lhsT=wb[i][:, dc, woff + ch * 128:woff + ch * 128 + m], rhs=hT[:, dc, tb * 512:(tb + 1) * 512],
                                start=(dc == 0), stop=(dc == NDC - 1)),
                                reads=[T_wb[i], T_hT[tb]], writes=[T_PB[bank]])
                        eng = "dve" if evc % 2 == 0 else "act"
                        evc += 1
                        if eng == "dve":
                            S.op("dve", lambda e, bank=bank, stg=stg, tb=tb, m=m: e.tensor_copy(
                                out=stg[0:m, tb * 512:(tb + 1) * 512], in_=PB[bank][0:m, :]),
                                reads=[T_PB[bank]], writes=[T_stg] if tb == 0 else (), pwrites=() if tb == 0 else [T_stg])
                        else:
                            S.op("act", lambda e, bank=bank, stg=stg, tb=tb, m=m: e.copy(
                                out=stg[0:m, tb * 512:(tb + 1) * 512], in_=PB[bank][0:m, :]),
                                reads=[T_PB[bank]], writes=[T_stg] if tb == 0 else (), pwrites=() if tb == 0 else [T_stg])
                    if kind == "kpe":
                        dst, T_dst = kpeT, T_kpeT
                    elif kind == "kpesw":
                        dst, T_dst = kpeswT, T_kpeT
                    else:
                        dst, T_dst = info[0][info[2] + ch], info[1]
                    S.op("sp", lambda e, dst=dst, stg=stg, m=m: e.dma_start(out=dst, in_=stg[0:m, :]),
                         reads=[T_stg], pwrites=[T_dst], dsem=d_stg)
            else:
                stg, T_stg, d_stg = tstage[ntst % 2], T_tst[ntst % 2], d_tst[ntst % 2]
                ntst += 1
                for t in range(NT):
                    bank = pbi % 8
                    pbi += 1
                    for dc in range(NDC):
                        S.op("pe", lambda e, bank=bank, i=i, dc=dc, t=t: e.matmul(
                            PB[bank][:, 0:CB], lhsT=hT[:, dc, t * 128:(t + 1) * 128], rhs=wb[i][:, dc, :],
                            start=(dc == 0), stop=(dc == NDC - 1)),
                            reads=[T_wb[i], T_hT[t // 4]], writes=[T_PB[bank]])
                    if kind == "tg":
                        S.op("act", lambda e, bank=bank, stg=stg, t=t: e.activation(out=stg[:, t, :], in_=PB[bank][:, 0:CB], func=AF.Sigmoid),
                             reads=[T_PB[bank]], writes=[T_stg] if t == 0 else (), pwrites=() if t == 0 else [T_stg])
                    else:
                        S.op("dve", lambda e, bank=bank, stg=stg, t=t: e.tensor_copy(out=stg[:, t, :], in_=PB[bank][:, 0:CB]),
                             reads=[T_PB[bank]], writes=[T_stg] if t == 0 else (), pwrites=() if t == 0 else [T_stg])
                if kind == "tv":
                    dst, T_dst = vd[:, info:info + CB], T_vd
                else:
                    dst, T_dst = gates[:, info:info + CB], T_gates
                S.op("sp", lambda e, dst=dst, stg=stg: e.dma_start(out=dst.rearrange("(t p) c -> p t c", p=128), in_=stg[:]),
                     reads=[T_stg], pwrites=[T_dst], dsem=d_stg)
        S.barrier()
    hstack.close()
    if stage <= 2:
        S.emit()
        es.close()
        return nc
    qnT = scratch("qnT", [16, 128, SEQ], BF16)
    qrT = scratch("qrT", [16, 64, SEQ], BF16)
    knT = scratch("knT", [16, 128, SEQ], BF16)
    krT = scratch("krT", [64, SEQ], BF16)
    vm = scratch("vm", [SEQ, 2048], BF16)
    T_qnT = Tl(); T_qrT = Tl(); T_knT = Tl(); T_krT = Tl(); T_vm = Tl()
    ones_b = sb("ones_b", [128, 128], BF16)
    p34 = ExitStack()
    rm_f = sb("rm_f", [128, 64], F32, p34)
    rm_b = sb("rm_b", [128, 64], BF16, p34)
    cos2 = sb("cos2", [64, SEQ], F32, p34)
    sin2 = sb("sin2", [64, SEQ], F32, p34)
    T_ones = Tl(); T_rm = Tl(); T_cs = Tl()
    S.op("dve", lambda e: e.memset(ones_b[:], 1.0), writes=[T_ones])
    S.op("sp", lambda e: e.dma_start(out=rm_f[:], in_=rmat_in), writes=[T_rm], dsem=T_rm)
    S.op("dve", lambda e: e.tensor_copy(out=rm_b[:], in_=rm_f[:]), reads=[T_rm], writes=[T_rm])
    PI = float(np.pi)
    with ExitStack() as p3a:
        posi = sb("posi", [64, SEQ], I32, p3a)
        ang = sb("ang", [64, SEQ], F32, p3a)
        tmpa = sb("tmpa", [64, SEQ], F32, p3a)
        pidx_i = sb("pidx_i", [64, 1], I32, p3a)
        invf = sb("invf", [64, 1], F32, p3a)
        frac = sb("frac", [64, SEQ], F32, p3a)
        T_posi = Tl(); T_ang = Tl(); T_tmpa = Tl(); T_invf = Tl(); T_frac = Tl()
        S.op("sp", lambda e: e.dma_start(out=posi[:], in_=pos_in.partition_broadcast(64)), writes=[T_posi], dsem=T_posi)
        S.op("pool", lambda e: e.iota(pidx_i[0:32, :], pattern=[[0, 1]], base=0, channel_multiplier=1), pwrites=[T_invf])
        S.op("pool", lambda e: e.iota(pidx_i[32:64, :], pattern=[[0, 1]], base=0, channel_multiplier=1), pwrites=[T_invf])
        S.op("dve", lambda e: e.tensor_copy(out=invf[:], in_=pidx_i[:]), reads=[T_invf], writes=[T_invf])
        S.op("act", lambda e: e.activation(out=invf[:], in_=invf[:], func=AF.Exp, scale=-float(np.log(10000.0)) / 32.0),
             reads=[T_invf], writes=[T_invf])
        S.op("dve", lambda e: e.tensor_scalar(out=invf[:], in0=invf[:], scalar1=1.0 / (2.0 * PI), scalar2=None, op0=ALU.mult),
             reads=[T_invf], writes=[T_invf])
        S.op("dve", lambda e: e.tensor_copy(out=ang[:], in_=posi[:]), reads=[T_posi], writes=[T_ang])
        S.op("dve", lambda e: e.tensor_scalar(out=ang[:], in0=ang[:], scalar1=invf[:, 0:1], scalar2=None, op0=ALU.mult),
             reads=[T_ang, T_invf], writes=[T_ang])
        ki = posi
        for (dst, shift) in ((sin2, 0.0), (cos2, 0.25)):
            S.op("dve", lambda e, shift=shift: e.tensor_scalar(out=tmpa[:], in0=ang[:], scalar1=shift, scalar2=None, op0=ALU.add),
                 reads=[T_ang], writes=[T_tmpa])
            S.op("dve", lambda e: e.tensor_copy(out=ki[:], in_=tmpa[:]), reads=[T_tmpa], writes=[T_posi])
            S.op("dve", lambda e: e.tensor_copy(out=frac[:], in_=ki[:]), reads=[T_posi], writes=[T_frac])
            S.op("dve", lambda e: e.tensor_tensor(out=tmpa[:], in0=tmpa[:], in1=frac[:], op=ALU.subtract),
                 reads=[T_tmpa, T_frac], writes=[T_tmpa])
            S.op("dve", lambda e: e.tensor_single_scalar(out=frac[:], in_=tmpa[:], scalar=0.5, op=ALU.is_gt),
                 reads=[T_tmpa], writes=[T_frac])
            S.op("dve", lambda e: e.tensor_tensor(out=tmpa[:], in0=tmpa[:], in1=frac[:], op=ALU.subtract),
                 reads=[T_tmpa, T_frac], writes=[T_tmpa])
            S.op("dve", lambda e: e.tensor_single_scalar(out=frac[:], in_=tmpa[:], scalar=-0.5, op=ALU.is_lt),
                 reads=[T_tmpa], writes=[T_frac])
            S.op("dve", lambda e: e.tensor_tensor(out=tmpa[:], in0=tmpa[:], in1=frac[:], op=ALU.add),
                 reads=[T_tmpa, T_frac], writes=[T_tmpa])
            S.op("act", lambda e, dst=dst: e.activation(out=dst[:], in_=tmpa[:], func=AF.Sin, scale=2.0 * PI), reads=[T_tmpa], pwrites=[T_cs])
        S.barrier()
    if "cs_dbg" in debug_out:
        csd = scratch("cs_dbg", [2, 64, SEQ], F32)
        S.op("sp", lambda e: e.dma_start(out=csd[0], in_=cos2[:]), reads=[T_cs], dsem=Tl())
        S.op("sp", lambda e: e.dma_start(out=csd[1], in_=sin2[:]), reads=[T_cs], dsem=Tl())
        S.barrier()
        S.emit(); es.close(); return nc

    def rms_bcast(src, nch, n_feat, scr, rbc, T_src, T_scr, T_rbc):
        for ch in range(nch):
            S.op("act", lambda e, ch=ch: e.activation(out=scr[:, ch, :], in_=src[:, ch, :], func=AF.Square),
                 reads=[T_src], writes=[T_scr[ch]])
        for tb in range(4):
            for ch in range(nch):
                S.op("pe", lambda e, ch=ch, tb=tb: e.matmul(PB[0][:, :], lhsT=ones_b[:], rhs=scr[:, ch, tb * 512:(tb + 1) * 512],
                                                            start=(ch == 0), stop=(ch == nch - 1)),
                     reads=[T_ones, T_scr[ch]], writes=[T_PB[0]])
            S.op("dve", lambda e, tb=tb: e.tensor_scalar(out=rbc[:, tb * 512:(tb + 1) * 512], in0=PB[0][:, :], scalar1=1.0 / n_feat,
                                                         scalar2=EPS, op0=ALU.mult, op1=ALU.add),
                 reads=[T_PB[0]], pwrites=[T_rbc])
        S.op("act", lambda e: e.activation(out=rbc[:], in_=rbc[:], func=AF.Sqrt), reads=[T_rbc], writes=[T_rbc])
        S.op("dve", lambda e: e.reciprocal(out=rbc[:], in_=rbc[:]), reads=[T_rbc], writes=[T_rbc])

    def rope_block(t_ps, T_t, sw_ps, T_sw, tb, dst_stage, T_dst_stage, first, tmpu, T_tmpu):
        sl = slice(tb * 512, (tb + 1) * 512)
        S.op("dve", lambda e: e.tensor_tensor(out=tmpu[:], in0=t_ps, in1=cos2[:, sl], op=ALU.mult),
             reads=[T_t, T_cs], writes=[T_tmpu])
        S.op("dve", lambda e: e.tensor_tensor(out=tmpv[:], in0=sw_ps, in1=sin2[:, sl], op=ALU.mult),
             reads=[T_sw, T_cs], writes=[T_tmpv])
        S.op("dve", lambda e: e.tensor_tensor(out=dst_stage[0:64, sl], in0=tmpu[:], in1=tmpv[:], op=ALU.add),
             reads=[T_tmpu, T_tmpv], writes=[T_dst_stage] if first else (), pwrites=() if first else [T_dst_stage])

    tmpv = sb("tmpv", [64, 512], F32, p34)
    T_tmpv = Tl()

    with ExitStack() as p3q:
        cq = sb("cq", [128, 6, SEQ], F32, p3q)
        cqn = sb("cqn", [128, 6, SEQ], BF16, p3q)
        rq = sb("rq", [128, SEQ], F32, p3q)
        wuq = sb("wuq", [128, 6, 3072], BF16, p3q)
        gq = sb("gq", [128, 6], F32, p3q)
        stg = [sb(f"q3s{i}", [128, SEQ], BF16, p3q) for i in range(2)]
        wsw = sb("wsw", [128, 6, 16, 64], BF16, p3q)
        tmpu = sb("tmpu", [64, 512], F32, p3q)
        T_cq = Tl(); T_cqn = [Tl() for _ in range(6)]; T_rq = Tl(); T_wuq = Tl(); T_gq = Tl()
        T_stg = [Tl(), Tl()]; T_tb16 = Tl(); T_tmpu = Tl()
        d_stg = [S.dsem("q3s0"), S.dsem("q3s1")]
        T_wsw = Tl()
        S.op("sp", lambda e: e.dma_start(out=cq[:], in_=cqT.rearrange("c p t -> p c t")), reads=[T_cqT], writes=[T_cq], dsem=T_cq)
        S.op("sp", lambda e: e.dma_start(out=gq[:], in_=W["mla_q_norm"].rearrange("(j p) -> p j", p=128), allow_slow_non_contiguous=True),
             writes=[T_gq], dsem=T_gq)
        for ch in range(6):
            S.op("pool", lambda e, ch=ch: e.dma_start(out=wuq[:, ch, :], in_=W["mla_w_uq"][ch * 128:(ch + 1) * 128, :], max_dma_last_dim=4096),
                 pwrites=[T_wuq], dsem=T_wuq)
        wr = wuq[:].rearrange("p c (h d) -> p c h d", d=192)
        for ch in range(6):
            S.op("dve", lambda e, ch=ch: e.tensor_scalar(out=wsw[:, ch, :, 0:32], in0=wr[:, ch, :, 160:192], scalar1=-1.0, scalar2=None, op0=ALU.mult),
                 reads=[T_wuq], pwrites=[T_wsw])
            S.op("dve", lambda e, ch=ch: e.tensor_copy(out=wsw[:, ch, :, 32:64], in_=wr[:, ch, :, 128:160]),
                 reads=[T_wuq], pwrites=[T_wsw])
        rms_bcast(cq, 6, 768.0, cqn, rq, T_cq, T_cqn, T_rq)
        for ch in range(6):
            S.op("dve", lambda e, ch=ch: e.scalar_tensor_tensor(out=cqn[:, ch, :], in0=cq[:, ch, :], scalar=gq[:, ch:ch + 1], in1=rq[:],
                                                                op0=ALU.mult, op1=ALU.mult),
                 reads=[T_cq, T_gq, T_rq], writes=[T_cqn[ch]])
        if "cqn_dbg" in debug_out:
            cqn_d = scratch("cqn_dbg", [128, 6, SEQ], BF16)
            rq_d = scratch("rq_dbg", [128, SEQ], F32)
            S.op("sp", lambda e: e.dma_start(out=cqn_d, in_=cqn[:]), reads=T_cqn, dsem=Tl())
            S.op("sp", lambda e: e.dma_start(out=rq_d, in_=rq[:]), reads=[T_rq], dsem=Tl())
        ns = 0
        for h in range(0 if "q_norm_only" not in debug_out else 16, 16):
            for part in range(2 if "q_nope_only" not in debug_out else 1):
                m = 128 if part == 0 else 64
                c0 = h * 192 + (0 if part == 0 else 128)
                st_i = ns % 2
                ns += 1
                for tb in range(4):
                    bank = 1 + (tb % 2) + 2 * part
                    for ch in range(6):
                        S.op("pe", lambda e, bank=bank, ch=ch, tb=tb, m=m, c0=c0: e.matmul(
                            PB[bank][0:m, :], lhsT=wuq[:, ch, c0:c0 + m], rhs=cqn[:, ch, tb * 512:(tb + 1) * 512],
                            start=(ch == 0), stop=(ch == 5)), reads=[T_wuq, T_cqn[ch]], writes=[T_PB[bank]])
                    if part == 0:
                        S.op("act", lambda e, bank=bank, tb=tb, st_i=st_i: e.copy(out=stg[st_i][:, tb * 512:(tb + 1) * 512], in_=PB[bank][:, :]),
                             reads=[T_PB[bank]], writes=[T_stg[st_i]] if tb == 0 else (), pwrites=() if tb == 0 else [T_stg[st_i]])
                    elif "rope_plain" in debug_out:
                        S.op("act", lambda e, bank=bank, tb=tb, st_i=st_i: e.copy(out=stg[st_i][0:64, tb * 512:(tb + 1) * 512], in_=PB[bank][0:64, :]),
                             reads=[T_PB[bank]], writes=[T_stg[st_i]] if tb == 0 else (), pwrites=() if tb == 0 else [T_stg[st_i]])
                    else:
                        bsw = 5 + (tb % 2)
                        for ch in range(6):
                            S.op("pe", lambda e, bsw=bsw, ch=ch, tb=tb, h=h: e.matmul(
                                PB[bsw][0:64, :], lhsT=wsw[:, ch, h, :], rhs=cqn[:, ch, tb * 512:(tb + 1) * 512],
                                start=(ch == 0), stop=(ch == 5)), reads=[T_wsw, T_cqn[ch]], writes=[T_PB[bsw]])
                        rope_block(PB[bank][0:64, :], T_PB[bank], PB[bsw][0:64, :], T_PB[bsw], tb, stg[st_i], T_stg[st_i], tb == 0, tmpu, T_tmpu)
                dst, T_dst = (qnT[h], T_qnT) if part == 0 else (qrT[h], T_qrT)
                S.op("sp", lambda e, dst=dst, st_i=st_i, m=m: e.dma_start(out=dst, in_=stg[st_i][0:m, :]),
                     reads=[T_stg[st_i]], pwrites=[T_dst], dsem=d_stg[st_i])
        S.barrier()
    if stage <= 3 and "q_only" in debug_out:
        S.emit(); es.close(); return nc

    with ExitStack() as p3k:
        ckv = sb("ckv", [128, 4, SEQ], F32, p3k)
        ckvn = sb("ckvn", [128, 4, SEQ], BF16, p3k)
        rkv = sb("rkv", [128, SEQ], F32, p3k)
        wukv = sb("wukv", [128, 4, 4096], BF16, p3k)
        gkv = sb("gkv", [128, 4], F32, p3k)
        kpe = sb("kpe", [64, SEQ], F32, p3k)
        kpesw = sb("kpesw", [64, SEQ], F32, p3k)
        kstg = [sb(f"k3s{i}", [128, SEQ], BF16, p3k) for i in range(2)]
        vst = [sb(f"v3s{i}", [128, NT, 512], BF16, p3k) for i in range(2)]
        ktmpu = sb("ktmpuk", [64, 512], F32, p3k)
        T_ckv = Tl(); T_ckvn = [Tl() for _ in range(4)]; T_rkv = Tl(); T_wukv = Tl(); T_gkv = Tl(); T_kpe = Tl()
        T_kkstg = [Tl(), Tl()]; T_vst = [Tl(), Tl()]; T_tb16 = Tl(); T_kktmpu = Tl()
        d_kkstg = [S.dsem("k3s0"), S.dsem("k3s1")]
        d_vst = [S.dsem("v3s0"), S.dsem("v3s1")]
        S.op("sp", lambda e: e.dma_start(out=ckv[:], in_=ckvT.rearrange("c p t -> p c t")), reads=[T_ckvT], writes=[T_ckv], dsem=T_ckv)
        S.op("sp", lambda e: e.dma_start(out=kpe[:], in_=kpeT), reads=[T_kpeT], writes=[T_kpe], dsem=T_kpe)
        S.op("sp", lambda e: e.dma_start(out=kpesw[:], in_=kpeswT), reads=[T_kpeT], pwrites=[T_kpe], dsem=T_kpe)
        S.op("sp", lambda e: e.dma_start(out=gkv[:], in_=W["mla_kv_norm"].rearrange("(j p) -> p j", p=128), allow_slow_non_contiguous=True),
             writes=[T_gkv], dsem=T_gkv)
        for ch in range(4):
            S.op("pool", lambda e, ch=ch: e.dma_start(out=wukv[:, ch, :], in_=W["mla_w_ukv"][ch * 128:(ch + 1) * 128, :], max_dma_last_dim=4096),
                 pwrites=[T_wukv], dsem=T_wukv)
        rms_bcast(ckv, 4, 512.0, ckvn, rkv, T_ckv, T_ckvn, T_rkv)
        for ch in range(4):
            S.op("dve", lambda e, ch=ch: e.scalar_tensor_tensor(out=ckvn[:, ch, :], in0=ckv[:, ch, :], scalar=gkv[:, ch:ch + 1], in1=rkv[:],
                                                                op0=ALU.mult, op1=ALU.mult),
                 reads=[T_ckv, T_gkv, T_rkv], writes=[T_ckvn[ch]])
        for tb in range(4):
            sl = slice(tb * 512, (tb + 1) * 512)
            S.op("dve", lambda e, sl=sl: e.tensor_tensor(out=ktmpu[:], in0=kpe[:, sl], in1=cos2[:, sl], op=ALU.mult),
                 reads=[T_kpe, T_cs], writes=[T_kktmpu])
            S.op("dve", lambda e, sl=sl: e.tensor_tensor(out=tmpv[:], in0=kpesw[:, sl], in1=sin2[:, sl], op=ALU.mult),
                 reads=[T_kpe, T_cs], writes=[T_tmpv])
            S.op("dve", lambda e, sl=sl, tb=tb: e.tensor_tensor(out=kstg[0][0:64, sl], in0=ktmpu[:], in1=tmpv[:], op=ALU.add),
                 reads=[T_kktmpu, T_tmpv], writes=[T_kkstg[0]] if tb == 0 else (), pwrites=() if tb == 0 else [T_kkstg[0]])
        S.op("sp", lambda e: e.dma_start(out=krT, in_=kstg[0][0:64, :]), reads=[T_kkstg[0]], writes=[T_krT], dsem=d_kkstg[0])
        ns = 1
        for h in range(16):
            st_i = ns % 2
            ns += 1
            for tb in range(4):
                bank = 1 + (tb % 2)
                for ch in range(4):
                    S.op("pe", lambda e, bank=bank, ch=ch, tb=tb, h=h: e.matmul(
                        PB[bank][:, :], lhsT=wukv[:, ch, h * 256:h * 256 + 128], rhs=ckvn[:, ch, tb * 512:(tb + 1) * 512],
                        start=(ch == 0), stop=(ch == 3)), reads=[T_wukv, T_ckvn[ch]], writes=[T_PB[bank]])
                S.op("act", lambda e, bank=bank, tb=tb, st_i=st_i: e.copy(out=kstg[st_i][:, tb * 512:(tb + 1) * 512], in_=PB[bank][:, :]),
                     reads=[T_PB[bank]], writes=[T_kkstg[st_i]] if tb == 0 else (), pwrites=() if tb == 0 else [T_kkstg[st_i]])
            S.op("sp", lambda e, h=h, st_i=st_i: e.dma_start(out=knT[h], in_=kstg[st_i][:, :]),
                 reads=[T_kkstg[st_i]], pwrites=[T_knT], dsem=d_kkstg[st_i])
        wv = wukv[:].rearrange("p c (h two d) -> p c h two d", two=2, d=128)
        for g4 in range(4):
            vi = g4 % 2
            for t in range(NT):
                bank = 3 + (t % 2)
                for ch in range(4):
                    S.op("pe", lambda e, bank=bank, ch=ch, t=t, g4=g4: e.matmul(
                        PB[bank][:, :], lhsT=ckvn[:, ch, t * 128:(t + 1) * 128], rhs=wv[:, ch, 4 * g4:4 * g4 + 4, 1, :],
                        start=(ch == 0), stop=(ch == 3)), reads=[T_wukv, T_ckvn[ch]], writes=[T_PB[bank]])
                S.op("dve", lambda e, bank=bank, t=t, vi=vi: e.tensor_copy(out=vst[vi][:, t, :], in_=PB[bank][:, :]),
                     reads=[T_PB[bank]], writes=[T_vst[vi]] if t == 0 else (), pwrites=() if t == 0 else [T_vst[vi]])
            S.op("sp", lambda e, g4=g4, vi=vi: e.dma_start(out=vm[:, g4 * 512:(g4 + 1) * 512].rearrange("(t p) c -> p t c", p=128), in_=vst[vi][:]),
                 reads=[T_vst[vi]], pwrites=[T_vm], dsem=d_vst[vi])
        S.barrier()
    if stage <= 3:
        S.emit()
        es.close()
        return nc
    oT = scratch("oT", [32, 128, SEQ], BF16)
    T_oT = Tl()

    def phase4():
        p4 = ExitStack()
        BIG = 1.0e9
        SC_D = 128.0 ** -0.5
        SC_M = 192.0 ** -0.5
        lamv = sb("lamv", [128, 512], F32, p4)
        lam2 = sb("lam2", [128, 4], F32, p4)
        gsub = sb("gsub", [128, 2], F32, p4)
        maskB = sb("maskB", [128, 4, 512], F32, p4)
        mki = sb("mki", [128, 512], I32, p4)
        posk_i = sb("posk_i", [128, NT], I32, p4)
        posk = sb("posk", [128, NT], F32, p4)
        posq_i = sb("posq_i", [128, 512], I32, p4)
        posq = sb("posq", [128, 512], F32, p4)
        dist = sb("dist", [128, NT, 512], F32, p4)
        T_lam = Tl(); T_gsub = Tl(); T_maskB = Tl(); T_mki = Tl(); T_posk = Tl(); T_posq = Tl()
        T_dist = [Tl() for _ in range(NT)]
        S.op("sp", lambda e: e.dma_start(out=lamv[:], in_=W["diff_lambda"].rearrange("a b -> (a b)").partition_broadcast(128)),
             writes=[T_lam], dsem=T_lam)
        S.op("dve", lambda e: e.tensor_tensor(out=lamv[:, 0:128], in0=lamv[:, 0:128], in1=lamv[:, 128:256], op=ALU.mult), reads=[T_lam], writes=[T_lam])
        S.op("dve", lambda e: e.tensor_tensor(out=lamv[:, 256:384], in0=lamv[:, 256:384], in1=lamv[:, 384:512], op=ALU.mult), reads=[T_lam], writes=[T_lam])
        S.op("dve", lambda e: e.reduce_sum(out=lam2[:, 0:1], in_=lamv[:, 0:128], axis=AX.X), reads=[T_lam], writes=[T_lam])
        S.op("dve", lambda e: e.reduce_sum(out=lam2[:, 1:2], in_=lamv[:, 256:384], axis=AX.X), reads=[T_lam], writes=[T_lam])
        S.op("act", lambda e: e.activation(out=lam2[:, 0:2], in_=lam2[:, 0:2], func=AF.Exp), reads=[T_lam], writes=[T_lam])
        S.op("dve", lambda e: e.tensor_tensor(out=lam2[:, 2:3], in0=lam2[:, 0:1], in1=lam2[:, 1:2], op=ALU.subtract), reads=[T_lam], writes=[T_lam])
        S.op("dve", lambda e: e.tensor_scalar(out=lam2[:, 2:3], in0=lam2[:, 2:3], scalar1=LAM_INIT, scalar2=None, op0=ALU.add), reads=[T_lam], writes=[T_lam])
        S.op("dve", lambda e: e.tensor_scalar(out=lam2[:, 3:4], in0=lam2[:, 2:3], scalar1=-1.0, scalar2=None, op0=ALU.mult), reads=[T_lam], writes=[T_lam])
        S.op("sp", lambda e: e.dma_start(out=gsub[:], in_=W["diff_subln"].rearrange("(j p) -> p j", p=128), allow_slow_non_contiguous=True),
             writes=[T_gsub], dsem=T_gsub)
        S.op("dve", lambda e: e.tensor_scalar(out=gsub[:], in0=gsub[:], scalar1=1.0 - LAM_INIT, scalar2=None, op0=ALU.mult), reads=[T_gsub], writes=[T_gsub])
        for j in range(4):
            S.op("pool", lambda e, j=j: e.iota(mki[:], pattern=[[1, 512]], base=-128 * j, channel_multiplier=-1), writes=[T_mki])
            S.op("dve", lambda e, j=j: e.tensor_copy(out=maskB[:, j, :], in_=mki[:]), reads=[T_mki], pwrites=[T_maskB])
            S.op("dve", lambda e, j=j: e.tensor_single_scalar(out=maskB[:, j, :], in_=maskB[:, j, :], scalar=0.0, op=ALU.is_lt),
                 reads=[T_maskB], pwrites=[T_maskB])
            S.op("dve", lambda e, j=j: e.tensor_scalar(out=maskB[:, j, :], in0=maskB[:, j, :], scalar1=BIG, scalar2=None, op0=ALU.mult),
                 reads=[T_maskB], pwrites=[T_maskB])
        S.op("sp", lambda e: e.dma_start(out=posk_i[:], in_=pos_in.rearrange("(t p) -> p t", p=128), allow_slow_non_contiguous=True),
             writes=[T_posk], dsem=T_posk)
        S.op("dve", lambda e: e.tensor_copy(out=posk[:], in_=posk_i[:]), reads=[T_posk], writes=[T_posk])
        qb_t = [sb(f"a_q{i}", [128, 2, 512], BF16, p4) for i in range(2)]
        kb_t = [sb(f"a_k{i}", [128, 2, SEQ], BF16, p4) for i in range(2)]
        vb_t = [sb(f"a_v{i}", [128, NT, 256], BF16, p4) for i in range(2)]
        qr_t = [sb(f"a_qr{i}", [128, 512], BF16, p4) for i in range(2)]
        kr_t = sb("a_kr", [128, SEQ], BF16, p4)
        T_q = [Tl(), Tl()]; T_k = [Tl(), Tl()]; T_v = [Tl(), Tl()]; T_qr = [Tl(), Tl()]; T_kr = Tl()
        d_q = [S.dsem("aq0"), S.dsem("aq1")]; d_k = [S.dsem("ak0"), S.dsem("ak1")]; d_v = [S.dsem("av0"), S.dsem("av1")]
        d_qr = [S.dsem("aqr0"), S.dsem("aqr1")]
        NPB = 4
        tmp_t = [sb(f"a_tmp{i}", [128, 512], F32, p4) for i in range(NPB)]
        p_t = [sb(f"a_p{i}", [128, 512], BF16, p4) for i in range(NPB)]
        T_tmp = [Tl() for _ in range(NPB)]; T_p = [Tl() for _ in range(NPB)]
        rcp = sb("a_rcp", [128, 512], F32, p4)
        res = sb("a_res", [128, 2, 2, 512], F32, p4)
        av = sb("a_av", [128, 2, 512], F32, p4)
        asq = sb("a_sq", [128, 2, 512], BF16, p4)
        rsd = sb("a_rsd", [128, 512], F32, p4)
        ost = [sb(f"a_o{i}", [128, 512], BF16, p4) for i in range(4)]
        T_rcp = Tl(); T_res = [[Tl(), Tl()], [Tl(), Tl()]]; T_av = [Tl(), Tl()]; T_asq = [Tl(), Tl()]; T_rsd = Tl()
        T_ost = [Tl() for _ in range(4)]
        d_ost = [S.dsem(f"ao{i}") for i in range(4)]
        S.op("dve", lambda e: e.memset(kr_t[:], 0.0), writes=[T_kr])
        for i in range(2):
            S.op("dve", lambda e, i=i: e.memset(qr_t[i][:], 0.0), writes=[T_qr[i]])
        S.op("sp", lambda e: e.dma_start(out=kr_t[0:64, :], in_=krT), reads=[T_krT], pwrites=[T_kr], dsem=T_kr)
        cnt = {"s": 0, "p": 0, "o": 0, "ld": 0}

        def run_head(qb, nkb, s_mms, diff_slope, pv_lhsT, acc_banks, T_srcs, s_banks=(0, 1)):
            for kb in range(nkb):
                sb_i = s_banks[cnt["s"] % len(s_banks)]
                cnt["s"] += 1
                s_mms(PB[sb_i], T_PB[sb_i], kb)
                pi = cnt["p"] % NPB
                cnt["p"] += 1
                diag = kb >= 4 * qb
                if diff_slope is not None:
                    S.op("dve", lambda e, pi=pi, sb_i=sb_i, kb=kb: e.scalar_tensor_tensor(
                        out=tmp_t[pi][:], in0=dist[:, kb, :], scalar=-diff_slope / SC_D, in1=PB[sb_i][:, :], op0=ALU.mult, op1=ALU.add),
                        reads=[T_dist[kb], T_PB[sb_i]], writes=[T_tmp[pi]])
                    S.op("act", lambda e, pi=pi: e.activation(out=p_t[pi][:], in_=tmp_t[pi][:], func=AF.Exp, scale=SC_D),
                         reads=[T_tmp[pi]], writes=[T_p[pi]])
                elif diag:
                    j = kb - 4 * qb
                    S.op("dve", lambda e, pi=pi, sb_i=sb_i, j=j: e.scalar_tensor_tensor(
                        out=tmp_t[pi][:], in0=maskB[:, j, :], scalar=-1.0e-4, in1=PB[sb_i][:, :], op0=ALU.mult, op1=ALU.add),
                        reads=[T_maskB, T_PB[sb_i]], writes=[T_tmp[pi]])
                    S.op("act", lambda e, pi=pi: e.activation(out=p_t[pi][:], in_=tmp_t[pi][:], func=AF.Exp, scale=SC_M),
                         reads=[T_tmp[pi]], writes=[T_p[pi]])
                else:
                    S.op("act", lambda e, pi=pi, sb_i=sb_i: e.activation(out=p_t[pi][:], in_=PB[sb_i][:, :], func=AF.Exp, scale=SC_M),
                         reads=[T_PB[sb_i]], writes=[T_p[pi]])
                for ai, lh in enumerate(pv_lhsT(kb)):
                    bk = acc_banks[ai]
                    S.op("pe", lambda e, bk=bk, lh=lh, pi=pi, kb=kb: e.matmul(PB[bk][:, :], lhsT=lh, rhs=p_t[pi][:],
                                                                               start=(kb == 0), stop=(kb == nkb - 1)),
                         reads=[T_p[pi]] + T_srcs, writes=[T_PB[bk]])

        for qb in range(4):
            nkb = 4 * qb + 4
            S.op("sp", lambda e, qb=qb: e.dma_start(out=posq_i[:], in_=pos_in[qb * 512:(qb + 1) * 512].partition_broadcast(128)),
                 writes=[T_posq], dsem=T_posq)
            S.op("dve", lambda e: e.tensor_copy(out=posq[:], in_=posq_i[:]), reads=[T_posq], writes=[T_posq])
            for kb in range(nkb):
                S.op("dve", lambda e, kb=kb: e.tensor_scalar(out=dist[:, kb, :], in0=posq[:], scalar1=posk[:, kb:kb + 1], scalar2=None,
                                                             op0=ALU.subtract), reads=[T_posq, T_posk], writes=[T_dist[kb]])
                S.op("dve", lambda e, kb=kb: e.scalar_tensor_tensor(out=dist[:, kb, :], in0=dist[:, kb, :], scalar=-1.0, in1=dist[:, kb, :],
                                                                    op0=ALU.mult, op1=ALU.max), reads=[T_dist[kb]], writes=[T_dist[kb]])
                if kb >= 4 * qb:
                    S.op("dve", lambda e, kb=kb, qb=qb: e.tensor_tensor(out=dist[:, kb, :], in0=dist[:, kb, :], in1=maskB[:, kb - 4 * qb, :], op=ALU.add),
                         reads=[T_maskB], writes=[T_dist[kb]])
            qs = slice(qb * 512, (qb + 1) * 512)
            nk = nkb * 128
            for h in range(8):
                li = cnt["ld"] % 2
                cnt["ld"] += 1
                for mi in range(2):
                    S.op("sp", lambda e, li=li, mi=mi, h=h, qs=qs: e.dma_start(out=qb_t[li][:, mi, :], in_=qdT[2 * h + mi][:, qs]),
                         reads=[T_qdT], writes=[T_q[li]] if mi == 0 else (), pwrites=() if mi == 0 else [T_q[li]], dsem=d_q[li])
                    S.op("sp", lambda e, li=li, mi=mi, h=h, nk=nk: e.dma_start(out=kb_t[li][:, mi, 0:nk], in_=kdT[2 * h + mi][:, 0:nk]),
                         reads=[T_kdT], writes=[T_k[li]] if mi == 0 else (), pwrites=() if mi == 0 else [T_k[li]], dsem=d_k[li])
                S.op("sp", lambda e, li=li, h=h, nk=nk, nkb=nkb: e.dma_start(
                    out=vb_t[li][:, 0:nkb, :], in_=vd[0:nk, h * 256:(h + 1) * 256].rearrange("(t p) c -> p t c", p=128)),
                    reads=[T_vd], writes=[T_v[li]], dsem=d_v[li])
                for mi in range(2):
                    banks = [2, 3, 4] if mi == 0 else [5, 6, 7]

                    def s_mms(ps, T_ps, kb, li=li, mi=mi):
                        S.op("pe", lambda e: e.matmul(ps[:, :], lhsT=kb_t[li][:, mi, kb * 128:(kb + 1) * 128], rhs=qb_t[li][:, mi, :],
                                                      start=True, stop=True), reads=[T_k[li], T_q[li]], writes=[T_ps])

                    def pv(kb, li=li):
                        return [vb_t[li][:, kb, 0:128], vb_t[li][:, kb, 128:256], ones_b[:]]
                    run_head(qb, nkb, s_mms, 2.0 ** -(h + 1), pv, banks, [T_v[li], T_ones])
                    S.op("dve", lambda e, banks=banks: e.reciprocal(out=rcp[:], in_=PB[banks[2]][:, :]), reads=[T_PB[banks[2]]], writes=[T_rcp])
                    for c in range(2):
                        S.op("dve", lambda e, banks=banks, c=c, mi=mi: e.tensor_tensor(out=res[:, mi, c, :], in0=PB[banks[c]][:, :], in1=rcp[:], op=ALU.mult),
                             reads=[T_PB[banks[c]], T_rcp], writes=[T_res[mi][c]])
                for c in range(2):
                    S.op("dve", lambda e, c=c: e.scalar_tensor_tensor(out=av[:, c, :], in0=res[:, 1, c, :], scalar=lam2[:, 3:4], in1=res[:, 0, c, :],
                                                                      op0=ALU.mult, op1=ALU.add),
                         reads=[T_res[0][c], T_res[1][c], T_lam], writes=[T_av[c]])
                    S.op("act", lambda e, c=c: e.activation(out=asq[:, c, :], in_=av[:, c, :], func=AF.Square), reads=[T_av[c]], writes=[T_asq[c]])
                for c in range(2):
                    S.op("pe", lambda e, c=c: e.matmul(PB[0][:, :], lhsT=ones_b[:], rhs=asq[:, c, :], start=(c == 0), stop=(c == 1)),
                         reads=[T_asq[c], T_ones], writes=[T_PB[0]])
                cnt["s"] += 1 if cnt["s"] % 2 == 0 else 0
                S.op("dve", lambda e: e.tensor_scalar(out=rsd[:], in0=PB[0][:, :], scalar1=1.0 / 256.0, scalar2=EPS, op0=ALU.mult, op1=ALU.add),
                     reads=[T_PB[0]], writes=[T_rsd])
                S.op("act", lambda e: e.activation(out=rsd[:], in_=rsd[:], func=AF.Sqrt), reads=[T_rsd], writes=[T_rsd])
                S.op("dve", lambda e: e.reciprocal(out=rsd[:], in_=rsd[:]), reads=[T_rsd], writes=[T_rsd])
                for c in range(2):
                    oi = cnt["o"] % 4
                    cnt["o"] += 1
                    S.op("dve", lambda e, c=c, oi=oi: e.scalar_tensor_tensor(out=ost[oi][:], in0=av[:, c, :], scalar=gsub[:, c:c + 1], in1=rsd[:],
                                                                             op0=ALU.mult, op1=ALU.mult),
                         reads=[T_av[c], T_gsub, T_rsd], writes=[T_ost[oi]])
                    S.op("sp", lambda e, c=c, oi=oi, h=h, qs=qs: e.dma_start(out=oT[2 * h + c][:, qs], in_=ost[oi][:]),
                         reads=[T_ost[oi]], pwrites=[T_oT], dsem=d_ost[oi])
            for h in range(16):
                li = cnt["ld"] % 2
                cnt["ld"] += 1
                S.op("sp", lambda e, li=li, h=h, qs=qs: e.dma_start(out=qb_t[li][:, 0, :], in_=qnT[h][:, qs]),
                     reads=[T_qnT], writes=[T_q[li]], dsem=d_q[li])
                S.op("sp", lambda e, li=li, h=h, qs=qs: e.dma_start(out=qr_t[li][0:64, :], in_=qrT[h][:, qs]),
                     reads=[T_qrT], pwrites=[T_qr[li]], dsem=d_qr[li])
                S.op("sp", lambda e, li=li, h=h, nk=nk: e.dma_start(out=kb_t[li][:, 0, 0:nk], in_=knT[h][:, 0:nk]),
                     reads=[T_knT], writes=[T_k[li]], dsem=d_k[li])
                S.op("sp", lambda e, li=li, h=h, nk=nk, nkb=nkb: e.dma_start(
                    out=vb_t[li][:, 0:nkb, 0:128], in_=vm[0:nk, h * 128:(h + 1) * 128].rearrange("(t p) c -> p t c", p=128)),
                    reads=[T_vm], writes=[T_v[li]], dsem=d_v[li])
                banks = [2, 4] if h % 2 == 0 else [5, 7]

                def s_mms(ps, T_ps, kb, li=li):
                    S.op("pe", lambda e: e.matmul(ps[:, :], lhsT=kb_t[li][:, 0, kb * 128:(kb + 1) * 128], rhs=qb_t[li][:, 0, :],
                                                  start=True, stop=False), reads=[T_k[li], T_q[li]], writes=[T_ps])
                    S.op("pe", lambda e: e.matmul(ps[:, :], lhsT=kr_t[:, kb * 128:(kb + 1) * 128], rhs=qr_t[li][:, :],
                                                  start=False, stop=True), reads=[T_kr, T_qr[li]], writes=[T_ps])

                def pv(kb, li=li):
                    return [vb_t[li][:, kb, 0:128], ones_b[:]]
                run_head(qb, nkb, s_mms, None, pv, banks, [T_v[li], T_ones], s_banks=(0, 1, 3, 6))
                oi = cnt["o"] % 4
                cnt["o"] += 1
                S.op("dve", lambda e, banks=banks: e.reciprocal(out=rcp[:], in_=PB[banks[1]][:, :]), reads=[T_PB[banks[1]]], writes=[T_rcp])
                S.op("dve", lambda e, banks=banks, oi=oi: e.tensor_tensor(out=ost[oi][:], in0=PB[banks[0]][:, :], in1=rcp[:], op=ALU.mult),
                     reads=[T_PB[banks[0]], T_rcp], writes=[T_ost[oi]])
                S.op("sp", lambda e, oi=oi, h=h, qs=qs: e.dma_start(out=oT[16 + h][:, qs], in_=ost[oi][:]),
                     reads=[T_ost[oi]], pwrites=[T_oT], dsem=d_ost[oi])
        S.barrier()
        p4.close()

    phase4()
    p34.close()
    if stage <= 4:
        S.emit()
        es.close()
        return nc
    x1d = scratch("x1d", [SEQ, D], F32)
    wob = scratch("wob", [8, 128, NDC, 512], BF16)
    T_wob = [Tl() for _ in range(8)]
    hfT = scratch("hfT", [NDC, 128, SEQ], BF16)
    T_x1d = Tl(); T_hfT = Tl()
    wts = sb("wts", [128, NT, NE], F32)
    T_wts = [Tl() for _ in range(NT)]

    def phase5():
        p5 = ExitStack()
        GT = 2
        GW = GT * 128
        oTg = [sb("e_o0", [128, NDC, GW], BF16, p5)] * 2
        wo = [sb(f"e_w{i}", [128, NDC, 512], BF16, p5) for i in range(2)]
        gt = [sb(f"e_g{i}", [128, GT, 2, 512], BF16, p5) for i in range(2)]
        xin = [sb(f"e_x{i}", [128, GT, 512], F32, p5) for i in range(2)]
        x1g = sb("e_x1g", [128, GT, D], F32, p5)
        xsb = sb("e_xs", [128, GT, D], BF16, p5)
        hfs = sb("e_hf", [128, NDC, GW], BF16, p5)
        gta = sb("e_gta", [128, D], F32, p5)
        t0 = [sb(f"e_t0{i}", [128, 512], F32, p5) for i in range(2)]
        t1 = [sb(f"e_t1{i}", [128, 512], F32, p5) for i in range(2)]
        st5 = sb("e_st", [128, NT], F32, p5)
        rw = sb("e_rw", [128, NDC, NE], BF16, p5)
        rbias = sb("e_rb", [128, NE], F32, p5)
        T_oTg = [Tl()] * 2; T_wo = [Tl(), Tl()]; T_gt = [Tl(), Tl()]; T_xin = [Tl(), Tl()]
        T_x1g = [Tl() for _ in range(GT)]; T_xsb = [Tl() for _ in range(GT)]; T_hfs = Tl(); T_gta = Tl()
        T_t0 = [Tl(), Tl()]; T_t1 = [Tl(), Tl()]; T_st5 = [Tl() for _ in range(NT)]; T_rw = Tl(); T_rb = Tl()
        d_oTg = [S.dsem("eo0")] * 2; d_wo = [S.dsem("ew0"), S.dsem("ew1")]
        d_gt = [S.dsem("eg0"), S.dsem("eg1")]; d_xin = [S.dsem("ex0"), S.dsem("ex1")]
        d_x1g = S.dsem("ex1g"); d_hfs = S.dsem("ehfs")
        S.op("sp", lambda e: e.dma_start(out=gta[:], in_=modv[2].partition_broadcast(128)), reads=[T_modv], writes=[T_gta], dsem=T_gta)
        S.op("pool", lambda e: e.dma_start(out=rw[:], in_=W["router_w"].rearrange("(j p) e -> p j e", p=128)), writes=[T_rw], dsem=T_rw)
        S.op("sp", lambda e: e.dma_start(out=rbias[:], in_=W["router_bias"].partition_broadcast(128)), writes=[T_rb], dsem=T_rb)
        k_sc = sb("k_sc", [128, NE], F32, p5); k_sel = sb("k_sel", [128, NE], F32, p5); k_cur = sb("k_cur", [128, NE], F32, p5)
        k_eq = sb("k_eq", [128, NE], F32, p5); k_m1 = sb("k_m1", [128, 8], F32, p5); k_m2 = sb("k_m2", [128, 8], F32, p5)
        k_gs = sb("k_gs", [128, 8], F32, p5); k_gc = sb("k_gc", [128, 8], F32, p5); k_ge = sb("k_ge", [128, 8], F32, p5)
        k_mx = sb("k_mx", [128, 1], F32, p5)
        T_k = Tl()
        BIGK = 1.0e4
        nw = 0
        tpi = 0
        for g in range(NT // GT):
            gi = g % 2
            gsl = slice(g * GW, (g + 1) * GW)
            S.op("sp", lambda e, gi=gi, gsl=gsl: e.dma_start(out=oTg[gi][:], in_=oT[:, :, gsl].rearrange("c p t -> p c t")),
                 reads=[T_oT], writes=[T_oTg[gi]], dsem=d_oTg[gi])
            for dmb in range(8):
                wi = nw % 2
                nw += 1
                dsl = slice(dmb * 512, (dmb + 1) * 512)
                if g == 0:
                    S.op("pool", lambda e, wi=wi, dsl=dsl: e.dma_start(out=wo[wi][:], in_=W["w_out"][:, dsl].rearrange("(j p) c -> p j c", p=128)),
                         writes=[T_wo[wi]], dsem=d_wo[wi])
                    S.op("sp", lambda e, wi=wi, dmb=dmb: e.dma_start(out=wob[dmb], in_=wo[wi][:]),
                         reads=[T_wo[wi]], writes=[T_wob[dmb]], dsem=d_wo[wi])
                else:
                    S.op("sp", lambda e, wi=wi, dmb=dmb: e.dma_start(out=wo[wi][:], in_=wob[dmb]),
                         reads=[T_wob[dmb]], writes=[T_wo[wi]], dsem=d_wo[wi])
                for br in range(2):
                    S.op("sp", lambda e, wi=wi, dmb=dmb, gsl=gsl, br=br: e.dma_start(
                        out=gt[wi][:, :, br, :], in_=gates[gsl, br * D + dmb * 512:br * D + (dmb + 1) * 512].rearrange("(t p) c -> p t c", p=128)),
                        reads=[T_gates], writes=[T_gt[wi]] if br == 0 else (), pwrites=() if br == 0 else [T_gt[wi]], dsem=d_gt[wi])
                S.op("sp", lambda e, wi=wi, dsl=dsl, gsl=gsl: e.dma_start(out=xin[wi][:], in_=x_in[gsl, dsl].rearrange("(t p) c -> p t c", p=128)),
                     writes=[T_xin[wi]], dsem=d_xin[wi])
                for tt in range(GT):
                    ti = tpi % 2
                    tpi += 1
                    bd, bm = (0, 1) if ti == 0 else (2, 3)
                    for fc in range(16):
                        S.op("pe", lambda e, bd=bd, gi=gi, wi=wi, fc=fc, tt=tt: e.matmul(
                            PB[bd][:, :], lhsT=oTg[gi][:, fc, tt * 128:(tt + 1) * 128], rhs=wo[wi][:, fc, :], start=(fc == 0), stop=(fc == 15)),
                            reads=[T_oTg[gi], T_wo[wi]], writes=[T_PB[bd]])
                    for fc in range(16, 32):
                        S.op("pe", lambda e, bm=bm, gi=gi, wi=wi, fc=fc, tt=tt: e.matmul(
                            PB[bm][:, :], lhsT=oTg[gi][:, fc, tt * 128:(tt + 1) * 128], rhs=wo[wi][:, fc, :], start=(fc == 16), stop=(fc == 31)),
                            reads=[T_oTg[gi], T_wo[wi]], writes=[T_PB[bm]])
                    S.op("dve", lambda e, bd=bd, wi=wi, tt=tt, ti=ti: e.tensor_tensor(out=t0[ti][:], in0=PB[bd][:, :], in1=gt[wi][:, tt, 0, :], op=ALU.mult),
                         reads=[T_PB[bd], T_gt[wi]], writes=[T_t0[ti]])
                    S.op("dve", lambda e, bm=bm, wi=wi, tt=tt, ti=ti: e.tensor_tensor(out=t1[ti][:], in0=PB[bm][:, :], in1=gt[wi][:, tt, 1, :], op=ALU.mult),
                         reads=[T_PB[bm], T_gt[wi]], writes=[T_t1[ti]])
                    S.op("pool", lambda e, ti=ti: e.tensor_tensor(out=t0[ti][:], in0=t0[ti][:], in1=t1[ti][:], op=ALU.add),
                         reads=[T_t0[ti], T_t1[ti]], writes=[T_t0[ti]])
                    S.op("pool", lambda e, ti=ti, dsl=dsl: e.tensor_tensor(out=t0[ti][:], in0=t0[ti][:], in1=gta[:, dsl], op=ALU.mult),
                         reads=[T_t0[ti], T_gta], writes=[T_t0[ti]])
                    S.op("pool", lambda e, ti=ti, wi=wi, tt=tt, dsl=dsl: e.tensor_tensor(out=x1g[:, tt, dsl], in0=t0[ti][:], in1=xin[wi][:, tt, :], op=ALU.add),
                         reads=[T_t0[ti], T_xin[wi]], writes=[T_x1g[tt]] if dmb == 0 else (), pwrites=() if dmb == 0 else [T_x1g[tt]])
            S.op("sp", lambda e, gsl=gsl: e.dma_start(out=x1d[gsl, :].rearrange("(t p) c -> p t c", p=128), in_=x1g[:]),
                 reads=T_x1g, pwrites=[T_x1d], dsem=d_x1g)
            for tt in range(GT):
                t = g * GT + tt
                S.op("act", lambda e, tt=tt, t=t: e.activation(out=xsb[:, tt, :], in_=x1g[:, tt, :], func=AF.Square, accum_out=st5[:, t:t + 1]),
                     reads=[T_x1g[tt]], writes=[T_xsb[tt], T_st5[t]])
                S.op("dve", lambda e, t=t: e.tensor_scalar(out=st5[:, t:t + 1], in0=st5[:, t:t + 1], scalar1=1.0 / D, scalar2=EPS, op0=ALU.mult, op1=ALU.add),
                     reads=[T_st5[t]], writes=[T_st5[t]])
                S.op("act", lambda e, t=t: e.activation(out=st5[:, t:t + 1], in_=st5[:, t:t + 1], func=AF.Sqrt), reads=[T_st5[t]], writes=[T_st5[t]])
                S.op("dve", lambda e, t=t: e.reciprocal(out=st5[:, t:t + 1], in_=st5[:, t:t + 1]), reads=[T_st5[t]], writes=[T_st5[t]])
                S.op("act", lambda e, tt=tt, t=t: e.activation(out=xsb[:, tt, :], in_=x1g[:, tt, :], func=AF.Copy, scale=st5[:, t:t + 1]),
                     reads=[T_x1g[tt], T_st5[t]], writes=[T_xsb[tt]])
            for dc in range(NDC):
                kk = 4 + (dc % 2)
                tpv = PB[kk][:].bitcast(BF16)
                for tt in range(GT):
                    S.op("pe", lambda e, tpv=tpv, tt=tt, dc=dc: e.transpose(tpv[:, tt * 128:(tt + 1) * 128], xsb[:, tt, dc * 128:(dc + 1) * 128], ident_b[:]),
                         reads=[T_xsb[tt], T_ident], writes=[T_PB[kk]] if tt == 0 else (), pwrites=() if tt == 0 else [T_PB[kk]])
                if dc % 2 == 0:
                    S.op("dve", lambda e, tpv=tpv, dc=dc: e.tensor_scalar(out=hfs[:, dc, :], in0=tpv[:, 0:GW], scalar1=modT[:, 224 + dc:225 + dc],
                                                                          scalar2=modT[:, 96 + dc:97 + dc], op0=ALU.mult, op1=ALU.add),
                         reads=[T_PB[kk], T_modT], writes=[T_hfs] if dc == 0 else (), pwrites=() if dc == 0 else [T_hfs])
                else:
                    S.op("act", lambda e, tpv=tpv, dc=dc: e.activation(out=hfs[:, dc, :], in_=tpv[:, 0:GW], func=AF.Identity,
                                                                       scale=modT[:, 224 + dc:225 + dc], bias=modT[:, 96 + dc:97 + dc]),
                         reads=[T_PB[kk], T_modT], pwrites=[T_hfs])
            S.op("sp", lambda e, gsl=gsl: e.dma_start(out=hfT[:, :, gsl].rearrange("c p t -> p c t"), in_=hfs[:]),
                 reads=[T_hfs], pwrites=[T_hfT], dsem=d_hfs)
            for tt in range(GT):
                t = g * GT + tt
                for dc in range(NDC):
                    S.op("pe", lambda e, dc=dc, tt=tt: e.matmul(PB[6][:, 0:NE], lhsT=hfs[:, dc, tt * 128:(tt + 1) * 128], rhs=rw[:, dc, :],
                                                                start=(dc == 0), stop=(dc == NDC - 1)), reads=[T_hfs, T_rw], writes=[T_PB[6]])
                S.op("act", lambda e: e.activation(out=k_sc[:], in_=PB[6][:, 0:NE], func=AF.Sigmoid), reads=[T_PB[6]], writes=[T_k])
                K = dict(reads=[T_k], writes=[T_k])
                v3 = lambda a: a[:].rearrange("p (g c) -> p g c", c=8)
                b3 = lambda a: a[:].unsqueeze(2).to_broadcast([128, 8, 8])
                S.op("dve", lambda e: e.tensor_tensor(out=k_sel[:], in0=k_sc[:], in1=rbias[:], op=ALU.add), reads=[T_k, T_rb], writes=[T_k])
                S.op("dve", lambda e: e.reduce_max(out=k_m1[:], in_=v3(k_sel), axis=AX.X), **K)
                S.op("dve", lambda e: e.tensor_tensor(out=v3(k_eq), in0=v3(k_sel), in1=b3(k_m1), op=ALU.is_equal), **K)
                S.op("dve", lambda e: e.scalar_tensor_tensor(out=k_cur[:], in0=k_eq[:], scalar=-BIGK, in1=k_sel[:], op0=ALU.mult, op1=ALU.add), **K)
                S.op("dve", lambda e: e.reduce_max(out=k_m2[:], in_=v3(k_cur), axis=AX.X), **K)
                S.op("dve", lambda e: e.tensor_tensor(out=k_gs[:], in0=k_m1[:], in1=k_m2[:], op=ALU.add), **K)
                S.op("dve", lambda e: e.tensor_copy(out=k_gc[:], in_=k_gs[:]), **K)
                for _ in range(3):
                    S.op("dve", lambda e: e.reduce_max(out=k_mx[:], in_=k_gc[:], axis=AX.X), **K)
                    S.op("dve", lambda e: e.tensor_scalar(out=k_ge[:], in0=k_gc[:], scalar1=k_mx[:, 0:1], scalar2=None, op0=ALU.is_equal), **K)
                    S.op("dve", lambda e: e.scalar_tensor_tensor(out=k_gc[:], in0=k_ge[:], scalar=-BIGK, in1=k_gc[:], op0=ALU.mult, op1=ALU.add), **K)
                S.op("dve", lambda e: e.reduce_max(out=k_mx[:], in_=k_gc[:], axis=AX.X), **K)
                S.op("dve", lambda e: e.tensor_scalar(out=k_ge[:], in0=k_gs[:], scalar1=k_mx[:, 0:1], scalar2=None, op0=ALU.is_ge), **K)
                S.op("dve", lambda e: e.tensor_scalar(out=k_ge[:], in0=k_ge[:], scalar1=-1.0, scalar2=BIGK, op0=ALU.add, op1=ALU.mult), **K)
                S.op("dve", lambda e: e.tensor_tensor(out=v3(k_sel), in0=v3(k_sel), in1=b3(k_ge), op=ALU.add), **K)
                S.op("dve", lambda e: e.tensor_copy(out=k_cur[:], in_=k_sel[:]), **K)
                for _ in range(5):
                    S.op("dve", lambda e: e.reduce_max(out=k_mx[:], in_=k_cur[:], axis=AX.X), **K)
                    S.op("dve", lambda e: e.tensor_scalar(out=k_eq[:], in0=k_cur[:], scalar1=k_mx[:, 0:1], scalar2=None, op0=ALU.is_equal), **K)
                    S.op("dve", lambda e: e.scalar_tensor_tensor(out=k_cur[:], in0=k_eq[:], scalar=-BIGK, in1=k_cur[:], op0=ALU.mult, op1=ALU.add), **K)
                S.op("dve", lambda e: e.reduce_max(out=k_mx[:], in_=k_cur[:], axis=AX.X), **K)
                S.op("dve", lambda e: e.tensor_scalar(out=k_eq[:], in0=k_sel[:], scalar1=k_mx[:, 0:1], scalar2=None, op0=ALU.is_ge), **K)
                S.op("dve", lambda e: e.tensor_tensor(out=k_sc[:], in0=k_sc[:], in1=k_eq[:], op=ALU.mult), **K)
                S.op("dve", lambda e: e.reduce_sum(out=k_mx[:], in_=k_sc[:], axis=AX.X), **K)
                S.op("dve", lambda e: e.reciprocal(out=k_mx[:], in_=k_mx[:]), **K)
                S.op("dve", lambda e, t=t: e.tensor_scalar(out=wts[:, t, :], in0=k_sc[:], scalar1=k_mx[:, 0:1], scalar2=2.5, op0=ALU.mult, op1=ALU.mult),
                     reads=[T_k], writes=[T_wts[t]])
        S.barrier()
        p5.close()

    phase5()
    if stage <= 5:
        if "wts_dbg" in debug_out:
            wd = scratch("wts_dbg", [128, NT, NE], F32)
            S.op("sp", lambda e: e.dma_start(out=wd, in_=wts[:]), reads=T_wts, dsem=Tl())
            S.barrier()
        S.emit()
        es.close()
        return nc
    accd = scratch("accd", [SEQ, D], F32)
    T_acc = [[Tl(), Tl()] for _ in range(NT)]

    def phase6():
        p6 = ExitStack()
        hfb = [sb(f"m_h{i}", [128, NDC, 512], BF16, p6) for i in range(2)]
        w1h = [sb(f"m_w1{i}", [128, NDC, 256], BF16, p6) for i in range(2)]
        w3h = [sb(f"m_w3{i}", [128, NDC, 256], BF16, p6) for i in range(2)]
        w2h = [sb(f"m_w2{i}", [128, 4, 2048], BF16, p6) for i in range(2)]
        aT = sb("m_a", [128, 4, SEQ], BF16, p6)
        sil = [sb(f"m_s{i}", [128, 512], F32, p6) for i in range(2)]
        yst = [sb(f"m_y{i}", [128, 2048], F32, p6) for i in range(2)]
        T_hfb = [Tl(), Tl()]; T_w1h = [Tl(), Tl()]; T_w3h = [Tl(), Tl()]; T_w2h = [Tl(), Tl()]
        T_aT = [[Tl() for _ in range(4)] for _ in range(4)]
        T_sil = [Tl(), Tl()]; T_yst = [Tl(), Tl()]
        d_hfb = [S.dsem("mh0"), S.dsem("mh1")]
        d_w1h = [S.dsem("mw10"), S.dsem("mw11")]; d_w3h = [S.dsem("mw30"), S.dsem("mw31")]; d_w2h = [S.dsem("mw20"), S.dsem("mw21")]
        d_yst = [S.dsem("my0"), S.dsem("my1")]
        NEX = NE + 1
        c = {"s1": 0, "sl": 0, "s2": 0, "y": 0, "ev": 0}
        if "sbuf_report" in debug_out:
            print("phase6 sbuf remaining", nc.sbuf_bytes_remaining)

        def wsrc(ex_):
            if ex_ < NE:
                return W["exp_w1"][ex_], W["exp_w3"][ex_], W["exp_w2"][ex_]
            return W["shared_w1"], W["shared_w3"], W["shared_w2"]

        def load_w13(ex_, hh):
            s1_, s3_, _ = wsrc(ex_)
            cs = slice(hh * 256, (hh + 1) * 256)
            S.op("pool", lambda e: e.dma_start(out=w1h[hh][:], in_=s1_[:, cs].rearrange("(j p) f -> p j f", p=128)), writes=[T_w1h[hh]], dsem=d_w1h[hh])
            S.op("pool", lambda e: e.dma_start(out=w3h[hh][:], in_=s3_[:, cs].rearrange("(j p) f -> p j f", p=128)), writes=[T_w3h[hh]], dsem=d_w3h[hh])

        def load_w2(ex_, hh):
            _, _, s2_ = wsrc(ex_)
            for j in range(4):
                S.op("pool", lambda e, j=j: e.dma_start(out=w2h[hh][:, j, :], in_=s2_[j * 128:(j + 1) * 128, hh * 2048:(hh + 1) * 2048], max_dma_last_dim=8192),
                     writes=[T_w2h[hh]] if j == 0 else (), pwrites=() if j == 0 else [T_w2h[hh]], dsem=d_w2h[hh])

        def load_hfb(k):
            tb = k % 4
            hi = k % 2
            S.op("sp", lambda e: e.dma_start(out=hfb[hi][:], in_=hfT[:, :, tb * 512:(tb + 1) * 512].rearrange("c p t -> p c t")),
                 reads=[T_hfT], writes=[T_hfb[hi]], dsem=d_hfb[hi])

        def s1_groups(k):
            tb = k % 4
            hi = k % 2
            out = []
            for fc in range(4):
                hh, co = fc // 2, (fc % 2) * 128
                k1 = c["s1"] % 2
                c["s1"] += 1
                b1, b3 = (0, 1) if k1 == 0 else (2, 3)

                def g1(fc=fc, hh=hh, co=co, b1=b1):
                    for dc in range(NDC):
                        S.op("pe", lambda e, dc=dc: e.matmul(PB[b1][:, :], lhsT=w1h[hh][:, dc, co:co + 128], rhs=hfb[hi][:, dc, :],
                                                             start=(dc == 0), stop=(dc == NDC - 1)),
                             reads=[T_w1h[hh], T_hfb[hi]], writes=[T_PB[b1]])

                def g3(fc=fc, hh=hh, co=co, b1=b1, b3=b3):
                    for dc in range(NDC):
                        S.op("pe", lambda e, dc=dc: e.matmul(PB[b3][:, :], lhsT=w3h[hh][:, dc, co:co + 128], rhs=hfb[hi][:, dc, :],
                                                             start=(dc == 0), stop=(dc == NDC - 1)),
                             reads=[T_w3h[hh], T_hfb[hi]], writes=[T_PB[b3]])
                    si = c["sl"] % 2
                    c["sl"] += 1
                    S.op("act", lambda e: e.activation(out=sil[si][:], in_=PB[b1][:, :], func=AF.Silu), reads=[T_PB[b1]], writes=[T_sil[si]])
                    S.op("dve", lambda e: e.tensor_tensor(out=aT[:, fc, tb * 512:(tb + 1) * 512], in0=sil[si][:], in1=PB[b3][:, :], op=ALU.mult),
                         reads=[T_sil[si], T_PB[b3]], writes=[T_aT[tb][fc]])
                out += [g1, g3]
            return out

        def s2_fills(k):
            ex, tb = k // 4, k % 4
            out = []
            for half in range(2):
                for tt in range(4):
                    def f(half=half, tt=tt):
                        t = tb * 4 + tt
                        yi = c["y"] % 2
                        c["y"] += 1
                        for q4 in range(4):
                            bk = 4 + (c["s2"] % 4)
                            c["s2"] += 1
                            for fc in range(4):
                                S.op("pe", lambda e, bk=bk, fc=fc, q4=q4: e.matmul(
                                    PB[bk][:, :], lhsT=aT[:, fc, tb * 512 + tt * 128:tb * 512 + (tt + 1) * 128], rhs=w2h[half][:, fc, q4 * 512:(q4 + 1) * 512],
                                    start=(fc == 0), stop=(fc == 3)), reads=[T_aT[tb][fc], T_w2h[half]], writes=[T_PB[bk]])
                            wr = [T_yst[yi]] if q4 == 0 else []
                            pw = [] if q4 == 0 else [T_yst[yi]]
                            use_act = (c["ev"] % 2 == 0)
                            c["ev"] += 1
                            osl = slice(q4 * 512, (q4 + 1) * 512)
                            if ex < NE:
                                if use_act:
                                    S.op("act", lambda e, bk=bk, osl=osl: e.activation(out=yst[yi][:, osl], in_=PB[bk][:, :], func=AF.Copy, scale=wts[:, t, ex:ex + 1]),
                                         reads=[T_PB[bk], T_wts[t]], writes=wr, pwrites=pw)
                                else:
                                    S.op("dve", lambda e, bk=bk, osl=osl: e.tensor_scalar(out=yst[yi][:, osl], in0=PB[bk][:, :], scalar1=wts[:, t, ex:ex + 1], scalar2=None, op0=ALU.mult),
                                         reads=[T_PB[bk], T_wts[t]], writes=wr, pwrites=pw)
                            else:
                                if use_act:
                                    S.op("act", lambda e, bk=bk, osl=osl: e.copy(out=yst[yi][:, osl], in_=PB[bk][:, :]), reads=[T_PB[bk]], writes=wr, pwrites=pw)
                                else:
                                    S.op("dve", lambda e, bk=bk, osl=osl: e.tensor_copy(out=yst[yi][:, osl], in_=PB[bk][:, :]), reads=[T_PB[bk]], writes=wr, pwrites=pw)
                        dst = accd[t * 128:(t + 1) * 128, half * 2048:(half + 1) * 2048]
                        if ex == 0:
                            S.op("pool", lambda e: e.dma_start(out=dst, in_=yst[yi][:]), reads=[T_yst[yi]], writes=[T_acc[t][half]], dsem=d_yst[yi])
                        else:
                            S.op("pool", lambda e: e.dma_start(out=dst, in_=yst[yi][:], accum_op=ALU.add), reads=[T_yst[yi]], writes=[T_acc[t][half]], dsem=d_yst[yi])
                    out.append(f)
            return out

        nslots = NEX * 4
        load_w13(0, 0); load_w13(0, 1); load_w2(0, 0); load_w2(0, 1)
        load_hfb(0)
        prev = None
        for k in range(nslots):
            ex, tb = k // 4, k % 4
            if k + 1 < nslots:
                load_hfb(k + 1)
            g = s1_groups(k)
            f2 = s2_fills(prev) if prev is not None else []
            for i in range(8):
                g[i]()
                if tb == 3 and ex + 1 < NEX and i == 3:
                    load_w13(ex + 1, 0)
                if f2:
                    f2[i]()
                    pex, ptb = prev // 4, prev % 4
                    if ptb == 3 and pex + 1 < NEX and i == 3:
                        load_w2(pex + 1, 0)
                    if ptb == 3 and pex + 1 < NEX and i == 7:
                        load_w2(pex + 1, 1)
            if tb == 3 and ex + 1 < NEX:
                load_w13(ex + 1, 1)
            prev = k
        for f in s2_fills(prev):
            f()
        S.barrier()
        p6.close()

    phase6()
    if stage <= 6:
        S.emit()
        es.close()
        return nc

    def phase7():
        p7 = ExitStack()
        gtf = sb("f_gtf", [128, D], F32, p7)
        fnb = sb("f_fn", [128, D], F32, p7)
        xa = [sb(f"f_x{i}", [128, D], F32, p7) for i in range(2)]
        ac = [sb(f"f_a{i}", [128, D], F32, p7) for i in range(2)]
        jk = sb("f_jk", [128, D], BF16, p7)
        st7 = sb("f_st", [128, NT], F32, p7)
        T_gtf = Tl(); T_fnb = Tl(); T_xa = [Tl(), Tl()]; T_ac = [Tl(), Tl()]; T_jk = Tl(); T_st7 = [Tl() for _ in range(NT)]
        d_xa = [S.dsem("fx0"), S.dsem("fx1")]; d_ac = [S.dsem("fa0"), S.dsem("fa1")]
        T_out = Tl()
        S.op("sp", lambda e: e.dma_start(out=gtf[:], in_=modv[5].partition_broadcast(128)), reads=[T_modv], writes=[T_gtf], dsem=T_gtf)
        S.op("sp", lambda e: e.dma_start(out=fnb[:], in_=W["final_norm"].partition_broadcast(128)), writes=[T_fnb], dsem=T_fnb)
        for t in range(NT):
            i = t % 2
            rows = slice(t * 128, (t + 1) * 128)
            S.op("sp", lambda e, i=i, rows=rows: e.dma_start(out=xa[i][:], in_=x1d[rows, :]), reads=[T_x1d], writes=[T_xa[i]], dsem=d_xa[i])
            S.op("sp", lambda e, i=i, rows=rows: e.dma_start(out=ac[i][:], in_=accd[rows, :]), reads=T_acc[t], writes=[T_ac[i]], dsem=d_ac[i])
            S.op("dve", lambda e, i=i: e.tensor_tensor(out=ac[i][:], in0=ac[i][:], in1=gtf[:], op=ALU.mult), reads=[T_ac[i], T_gtf], writes=[T_ac[i]])
            S.op("pool", lambda e, i=i: e.tensor_tensor(out=xa[i][:], in0=xa[i][:], in1=ac[i][:], op=ALU.add), reads=[T_xa[i], T_ac[i]], writes=[T_xa[i]])
            S.op("act", lambda e, i=i, t=t: e.activation(out=jk[:], in_=xa[i][:], func=AF.Square, accum_out=st7[:, t:t + 1]),
                 reads=[T_xa[i]], writes=[T_jk, T_st7[t]])
            S.op("dve", lambda e, t=t: e.tensor_scalar(out=st7[:, t:t + 1], in0=st7[:, t:t + 1], scalar1=1.0 / D, scalar2=EPS, op0=ALU.mult, op1=ALU.add),
                 reads=[T_st7[t]], writes=[T_st7[t]])
            S.op("act", lambda e, t=t: e.activation(out=st7[:, t:t + 1], in_=st7[:, t:t + 1], func=AF.Sqrt), reads=[T_st7[t]], writes=[T_st7[t]])
            S.op("dve", lambda e, t=t: e.reciprocal(out=st7[:, t:t + 1], in_=st7[:, t:t + 1]), reads=[T_st7[t]], writes=[T_st7[t]])
            S.op("act", lambda e, i=i, t=t: e.activation(out=ac[i][:], in_=xa[i][:], func=AF.Copy, scale=st7[:, t:t + 1]),
                 reads=[T_xa[i], T_st7[t]], writes=[T_ac[i]])
            S.op("dve", lambda e, i=i: e.tensor_tensor(out=ac[i][:], in0=ac[i][:], in1=fnb[:], op=ALU.mult), reads=[T_ac[i], T_fnb], writes=[T_ac[i]])
            S.op("sp", lambda e, i=i, rows=rows: e.dma_start(out=out_d[rows, :], in_=ac[i][:]), reads=[T_ac[i]], pwrites=[T_out], dsem=d_ac[i])
        S.barrier()
        p7.close()

    phase7()
    S.emit()
    es.close()
    return nc


RMAT = np.zeros((128, 64), np.float32)
for _m in range(32):
    RMAT[_m + 32, _m] = -1.0
    RMAT[_m, _m + 32] = 1.0

IMPLEMENTED_STAGE = 99
STAGE_WEIGHTS = ["w_ada", "b_ada", "norm_attn", "norm_ffn", "w_in"]


def make_in_maps(inputs, names=None):
    names = WEIGHT_NAMES if names is None else names
    maps = []
    ws = {n: np.ascontiguousarray(np.asarray(inputs[n])[0] if n != "final_norm" else np.asarray(inputs[n]), dtype=np.float32)
          for n in names}
    x = np.asarray(inputs["x"], dtype=np.float32)
    c = np.asarray(inputs["c"], dtype=np.float32)
    pos = np.asarray(inputs["positions"], dtype=np.int32)
    ident = np.eye(128, dtype=np.float32)
    for b in range(8):
        m = dict(ws)
        m["x"] = np.ascontiguousarray(x[b])
        m["c"] = np.ascontiguousarray(c[b])
        m["pos"] = np.ascontiguousarray(pos[b])
        m["ident"] = ident
        m["rmat"] = RMAT
        maps.append(m)
    return maps


def kernel(**inputs):
    nc = build(stage=IMPLEMENTED_STAGE)
    names = stage_weights(IMPLEMENTED_STAGE)
    res = run_bass_kernel_spmd(nc, make_in_maps(inputs, names), core_ids=list(range(8)))
    return np.stack([np.asarray(r["out"]).reshape(SEQ, D) for r in res.results], axis=0).astype(np.float32)
```
